# Optimizing a Trainium2 kernel written in Bass

```python
import math
import numpy as np
import jax
import jax.numpy as jnp
from jax import lax

D_MODEL = 4096
BATCH = 4
SEQ = 4096
DEPTH = 1

RMS_EPS = 1e-6
NEG_INF = -1e30
FORCE_SCORE = 1e6

GDN_HEADS = 16
GDN_HEAD_DIM = 128
GDN_WIDTH = GDN_HEADS * GDN_HEAD_DIM
GDN_CONV = 4
GDN_CHUNK = 64

NSA_HEADS = 16
NSA_GROUPS = 4
NSA_HPG = NSA_HEADS // NSA_GROUPS
NSA_HEAD_DIM = 128
NSA_WIDTH = NSA_HEADS * NSA_HEAD_DIM
NSA_KV_WIDTH = NSA_GROUPS * NSA_HEAD_DIM
CMP_BLOCK = 32
CMP_STRIDE = 16
SEL_BLOCK = 64
SEL_TOP_N = 16
WINDOW = 512
NSA_Q_BLOCK = 32

N_GROUPS = 8
EXPERTS_PER_GROUP = 8
N_EXPERTS = N_GROUPS * EXPERTS_PER_GROUP
TOP_K = 2
D_FF = 768
MOE_BLOCK = 128

IN_SIZES = (3 * GDN_WIDTH,
            GDN_WIDTH,
            GDN_HEADS,
            GDN_HEADS,
            NSA_WIDTH,
            6 * NSA_KV_WIDTH,
            3 * NSA_HEADS,
            2 * D_MODEL)
IN_COLS = sum(IN_SIZES)

kernel_name = 'hybrid_gdn_nsa_hmoe_block'


def rms_norm(x, gain):
    xf = x.astype(jnp.float32)
    y = xf * lax.rsqrt(jnp.mean(jnp.square(xf), axis=-1, keepdims=True) + RMS_EPS)
    return (y * gain.astype(jnp.float32)).astype(x.dtype)


def l2_normalize(x):
    xf = x.astype(jnp.float32)
    return xf * lax.rsqrt(jnp.sum(jnp.square(xf), axis=-1, keepdims=True) + RMS_EPS)


def causal_depthwise_conv(x, w):
    width = w.shape[0]
    return lax.conv_general_dilated(x, w[:, None, :].astype(x.dtype), window_strides=(1,),
                                    padding=((width - 1, 0),), dimension_numbers=('NWC', 'WIO', 'NWC'),
                                    feature_group_count=x.shape[-1])


def masked_softmax(scores, mask):
    s = jnp.where(mask, scores.astype(jnp.float32), NEG_INF)
    return jnp.where(mask, jax.nn.softmax(s, axis=-1), 0.0)


def gated_delta_rule_chunked(q, k, v, g, beta):
    B, H, T, dk = k.shape
    dv = v.shape[-1]
    C = GDN_CHUNK
    N = T // C
    q, k, v = (t.reshape(B, H, N, C, t.shape[-1]) for t in (q, k, v))
    g = g.reshape(B, H, N, C)
    beta = beta.reshape(B, H, N, C)
    gc = jnp.cumsum(g, axis=-1)
    incl = jnp.tril(jnp.ones((C, C), bool))
    strict = jnp.tril(jnp.ones((C, C), bool), -1)
    decay = jnp.exp(jnp.where(incl, gc[..., :, None] - gc[..., None, :], -jnp.inf))
    k_beta = k * beta[..., None]
    lower = jnp.where(strict, jnp.einsum('bhncd,bhnsd->bhncs', k_beta, k) * decay, 0.0)
    rhs = jnp.concatenate([v * beta[..., None], k_beta * jnp.exp(gc)[..., None]], axis=-1)
    sol = lax.linalg.triangular_solve(lower + jnp.eye(C, dtype=lower.dtype), rhs,
                                      left_side=True, lower=True, unit_diagonal=True)
    u, w = sol[..., :dv], sol[..., dv:]
    attn = jnp.where(incl, jnp.einsum('bhncd,bhnsd->bhncs', q, k) * decay, 0.0)
    q_in = q * jnp.exp(gc)[..., None]
    k_out = k * jnp.exp(gc[..., -1:] - gc)[..., None]
    chunk_decay = jnp.exp(gc[..., -1])

    def step(state, xs):
        q_i, k_i, u_i, w_i, a_i, d_i = xs
        v_new = u_i - jnp.einsum('bhcd,bhde->bhce', w_i, state)
        o_i = jnp.einsum('bhcd,bhde->bhce', q_i, state) + jnp.einsum('bhcs,bhse->bhce', a_i, v_new)
        state = state * d_i[..., None, None] + jnp.einsum('bhcd,bhce->bhde', k_i, v_new)
        return state, o_i

    xs = tuple(jnp.moveaxis(t, 2, 0) for t in (q_in, k_out, u, w, attn, chunk_decay))
    _, o = lax.scan(step, jnp.zeros((B, H, dk, dv), jnp.float32), xs)
    return jnp.moveaxis(o, 0, 2).reshape(B, H, T, dv)


def gdn_mixer(qkv, z, a_raw, b_raw, conv_w, a_log, dt_bias, norm_w):
    B, T, _ = qkv.shape
    H, d = GDN_HEADS, GDN_HEAD_DIM
    qkv = jax.nn.silu(causal_depthwise_conv(qkv, conv_w))
    q, k, v = jnp.split(qkv, 3, axis=-1)
    heads = lambda t: t.reshape(B, T, H, d).transpose(0, 2, 1, 3)
    q = l2_normalize(heads(q)) * (d ** -0.5)
    k = l2_normalize(heads(k))
    v = heads(v).astype(jnp.float32)
    beta = jax.nn.sigmoid(b_raw.astype(jnp.float32)).transpose(0, 2, 1)
    g = (-jnp.exp(a_log.astype(jnp.float32))
         * jax.nn.softplus(a_raw.astype(jnp.float32) + dt_bias.astype(jnp.float32))).transpose(0, 2, 1)
    o = gated_delta_rule_chunked(q, k, v, g, beta).transpose(0, 2, 1, 3)
    o = rms_norm(o, norm_w) * jax.nn.silu(z.reshape(B, T, H, d).astype(jnp.float32))
    return o.reshape(B, T, GDN_WIDTH).astype(qkv.dtype)


def nsa_compress(blocks, pe, w1, w2):
    hid = jax.nn.silu(jnp.einsum('bgnld,lde->bgne', blocks + pe.astype(blocks.dtype), w1))
    return jnp.einsum('bgne,ef->bgnf', hid, w2)


def nsa_mixer(q, kv, gate_logits, pe_k, w1_k, w2_k, pe_v, w1_v, w2_v):
    B, T, _ = q.shape
    G, Hg, d, QB = NSA_GROUPS, NSA_HPG, NSA_HEAD_DIM, NSA_Q_BLOCK
    n_cmp = (T - CMP_BLOCK) // CMP_STRIDE + 1
    n_sel = T // SEL_BLOCK
    n_top = min(SEL_TOP_N, n_sel)
    n_qb = T // QB
    q = q.reshape(B, T, G, Hg, d).transpose(0, 2, 3, 1, 4) * (d ** -0.5)
    k_c, v_c, k_s, v_s, k_w, v_w = [t.reshape(B, T, G, d).transpose(0, 2, 1, 3)
                                    for t in jnp.split(kv, 6, axis=-1)]
    gates = jax.nn.sigmoid(gate_logits).reshape(B, T, G, Hg, 3).transpose(0, 2, 3, 1, 4)

    cmp_idx = np.arange(n_cmp)[:, None] * CMP_STRIDE + np.arange(CMP_BLOCK)[None, :]
    k_cmp = nsa_compress(k_c[:, :, cmp_idx], pe_k, w1_k, w2_k)
    v_cmp = nsa_compress(v_c[:, :, cmp_idx], pe_v, w1_v, w2_v)
    cmp_last = jnp.asarray(cmp_idx[:, -1], jnp.int32)
    cmp_start = np.arange(n_cmp) * CMP_STRIDE
    sel_start = np.arange(n_sel) * SEL_BLOCK
    overlap = (np.minimum(cmp_start[:, None] + CMP_BLOCK, sel_start[None, :] + SEL_BLOCK)
               - np.maximum(cmp_start[:, None], sel_start[None, :]))
    sel_map = jnp.asarray(np.clip(overlap, 0, None) / CMP_STRIDE, jnp.float32)

    k_sel_blocks = k_s.reshape(B, G, n_sel, SEL_BLOCK, d)
    v_sel_blocks = v_s.reshape(B, G, n_sel, SEL_BLOCK, d)
    k_win = jnp.pad(k_w, ((0, 0), (0, 0), (WINDOW, 0), (0, 0)))
    v_win = jnp.pad(v_w, ((0, 0), (0, 0), (WINDOW, 0), (0, 0)))
    b_idx = jnp.arange(B)[:, None, None, None]
    g_idx = jnp.arange(G)[None, :, None, None]
    blk_ids = jnp.arange(n_sel)
    in_block = jnp.arange(SEL_BLOCK)
    win_offsets = jnp.arange(WINDOW + QB) - WINDOW

    def query_block(args):
        qb, q_b, g_b = args
        t0 = qb * QB
        t = t0 + jnp.arange(QB)
        s = jnp.einsum('bghqd,bgnd->bghqn', q_b, k_cmp)
        p_cmp = masked_softmax(s, cmp_last[None, :] <= t[:, None])
        o_cmp = jnp.einsum('bghqn,bgnd->bghqd', p_cmp.astype(v_cmp.dtype), v_cmp)
        importance = jnp.einsum('bghqn,nj->bgqj', p_cmp, sel_map)
        cur = (t // SEL_BLOCK)[:, None]
        forced = (blk_ids == 0) | (blk_ids == cur) | (blk_ids == cur - 1)
        causal = blk_ids * SEL_BLOCK <= t[:, None]
        score = jnp.where(forced, FORCE_SCORE, jnp.where(causal, importance, NEG_INF))
        _, sel = lax.top_k(score, n_top)
        k_g = k_sel_blocks[b_idx, g_idx, sel].reshape(B, G, QB, n_top * SEL_BLOCK, d)
        v_g = v_sel_blocks[b_idx, g_idx, sel].reshape(B, G, QB, n_top * SEL_BLOCK, d)
        pos = (sel[..., None] * SEL_BLOCK + in_block).reshape(B, G, QB, n_top * SEL_BLOCK)
        s = jnp.einsum('bghqd,bgqkd->bghqk', q_b, k_g)
        p = masked_softmax(s, (pos <= t[:, None])[:, :, None])
        o_sel = jnp.einsum('bghqk,bgqkd->bghqd', p.astype(v_g.dtype), v_g)
        k_b = lax.dynamic_slice_in_dim(k_win, t0, WINDOW + QB, axis=2)
        v_b = lax.dynamic_slice_in_dim(v_win, t0, WINDOW + QB, axis=2)
        kpos = t0 + win_offsets
        rel = t[:, None] - kpos[None, :]
        vis = (kpos[None, :] >= 0) & (rel >= 0) & (rel < WINDOW)
        s = jnp.einsum('bghqd,bgkd->bghqk', q_b, k_b)
        p = masked_softmax(s, vis)
        o_win = jnp.einsum('bghqk,bgkd->bghqd', p.astype(v_b.dtype), v_b)
        return g_b[..., 0:1] * o_cmp + g_b[..., 1:2] * o_sel + g_b[..., 2:3] * o_win

    q_blk = q.reshape(B, G, Hg, n_qb, QB, d).transpose(3, 0, 1, 2, 4, 5)
    g_blk = gates.reshape(B, G, Hg, n_qb, QB, 3).transpose(3, 0, 1, 2, 4, 5)
    o = lax.map(query_block, (jnp.arange(n_qb), q_blk, g_blk))
    return o.transpose(1, 0, 4, 2, 3, 5).reshape(B, T, NSA_WIDTH)


def hier_moe(x, w_group, b_group, w_expert, b_expert, w_gate_up, w_down):
    n_tok, d = x.shape
    xf = x.astype(jnp.float32)
    p_group = jax.nn.softmax(xf @ w_group.astype(jnp.float32) + b_group.astype(jnp.float32), axis=-1)
    p_g, grp = lax.top_k(p_group, 1)
    e_logits = (xf @ w_expert.astype(jnp.float32) + b_expert.astype(jnp.float32)).reshape(
        n_tok, N_GROUPS, EXPERTS_PER_GROUP)
    e_logits = jnp.take_along_axis(e_logits, grp[:, :, None], axis=1)[:, 0]
    p_e, local = lax.top_k(jax.nn.softmax(e_logits, axis=-1), TOP_K)
    weight = (p_g * p_e / jnp.sum(p_e, axis=-1, keepdims=True)).astype(x.dtype)
    expert = grp * EXPERTS_PER_GROUP + local

    n_slot = n_tok * TOP_K
    e_flat = expert.reshape(n_slot)
    order = jnp.argsort(e_flat)
    e_sorted = e_flat[order]
    tok_sorted = (order // TOP_K).astype(jnp.int32)
    w_sorted = weight.reshape(n_slot)[order]
    counts = jnp.bincount(e_flat, length=N_EXPERTS)
    padded = (counts + MOE_BLOCK - 1) // MOE_BLOCK * MOE_BLOCK
    pad_end = jnp.cumsum(padded)
    dest = (pad_end - padded)[e_sorted] + jnp.arange(n_slot) - (jnp.cumsum(counts) - counts)[e_sorted]
    n_blk = -(-n_slot // MOE_BLOCK) + N_EXPERTS
    tok_buf = jnp.full((n_blk * MOE_BLOCK,), n_tok, jnp.int32).at[dest].set(tok_sorted)
    w_buf = jnp.zeros((n_blk * MOE_BLOCK,), x.dtype).at[dest].set(w_sorted)
    blk_expert = jnp.minimum(jnp.searchsorted(pad_end, jnp.arange(n_blk) * MOE_BLOCK, side='right'),
                             N_EXPERTS - 1)
    x_pad = jnp.concatenate([x, jnp.zeros((1, d), x.dtype)], axis=0)

    def expert_block(args):
        tok_b, w_b, e = args
        xb = x_pad[tok_b]
        gate, up = jnp.split(xb @ w_gate_up[e], 2, axis=-1)
        return ((jax.nn.silu(gate) * up) @ w_down[e]) * w_b[:, None]

    y_buf = lax.map(expert_block, (tok_buf.reshape(n_blk, MOE_BLOCK), w_buf.reshape(n_blk, MOE_BLOCK),
                                   blk_expert))
    return jax.ops.segment_sum(y_buf.reshape(-1, d), tok_buf, num_segments=n_tok + 1)[:n_tok]


def hybrid_layer(x, g_mix, w_in, gdn_conv_w, gdn_a_log, gdn_dt_bias, gdn_norm_w,
                 cmp_pe_k, cmp_w1_k, cmp_w2_k, cmp_pe_v, cmp_w1_v, cmp_w2_v,
                 w_branch_gdn, w_branch_nsa, w_out, g_ffn,
                 w_group, b_group, w_expert, b_expert, w_gate_up, w_down):
    B, T, D = x.shape
    xn = rms_norm(x, g_mix)
    proj = jnp.einsum('btd,de->bte', xn, w_in)
    offsets = [int(o) for o in np.cumsum(IN_SIZES)[:-1]]
    qkv, z, a_raw, b_raw, nsa_q, nsa_kv, nsa_gate, merge_gate = jnp.split(proj, offsets, axis=-1)
    o_gdn = gdn_mixer(qkv, z, a_raw, b_raw, gdn_conv_w, gdn_a_log, gdn_dt_bias, gdn_norm_w)
    o_nsa = nsa_mixer(nsa_q, nsa_kv, nsa_gate, cmp_pe_k, cmp_w1_k, cmp_w2_k, cmp_pe_v, cmp_w1_v, cmp_w2_v)
    gate_gdn, gate_nsa = jnp.split(jax.nn.sigmoid(merge_gate), 2, axis=-1)
    merged = gate_gdn * (o_gdn @ w_branch_gdn) + gate_nsa * (o_nsa @ w_branch_nsa)
    h = x + merged @ w_out
    ffn = hier_moe(rms_norm(h, g_ffn).reshape(B * T, D), w_group, b_group, w_expert, b_expert,
                   w_gate_up, w_down)
    return h + ffn.reshape(B, T, D)


def setup_inputs(seed: int = 0) -> dict:
    key = jax.random.key(seed)
    ks = jax.random.split(key, 24)
    f32 = jnp.float32
    L, D, H, d = DEPTH, D_MODEL, GDN_HEADS, NSA_HEAD_DIM

    def normal(k, shape, scale):
        return jax.random.normal(k, shape, f32) * scale

    def gain(k, shape):
        return 1.0 + 0.02 * jax.random.normal(k, shape, f32)

    dt = jnp.exp(jax.random.uniform(ks[5], (L, H), f32, math.log(1e-3), math.log(1e-1)))
    return {
        'x': jax.random.normal(ks[0], (BATCH, SEQ, D), f32),
        'g_mix': gain(ks[1], (L, D)),
        'w_in': normal(ks[2], (L, D, IN_COLS), D ** -0.5),
        'gdn_conv_w': normal(ks[3], (L, GDN_CONV, 3 * GDN_WIDTH), GDN_CONV ** -0.5),
        'gdn_a_log': jnp.log(jax.random.uniform(ks[4], (L, H), f32, 1.0, 16.0)),
        'gdn_dt_bias': dt + jnp.log(-jnp.expm1(-dt)),
        'gdn_norm_w': gain(ks[6], (L, GDN_HEAD_DIM)),
        'cmp_pe_k': normal(ks[7], (L, CMP_BLOCK, d), 0.02),
        'cmp_w1_k': normal(ks[8], (L, CMP_BLOCK, d, d), (CMP_BLOCK * d) ** -0.5),
        'cmp_w2_k': normal(ks[9], (L, d, d), d ** -0.5),
        'cmp_pe_v': normal(ks[10], (L, CMP_BLOCK, d), 0.02),
        'cmp_w1_v': normal(ks[11], (L, CMP_BLOCK, d, d), (CMP_BLOCK * d) ** -0.5),
        'cmp_w2_v': normal(ks[12], (L, d, d), d ** -0.5),
        'w_branch_gdn': normal(ks[13], (L, GDN_WIDTH, D), GDN_WIDTH ** -0.5),
        'w_branch_nsa': normal(ks[14], (L, NSA_WIDTH, D), NSA_WIDTH ** -0.5),
        'w_out': normal(ks[15], (L, D, D), D ** -0.5),
        'g_ffn': gain(ks[16], (L, D)),
        'w_group': normal(ks[17], (L, D, N_GROUPS), D ** -0.5),
        'b_group': normal(ks[18], (L, N_GROUPS), 0.01),
        'w_expert': normal(ks[19], (L, D, N_EXPERTS), D ** -0.5),
        'b_expert': normal(ks[20], (L, N_EXPERTS), 0.01),
        'w_gate_up': normal(ks[21], (L, N_EXPERTS, D, 2 * D_FF), D ** -0.5),
        'w_down': normal(ks[22], (L, N_EXPERTS, D_FF, D), D_FF ** -0.5),
        'g_final': gain(ks[23], (D,)),
    }


def reference(x, g_mix, w_in, gdn_conv_w, gdn_a_log, gdn_dt_bias, gdn_norm_w,
              cmp_pe_k, cmp_w1_k, cmp_w2_k, cmp_pe_v, cmp_w1_v, cmp_w2_v,
              w_branch_gdn, w_branch_nsa, w_out, g_ffn,
              w_group, b_group, w_expert, b_expert, w_gate_up, w_down, g_final):
    h = x
    for layer in range(DEPTH):
        h = hybrid_layer(h, g_mix[layer], w_in[layer], gdn_conv_w[layer], gdn_a_log[layer],
                         gdn_dt_bias[layer], gdn_norm_w[layer],
                         cmp_pe_k[layer], cmp_w1_k[layer], cmp_w2_k[layer],
                         cmp_pe_v[layer], cmp_w1_v[layer], cmp_w2_v[layer],
                         w_branch_gdn[layer], w_branch_nsa[layer], w_out[layer], g_ffn[layer],
                         w_group[layer], b_group[layer], w_expert[layer], b_expert[layer],
                         w_gate_up[layer], w_down[layer])
    return rms_norm(h, g_final)
```

```python
from contextlib import ExitStack
import numpy as np
import concourse.bass as bass
import concourse.mybir as mybir
from concourse.bass_utils import run_bass_kernel_spmd

F32 = mybir.dt.float32
BF16 = mybir.dt.bfloat16
I32 = mybir.dt.int32
AF = mybir.ActivationFunctionType
ALU = mybir.AluOpType
AX = mybir.AxisListType

SEM_LIM = 24000
D = 4096
TL = 4096
OWN0 = 2048
NCOL = 21584
EPS = 1e-6
C_QKV, C_Z, C_A, C_B, C_NQ, C_NKV, C_NG, C_MG = 0, 6144, 8192, 8208, 8224, 10272, 13344, 13392
NEG = -30000.0


class Prog:
    ENGS = ("pe", "act", "dve", "pool", "sp")
    NDMA = {"sp": 16, "pool": 8, "act": 4}

    def __init__(self, nc):
        self.nc = nc
        self.ops = []
        self.last_w = {}
        self.readers = {}
        self.dma_count = {"sp": 0, "pool": 0, "act": 0}
        self.dma_ops = {"sp": [], "pool": [], "act": []}
        self.last_on = {}
        self.barrier_idx = None

    def op(self, eng, fn, reads=(), writes=(), dma=False):
        idx = len(self.ops)
        deps = set()
        if self.barrier_idx is not None:
            deps.add(self.barrier_idx)
        for r in reads:
            w = self.last_w.get(r)
            if w is not None:
                deps.add(w)
        for w_ in writes:
            w = self.last_w.get(w_)
            if w is not None:
                deps.add(w)
            for r in self.readers.get(w_, ()):
                deps.add(r)
        o = dict(eng=eng, fn=fn, deps=deps, dma=dma, idx=idx, marked=False)
        if dma:
            n = self.dma_count[eng]
            self.dma_count[eng] = n + 1
            nd = self.NDMA[eng]
            o["dma_n"] = n
            lst = self.dma_ops[eng]
            if n >= nd:
                deps.add(lst[n - nd])
            lst.append(idx)
        else:
            self.last_on[eng] = idx
        deps.discard(idx)
        self.ops.append(o)
        for r in reads:
            self.readers.setdefault(r, []).append(idx)
        for w_ in writes:
            self.last_w[w_] = idx
            self.readers[w_] = []
        return idx

    def dma(self, out, in_, reads=(), writes=(), q="sp", **kw):
        return self.op(q, lambda e: e.dma_start(out=out, in_=in_, **kw), reads, writes, dma=True)

    def barrier(self):
        deps = set(self.last_on.values())
        for q, lst in self.dma_ops.items():
            deps.update(lst[-self.NDMA[q]:])
        idx = self.op("sp", lambda e: e.nop(), (), ())
        self.ops[idx]["deps"] |= deps
        self.ops[idx]["deps"].discard(idx)
        self.barrier_idx = idx
        self.last_w = {}
        self.readers = {}

    def emit(self):
        nc = self.nc
        ops = self.ops
        for o in ops:
            for d in o["deps"]:
                p = ops[d]
                if p["eng"] == "pe" and o["eng"] == "pe" and not p["dma"] and not o["dma"]:
                    continue
                p["marked"] = True
        cnt = {e: 0 for e in self.ENGS}
        for o in ops:
            if o["dma"]:
                nd = self.NDMA[o["eng"]]
                o["tok"] = ("dma_" + o["eng"], o["dma_n"] % nd, 16 * (o["dma_n"] // nd + 1))
            elif o["marked"]:
                m = cnt[o["eng"]]
                cnt[o["eng"]] = m + 1
                o["tok"] = (o["eng"], m // SEM_LIM, m % SEM_LIM + 1)
            else:
                o["tok"] = None
        sems = {}
        for e in self.ENGS:
            for j in range(cnt[e] // SEM_LIM + 1):
                sems[(e, j)] = nc.alloc_semaphore(name=f"s_{e}_{j}")
        for q, nd in self.NDMA.items():
            if self.dma_count[q] > 0:
                for j in range(nd):
                    sems[("dma_" + q, j)] = nc.alloc_semaphore(name=f"d_{q}_{j}")
        final_dma = {}
        for o in ops:
            if o["dma"]:
                t = o["tok"]
                final_dma[(t[0], t[1])] = max(final_dma.get((t[0], t[1]), 0), t[2])

        def run_engine(ename, eobj, is_last_waiter=False):
            waited = {}
            for o in ops:
                if o["eng"] != ename:
                    continue
                for d in sorted(o["deps"]):
                    p = ops[d]
                    if p["eng"] == "pe" and ename == "pe" and not p["dma"] and not o["dma"]:
                        continue
                    t = p["tok"]
                    key = (t[0], t[1])
                    if waited.get(key, 0) >= t[2]:
                        continue
                    eobj.wait_ge(sems[key], t[2])
                    waited[key] = t[2]
                ins = o["fn"](eobj)
                t = o["tok"]
                if t is not None:
                    ins.then_inc(sems[(t[0], t[1])], 16 if o["dma"] else 1)
            if is_last_waiter:
                for key, v in final_dma.items():
                    if waited.get(key, 0) < v:
                        eobj.wait_ge(sems[key], v)

        with nc.Block() as block:
            @block.sync
            def _(e):
                run_engine("sp", e, True)

            @block.tensor
            def _(e):
                run_engine("pe", e)

            @block.scalar
            def _(e):
                run_engine("act", e)

            @block.vector
            def _(e):
                run_engine("dve", e)

            @block.gpsimd
            def _(e):
                run_engine("pool", e)


class Stage:
    def __init__(self, nc, P):
        self.nc, self.P = nc, P
        self.es = ExitStack()

    def sb(self, name, shape, dt):
        return self.es.enter_context(self.nc.sbuf_tensor(name, shape, dt)).ap()

    def ps(self, name, shape, dt=F32):
        return self.es.enter_context(self.nc.psum_tensor(name, shape, dt)).ap()

    def close(self):
        self.P.barrier()
        self.es.close()


def evac(P, k, out, in_, reads, writes):
    if k % 2 == 0:
        return P.op("act", lambda e: e.copy(out=out, in_=in_), reads, writes)
    return P.op("dve", lambda e: e.tensor_copy(out=out, in_=in_), reads, writes)


def rsqrt_col(P, out, in_, add, rkey, wkey, mul=1.0):
    P.op("dve", lambda e: e.tensor_scalar(out=out, in0=in_, scalar1=float(mul), scalar2=float(add), op0=ALU.mult, op1=ALU.add),
         reads=[rkey], writes=[wkey])
    P.op("act", lambda e: e.sqrt(out=out, in_=out), reads=[wkey], writes=[wkey])
    P.op("dve", lambda e: e.reciprocal(out=out, in_=out), reads=[wkey], writes=[wkey])


def make_consts(nc, P):
    c = {}
    c["identf"] = nc.alloc_sbuf_tensor("identf", [128, 128], F32).ap()
    c["ident"] = nc.alloc_sbuf_tensor("ident", [128, 128], BF16).ap()
    c["onesf"] = nc.alloc_sbuf_tensor("onesf", [128, 128], F32).ap()
    c["ones"] = nc.alloc_sbuf_tensor("ones", [128, 128], BF16).ap()
    P.op("pool", lambda e: e.memset(c["identf"], 0.0), writes=["identf"])
    P.op("pool", lambda e: e.affine_select(out=c["identf"], in_=c["identf"], pattern=[[-1, 128]],
                                           compare_op=ALU.not_equal, fill=1.0, base=0, channel_multiplier=1),
         reads=["identf"], writes=["identf"])
    P.op("dve", lambda e: e.tensor_copy(out=c["ident"], in_=c["identf"]), reads=["identf"], writes=["ident"])
    P.op("pool", lambda e: e.memset(c["onesf"], 1.0), writes=["onesf"])
    P.op("pool", lambda e: e.memset(c["ones"], 1.0), writes=["ones"])
    return c


def col_tiles(prefix, tb=1):
    if prefix and tb == 0:
        groups = [(C_QKV + 2048, 4096), (C_NKV, 2048)]
    elif prefix:
        groups = [(C_QKV, 6144), (C_NKV, 3072)]
    else:
        groups = [(C_QKV, 6144), (C_Z, 2048), (C_NQ, 2048), (C_NKV, 3072), (C_NG, 48), (C_MG, 8192)]
    out = []
    for c0, n in groups:
        o = 0
        while o < n:
            w = min(256, n - o)
            out.append((c0 + o, w))
            o += w
    return out


def stage_proj(nc, P, cst, x_d, gmix_d, win_d, PT_d, ab_tok, nblocks=4):
    st = Stage(nc, P)
    xt = [st.sb(f"xt{i}", [128, D], F32) for i in range(2)]
    xb = [st.sb(f"xb{i}", [128, D], BF16) for i in range(2)]
    gm = st.sb("gm", [128, D], F32)
    junk = st.sb("junk", [128, D], BF16)
    xnT = st.sb("xnT", [128, 32, 1024], BF16)
    wt = [st.sb(f"wt{i}", [128, 32, 256], BF16) for i in range(2)]
    og = [st.sb(f"og{i}", [128, 1024], F32) for i in range(2)]
    ss = st.sb("ss", [128, 2], F32)
    rs = st.sb("rs", [128, 2], F32)
    wab = st.sb("wab", [128, 32, 32], BF16)
    ptp = [st.ps(f"ptp{i}", [128, 4, 128], BF16) for i in range(2)]
    pp = [st.ps(f"pp{i}", [128, 1024], F32) for i in range(2)]
    pab = st.ps("pab", [128, 32], F32)

    P.dma(gm, gmix_d.partition_broadcast(128), writes=["gm"])
    P.op("dve", lambda e: e.tensor_scalar(out=gm, in0=gm, scalar1=float(np.sqrt(D)), scalar2=None, op0=ALU.mult),
         reads=["gm"], writes=["gm"])
    P.dma(wab, win_d[:, C_A:C_A + 32].rearrange("(c p) n -> p c n", p=128), writes=["wab"], q="pool")

    wcount = 0
    ocount = 0
    tcount = 0
    for tb in range(4 - nblocks, 4):
        prefix = tb < 2
        for t in range(8):
            i = tcount % 2
            tcount += 1
            r0 = tb * 1024 + t * 128
            P.dma(xt[i], x_d[r0:r0 + 128, :], writes=[f"xt{i}"])
            P.op("act", lambda e, i=i: e.activation(out=junk, in_=xt[i], func=AF.Square, accum_out=ss[:, i:i + 1]),
                 reads=[f"xt{i}"], writes=["junk", f"ss{i}"])
            rsqrt_col(P, rs[:, i:i + 1], ss[:, i:i + 1], float(D * EPS), f"ss{i}", f"rs{i}")
            P.op("dve", lambda e, i=i: e.scalar_tensor_tensor(out=xb[i], in0=xt[i], scalar=rs[:, i:i + 1], in1=gm,
                                                              op0=ALU.mult, op1=ALU.mult),
                 reads=[f"xt{i}", f"rs{i}", "gm"], writes=[f"xb{i}"])
            for g in range(8):
                pj = g % 2
                for j in range(4):
                    c = g * 4 + j
                    P.op("pe", lambda e, c=c, j=j, pj=pj, i=i: e.transpose(out=ptp[pj][:, j, :], in_=xb[i][:, c * 128:(c + 1) * 128],
                                                                         identity=cst["ident"]),
                         reads=[f"xb{i}", "ident"], writes=[f"ptp{pj}"])
                evac(P, g, xnT[:, g * 4:(g + 1) * 4, t * 128:(t + 1) * 128], ptp[pj], [f"ptp{pj}"], [f"xnT{t}_{g}"])
        for t in range(8):
            for c in range(32):
                P.op("pe", lambda e, c=c, t=t: e.matmul(pab, lhsT=xnT[:, c, t * 128:(t + 1) * 128], rhs=wab[:, c, :],
                                                        start=(c == 0), stop=(c == 31)),
                     reads=[f"xnT{t}_{c // 4}", "wab"], writes=["pab"])
            evac(P, t, ab_tok[:, tb * 8 + t, :], pab, ["pab"], [f"ab{tb * 8 + t}"])
        for (c0, ncw) in col_tiles(prefix, tb):
            j = wcount % 2
            wcount += 1
            P.dma(wt[j][:, :, :ncw], win_d[:, c0:c0 + ncw].rearrange("(c p) n -> p c n", p=128),
                  writes=[f"wt{j}"], q="pool")
            for sub in range(0, ncw, 128):
                m = min(128, ncw - sub)
                k = ocount % 2
                ocount += 1
                for half in range(2):
                    for c in range(32):
                        P.op("pe", lambda e, c=c, j=j, k=k, half=half, sub=sub, m=m: e.matmul(
                            pp[k][:m, half * 512:(half + 1) * 512], lhsT=wt[j][:, c, sub:sub + m],
                            rhs=xnT[:, c, half * 512:(half + 1) * 512], start=(c == 0), stop=(c == 31)),
                            reads=[f"wt{j}"] + [f"xnT{t}_{c // 4}" for t in range(half * 4, half * 4 + 4)],
                            writes=[f"pp{k}_{half}"])
                    evac(P, half, og[k][:m, half * 512:(half + 1) * 512], pp[k][:m, half * 512:(half + 1) * 512],
                         [f"pp{k}_{half}"], [f"og{k}_{half}"])
                P.dma(PT_d[c0 + sub:c0 + sub + m, tb * 1024:(tb + 1) * 1024], og[k][:m, :],
                      reads=[f"og{k}_0", f"og{k}_1"])
    st.close()


def stage_gdn(nc, P, cst, PT_d, ab_tok, convw_d, alog_d, dtb_d, normw_d, gmk_d, OG_d, groups=range(8), dbg=None):
    st = Stage(nc, P)
    identf, ident, onesf, ones = cst["identf"], cst["ident"], cst["onesf"], cst["ones"]
    NT = 32
    sc = {}
    for nm in ("g", "gc", "ngc", "beta", "nbeta", "be1", "e1", "e2", "dch"):
        sc[nm] = st.sb("sc_" + nm, [128, NT * 16], F32)
    dtb = st.sb("dtb", [128, 16], F32)
    nA = st.sb("nA", [128, 16], F32)
    normw = st.sb("normw", [128, 1], F32)
    cw = st.sb("cw", [128, 48, 4], F32)
    zeros = st.sb("zeros", [128, 128], F32)
    Tri = st.sb("Tri", [128, 128], F32)
    TriN = st.sb("TriN", [128, 128], F32)
    maskU = st.sb("maskU", [128, 128], F32)
    maskL = st.sb("maskL", [128, 128], F32)
    gmk = st.sb("gmk", [128, 8, 128], BF16)
    P.dma(gmk, gmk_d, writes=["gmk"])
    xr = st.sb("xr", [128, TL], F32)
    yy = st.sb("yy", [128, TL], F32)
    sq = st.sb("sq", [128, TL], BF16)
    rt = [st.sb(f"rt{i}", [128, 512], F32) for i in range(2)]
    qkv = [[st.sb(f"qkv{hs}_{w}", [128, TL], BF16) for w in range(3)] for hs in range(2)]
    zs = [st.sb(f"zs{hs}", [128, 2048], F32) for hs in range(2)]
    OGb = [st.sb(f"OGb{hs}", [128, 2048], BF16) for hs in range(2)]
    pbig = st.ps("pbig", [128, 512], F32)
    pbig2 = st.ps("pbig2", [128, 512], F32)

    P.dma(dtb, dtb_d.partition_broadcast(128), writes=["dtb"])
    P.dma(nA, alog_d.partition_broadcast(128), writes=["nA"])
    P.dma(normw, normw_d.rearrange("(c o) -> c o", o=1), writes=["normw"])
    P.dma(cw, convw_d, writes=["cw"])
    P.op("pool", lambda e: e.memset(zeros, 0.0), writes=["zeros"])
    P.op("pool", lambda e: e.affine_select(out=Tri, in_=onesf, pattern=[[1, 128]], compare_op=ALU.is_ge, fill=0.0,
                                           base=0, channel_multiplier=-1), reads=["onesf"], writes=["Tri"])
    P.op("dve", lambda e: e.tensor_scalar(out=TriN, in0=Tri, scalar1=-1.0, scalar2=None, op0=ALU.mult),
         reads=["Tri"], writes=["TriN"])
    P.op("pool", lambda e: e.affine_select(out=maskU, in_=zeros, pattern=[[1, 128]], compare_op=ALU.is_ge, fill=NEG,
                                           base=0, channel_multiplier=-1), reads=["zeros"], writes=["maskU"])
    P.op("pool", lambda e: e.affine_select(out=maskL, in_=zeros, pattern=[[-1, 128]], compare_op=ALU.is_gt, fill=NEG,
                                           base=0, channel_multiplier=1), reads=["zeros"], writes=["maskL"])
    if dbg == 1:
        st.close()
        return
    g3 = sc["g"].rearrange("p (t h) -> p t h", h=16)
    b3 = sc["beta"].rearrange("p (t h) -> p t h", h=16)
    abk = [f"ab{t}" for t in range(NT)]
    P.op("act", lambda e: e.activation(out=nA, in_=nA, func=AF.Exp), reads=["nA"], writes=["nA"])
    P.op("dve", lambda e: e.tensor_scalar(out=nA, in0=nA, scalar1=-1.0, scalar2=None, op0=ALU.mult), reads=["nA"], writes=["nA"])
    P.op("dve", lambda e: e.tensor_tensor(out=g3, in0=ab_tok[:, :, 0:16], in1=dtb.unsqueeze(1).to_broadcast([128, NT, 16]),
                                          op=ALU.add), reads=abk + ["dtb"], writes=["g"])
    P.op("act", lambda e: e.activation(out=sc["g"], in_=sc["g"], func=AF.Exp), reads=["g"], writes=["g"])
    P.op("act", lambda e: e.activation(out=sc["g"], in_=sc["g"], func=AF.Ln, bias=1.0), reads=["g"], writes=["g"])
    P.op("dve", lambda e: e.tensor_tensor(out=g3, in0=g3, in1=nA.unsqueeze(1).to_broadcast([128, NT, 16]), op=ALU.mult),
         reads=["g", "nA"], writes=["g"])
    P.op("act", lambda e: e.activation(out=b3, in_=ab_tok[:, :, 16:32], func=AF.Sigmoid), reads=abk, writes=["beta"])
    if dbg == 2:
        st.close()
        return
    P.op("pe", lambda e: e.matmul(pbig, lhsT=Tri, rhs=sc["g"], start=True, stop=True), reads=["Tri", "g"], writes=["pbig"])
    P.op("pe", lambda e: e.matmul(pbig2, lhsT=onesf, rhs=sc["g"], start=True, stop=True), reads=["onesf", "g"], writes=["pbig2"])
    P.op("act", lambda e: e.copy(out=sc["gc"], in_=pbig), reads=["pbig"], writes=["gc"])
    P.op("act", lambda e: e.activation(out=sc["e1"], in_=pbig, func=AF.Exp), reads=["pbig"], writes=["e1"])
    if dbg == 3:
        st.close()
        return
    P.op("dve", lambda e: e.tensor_scalar(out=sc["ngc"], in0=sc["gc"], scalar1=-1.0, scalar2=None, op0=ALU.mult), reads=["gc"], writes=["ngc"])
    P.op("act", lambda e: e.copy(out=sc["e2"], in_=pbig2), reads=["pbig2"], writes=["e2"])
    P.op("dve", lambda e: e.tensor_tensor(out=sc["e2"], in0=sc["e2"], in1=sc["gc"], op=ALU.subtract), reads=["e2", "gc"], writes=["e2"])
    if dbg == 5:
        st.close()
        return
    P.op("act", lambda e: e.activation(out=sc["e2"], in_=sc["e2"], func=AF.Exp), reads=["e2"], writes=["e2"])
    if dbg == 6:
        st.close()
        return
    P.op("act", lambda e: e.activation(out=sc["dch"], in_=pbig2, func=AF.Exp), reads=["pbig2"], writes=["dch"])
    P.op("dve", lambda e: e.tensor_tensor(out=sc["be1"], in0=sc["beta"], in1=sc["e1"], op=ALU.mult), reads=["beta", "e1"], writes=["be1"])
    P.op("dve", lambda e: e.tensor_scalar(out=sc["nbeta"], in0=sc["beta"], scalar1=-1.0, scalar2=None, op0=ALU.mult),
         reads=["beta"], writes=["nbeta"])
    SCK = ["g", "gc", "ngc", "beta", "nbeta", "be1", "e1", "e2", "dch"]
    if dbg == 4:
        st.close()
        return

    W = []
    for hs in range(2):
        w = {}
        if hs == 0:
            w["pA"] = pbig[:, 0:128]
            w["pB"] = pbig2[:, 0:128]
        else:
            w["pA"] = st.ps(f"pA{hs}", [128, 512], F32)[:, 0:128]
            w["pB"] = st.ps(f"pB{hs}", [128, 512], F32)[:, 0:128]
        w["pC"] = st.ps(f"pC{hs}", [128, 512], F32)[:, 0:256]
        w["pT"] = st.ps(f"pT{hs}", [128, 8, 128], BF16)[:, 0:2, :]
        w["Gb"] = st.sb(f"Gb{hs}", [128, 128], F32)
        w["E"] = st.sb(f"E{hs}", [128, 128], F32)
        w["ET"] = st.sb(f"ET{hs}", [128, 128], F32)
        w["X"] = [st.sb(f"X{hs}_{i}", [128, 128], BF16) for i in range(2)]
        w["XT"] = [st.sb(f"XT{hs}_{i}", [128, 128], BF16) for i in range(2)]
        w["Yb"] = st.sb(f"Yb{hs}", [128, 256], BF16)
        w["Pm"] = st.sb(f"Pm{hs}", [128, 128], BF16)
        w["NkTs"] = [st.sb(f"NkT{hs}_{l}", [128, 128], BF16) for l in range(6)]
        for pb in range(2):
            w[f"Y{pb}"] = st.sb(f"Y{hs}_{pb}", [128, 256], F32)
            w[f"attnT{pb}"] = st.sb(f"attnT{hs}_{pb}", [128, 128], BF16)
            w[f"kout{pb}"] = st.sb(f"kout{hs}_{pb}", [128, 128], BF16)
            w[f"wT{pb}"] = st.sb(f"wT{hs}_{pb}", [128, 128], BF16)
        w["S"] = st.sb(f"S{hs}", [128, 128], F32)
        w["Sb"] = st.sb(f"Sb{hs}", [128, 128], BF16)
        w["vnew"] = st.sb(f"vnew{hs}", [128, 128], BF16)
        w["oS"] = st.sb(f"oS{hs}", [128, 128], F32)
        w["o"] = st.sb(f"o{hs}", [128, 128], F32)
        w["on"] = st.sb(f"on{hs}", [128, 128], BF16)
        w["junk"] = st.sb(f"gjunk{hs}", [128, 128], BF16)
        w["ss"] = st.sb(f"gss{hs}", [128, 1], F32)
        W.append(w)

    def K(hs, nm):
        if hs == 0 and nm == "pA":
            return "pbig"
        if hs == 0 and nm == "pB":
            return "pbig2"
        return f"{nm}@{hs}"

    def preprocess(grp):
        for hs in range(2):
            h = grp * 2 + hs
            for wi, coff in enumerate((0, 2048, 4096)):
                ct = (coff + h * 128) // 128
                row0 = C_QKV + coff + h * 128
                dst = qkv[hs][wi]
                lo = OWN0 - 3 if wi == 0 else 0
                P.dma(xr[:, lo:], PT_d[row0:row0 + 128, lo:TL], writes=["xr"])
                eng = "dve"
                P.op(eng, lambda e, ct=ct, lo=lo: e.tensor_scalar(out=yy[:, lo:], in0=xr[:, lo:], scalar1=cw[:, ct, 3:4], scalar2=None, op0=ALU.mult),
                     reads=["xr", "cw"], writes=["yy"])
                for sh in (1, 2, 3):
                    P.op(eng, lambda e, ct=ct, sh=sh, lo=lo: e.scalar_tensor_tensor(out=yy[:, lo + sh:], in0=xr[:, lo:TL - sh],
                                                                                 scalar=cw[:, ct, 3 - sh:4 - sh], in1=yy[:, lo + sh:],
                                                                                 op0=ALU.mult, op1=ALU.add),
                         reads=["xr", "cw", "yy"], writes=["yy"])
                if wi == 2:
                    P.op("act", lambda e, dst=dst: e.activation(out=dst, in_=yy, func=AF.Silu), reads=["yy"], writes=[K(hs, f"qkv{wi}")])
                    continue
                c0 = OWN0 if wi == 0 else 0
                P.op("act", lambda e, c0=c0: e.activation(out=yy[:, c0:], in_=yy[:, c0:], func=AF.Silu), reads=["yy"], writes=["yy"])
                P.op("pool", lambda e, c0=c0: e.tensor_tensor(out=sq[:, c0:], in0=yy[:, c0:], in1=yy[:, c0:], op=ALU.mult), reads=["yy"], writes=["sq"])
                scale = float(128 ** -0.5) if wi == 0 else 1.0
                for ch in range(c0 // 512, 8):
                    cs = slice(ch * 512, (ch + 1) * 512)
                    r = rt[ch % 2]
                    P.op("pe", lambda e, cs=cs: e.matmul(pbig, lhsT=ones, rhs=sq[:, cs], start=True, stop=True),
                         reads=["ones", "sq"], writes=["pbig"])
                    P.op("act", lambda e, r=r: e.activation(out=r, in_=pbig, func=AF.Ln, bias=float(EPS)), reads=["pbig"], writes=[f"rt{ch % 2}"])
                    P.op("act", lambda e, r=r: e.activation(out=r, in_=r, func=AF.Exp, scale=-0.5), reads=[f"rt{ch % 2}"], writes=[f"rt{ch % 2}"])
                    P.op("dve", lambda e, r=r, cs=cs, dst=dst, scale=scale: e.scalar_tensor_tensor(
                        out=dst[:, cs], in0=yy[:, cs], scalar=scale, in1=r, op0=ALU.mult, op1=ALU.mult),
                        reads=["yy", f"rt{ch % 2}"], writes=[K(hs, f"qkv{wi}")])
            P.dma(zs[hs], PT_d[C_Z + h * 128:C_Z + (h + 1) * 128, OWN0:TL], writes=[K(hs, "zs")])
            P.op("act", lambda e, hs=hs: e.activation(out=zs[hs], in_=zs[hs], func=AF.Silu), reads=[K(hs, "zs")], writes=[K(hs, "zs")])
            P.op("pool", lambda e, hs=hs: e.memset(W[hs]["S"], 0.0), writes=[K(hs, "S")])
            P.op("pool", lambda e, hs=hs: e.memset(W[hs]["Sb"], 0.0), writes=[K(hs, "Sb")])

    def pre(grp, hs, n, Q):
        h = grp * 2 + hs
        w = W[hs]
        pb = n % 2
        col = n * 16 + h
        cs = slice(n * 128, (n + 1) * 128)
        qT, kT, vT = (qkv[hs][i][:, cs] for i in range(3))
        kq, kk, kv = (K(hs, f"qkv{i}") for i in range(3))
        sv = lambda nm: sc[nm][:, col:col + 1]
        k_ = lambda nm: K(hs, nm)
        Q.op("dve", lambda e: e.tensor_scalar(out=w["Gb"], in0=onesf, scalar1=sv("g"), scalar2=None, op0=ALU.mult),
             reads=["onesf", "g"], writes=[k_("Gb")])
        if n >= 16:
            Q.op("pe", lambda e: e.matmul(w["pA"], lhsT=w["Gb"], rhs=Tri, start=True, stop=False), reads=[k_("Gb"), "Tri"], writes=[k_("pA")])
            Q.op("pe", lambda e: e.matmul(w["pA"], lhsT=identf, rhs=maskU, start=False, stop=True), reads=["identf", "maskU"], writes=[k_("pA")])
        Q.op("pe", lambda e: e.matmul(w["pB"], lhsT=w["Gb"], rhs=TriN, start=True, stop=False), reads=[k_("Gb"), "TriN"], writes=[k_("pB")])
        Q.op("pe", lambda e: e.matmul(w["pB"], lhsT=identf, rhs=maskL, start=False, stop=True), reads=["identf", "maskL"], writes=[k_("pB")])
        if n >= 16:
            Q.op("act", lambda e: e.activation(out=w["ET"], in_=w["pA"], func=AF.Exp, bias=sv("ngc")), reads=[k_("pA"), "ngc"], writes=[k_("ET")])
        Q.op("act", lambda e: e.activation(out=w["E"], in_=w["pB"], func=AF.Exp, bias=sv("gc")), reads=[k_("pB"), "gc"], writes=[k_("E")])
        Q.op("pe", lambda e: e.matmul(w["pA"], lhsT=kT, rhs=kT, start=True, stop=True), reads=[kk], writes=[k_("pA")])
        Q.op("dve", lambda e: e.scalar_tensor_tensor(out=w["X"][0], in0=w["pA"], scalar=sv("nbeta"), in1=w["E"], op0=ALU.mult, op1=ALU.mult),
             reads=[k_("pA"), "nbeta", k_("E")], writes=[k_("X0")])
        if n >= 16:
            Q.op("pe", lambda e: e.matmul(w["pB"], lhsT=kT, rhs=qT, start=True, stop=True), reads=[kk, kq], writes=[k_("pB")])
            Q.op("dve", lambda e: e.tensor_tensor(out=w[f"attnT{pb}"], in0=w["pB"], in1=w["ET"], op=ALU.mult),
                 reads=[k_("pB"), k_("ET")], writes=[k_(f"attnT{pb}")])
        Q.op("pe", lambda e: e.transpose(out=w["pT"][:, 0, :], in_=vT, identity=ident), reads=[kv, "ident"], writes=[k_("pT")])
        Q.op("pe", lambda e: e.transpose(out=w["pT"][:, 1, :], in_=kT, identity=ident), reads=[kk, "ident"], writes=[k_("pT")])
        Y = w[f"Y{pb}"]
        Q.op("act", lambda e: e.activation(out=Y[:, 0:128], in_=w["pT"][:, 0, :], func=AF.Copy, scale=sv("beta")),
             reads=[k_("pT"), "beta"], writes=[k_(f"Y{pb}")])
        Q.op("act", lambda e: e.activation(out=Y[:, 128:256], in_=w["pT"][:, 1, :], func=AF.Copy, scale=sv("be1")),
             reads=[k_("pT"), "be1"], writes=[k_(f"Y{pb}")])
        Q.op("act", lambda e: e.activation(out=w[f"kout{pb}"], in_=w["pT"][:, 1, :], func=AF.Copy, scale=sv("e2")),
             reads=[k_("pT"), "e2"], writes=[k_(f"kout{pb}")])
        Q.op("act", lambda e: e.copy(out=w["Yb"], in_=Y), reads=[k_(f"Y{pb}")], writes=[k_("Yb")])
        Q.op("pe", lambda e: e.transpose(out=w["pT"][:, 0, :], in_=w["X"][0], identity=ident), reads=[k_("X0"), "ident"], writes=[k_("pT")])
        Q.op("act", lambda e: e.copy(out=w["XT"][0], in_=w["pT"][:, 0, :]), reads=[k_("pT")], writes=[k_("XT0")])
        Am, Bm, Pm = w["X"][1], w["XT"][1], w["Pm"]
        Q.op("pool", lambda e: e.tensor_tensor(out=Am, in0=w["X"][0], in1=gmk[:, 7, :], op=ALU.mult), reads=[k_("X0"), "gmk"], writes=[k_("Am")])
        Q.op("pool", lambda e: e.tensor_tensor(out=Am, in0=Am, in1=ident, op=ALU.add), reads=[k_("Am"), "ident"], writes=[k_("Am")])
        Q.op("pool", lambda e: e.tensor_tensor(out=Bm, in0=w["XT"][0], in1=gmk[:, 0, :], op=ALU.mult), reads=[k_("XT0"), "gmk"], writes=[k_("Bm")])
        Q.op("pool", lambda e: e.tensor_tensor(out=Bm, in0=Bm, in1=ident, op=ALU.add), reads=[k_("Bm"), "ident"], writes=[k_("Bm")])
        for lvl in range(1, 7):
            Q.op("pool", lambda e, lvl=lvl: e.tensor_tensor(out=w["NkTs"][lvl - 1], in0=w["XT"][0], in1=gmk[:, lvl, :], op=ALU.mult),
                 reads=[k_("XT0"), "gmk"], writes=[k_(f"NkT{lvl}")])
        for lvl in range(1, 7):
            NkT_l = w["NkTs"][lvl - 1]
            Q.op("pe", lambda e, NkT_l=NkT_l: e.matmul(w["pA"], lhsT=NkT_l, rhs=Am, start=True, stop=True), reads=[k_(f"NkT{lvl}"), k_("Am")], writes=[k_("pA")])
            if lvl % 2 == 0:
                Q.op("act", lambda e: e.copy(out=Pm, in_=w["pA"]), reads=[k_("pA")], writes=[k_("Pm")])
            else:
                Q.op("dve", lambda e: e.tensor_copy(out=Pm, in_=w["pA"]), reads=[k_("pA")], writes=[k_("Pm")])
            Q.op("pe", lambda e: e.matmul(w["pB"], lhsT=Bm, rhs=Pm, start=True, stop=False), reads=[k_("Bm"), k_("Pm")], writes=[k_("pB")])
            Q.op("pe", lambda e: e.matmul(w["pB"], lhsT=ident, rhs=Am, start=False, stop=True), reads=["ident", k_("Am")], writes=[k_("pB")])
            Q.op("pe", lambda e: e.matmul(w["pC"][:, 0:128], lhsT=Pm, rhs=Bm, start=True, stop=False), reads=[k_("Bm"), k_("Pm")], writes=[k_("pC")])
            Q.op("pe", lambda e: e.matmul(w["pC"][:, 0:128], lhsT=ident, rhs=Bm, start=False, stop=True), reads=["ident", k_("Bm")], writes=[k_("pC")])
            Q.op("act", lambda e: e.copy(out=Am, in_=w["pB"]), reads=[k_("pB")], writes=[k_("Am")])
            Q.op("dve", lambda e: e.tensor_copy(out=Bm, in_=w["pC"][:, 0:128]), reads=[k_("pC")], writes=[k_("Bm")])
        Q.op("pe", lambda e: e.matmul(w["pC"], lhsT=Bm, rhs=w["Yb"], start=True, stop=True), reads=[k_("Bm"), k_("Yb")], writes=[k_("pC")])
        Q.op("dve", lambda e: e.tensor_copy(out=Y, in_=w["pC"]), reads=[k_("pC")], writes=[k_(f"Y{pb}")])
        Q.op("act", lambda e: e.copy(out=w["Yb"], in_=Y), reads=[k_(f"Y{pb}")], writes=[k_("Yb")])
        Q.op("pe", lambda e: e.transpose(out=w["pT"][:, 1, :], in_=w["Yb"][:, 128:256], identity=ident), reads=[k_("Yb"), "ident"], writes=[k_("pT")])
        Q.op("act", lambda e: e.copy(out=w[f"wT{pb}"], in_=w["pT"][:, 1, :]), reads=[k_("pT")], writes=[k_(f"wT{pb}")])

    def seq(grp, hs, n, Q):
        h = grp * 2 + hs
        w = W[hs]
        pb = n % 2
        col = n * 16 + h
        cs = slice(n * 128, (n + 1) * 128)
        qT = qkv[hs][0][:, cs]
        kq = K(hs, "qkv0")
        sv = lambda nm: sc[nm][:, col:col + 1]
        k_ = lambda nm: K(hs, nm)
        Y = w[f"Y{pb}"]
        Q.op("pe", lambda e: e.matmul(w["pA"], lhsT=w[f"wT{pb}"], rhs=w["Sb"], start=True, stop=True),
             reads=[k_(f"wT{pb}"), k_("Sb")], writes=[k_("pA")])
        Q.op("dve", lambda e: e.tensor_tensor(out=w["vnew"], in0=Y[:, 0:128], in1=w["pA"], op=ALU.subtract),
             reads=[k_("pA"), k_(f"Y{pb}")], writes=[k_("vnew")])
        if n >= 16:
            Q.op("pe", lambda e: e.matmul(w["pB"], lhsT=qT, rhs=w["Sb"], start=True, stop=True), reads=[kq, k_("Sb")], writes=[k_("pB")])
            Q.op("act", lambda e: e.activation(out=w["oS"], in_=w["pB"], func=AF.Copy, scale=sv("e1")), reads=[k_("pB"), "e1"], writes=[k_("oS")])
            Q.op("pe", lambda e: e.matmul(w["pC"][:, 0:128], lhsT=w[f"attnT{pb}"], rhs=w["vnew"], start=True, stop=True),
                 reads=[k_(f"attnT{pb}"), k_("vnew")], writes=[k_("pC")])
            Q.op("dve", lambda e: e.tensor_tensor(out=w["o"], in0=w["pC"][:, 0:128], in1=w["oS"], op=ALU.add),
                 reads=[k_("pC"), k_("oS")], writes=[k_("o")])
        Q.op("pe", lambda e: e.matmul(w["pC"][:, 128:256], lhsT=w[f"kout{pb}"], rhs=w["vnew"], start=True, stop=True),
             reads=[k_(f"kout{pb}"), k_("vnew")], writes=[k_("pC")])
        Q.op("dve", lambda e: e.scalar_tensor_tensor(out=w["S"], in0=w["S"], scalar=sv("dch"), in1=w["pC"][:, 128:256], op0=ALU.mult, op1=ALU.add),
             reads=[k_("pC"), "dch", k_("S")], writes=[k_("S")])
        Q.op("act", lambda e: e.copy(out=w["Sb"], in_=w["S"]), reads=[k_("S")], writes=[k_("Sb")])
        if n >= 16:
            Q.op("act", lambda e: e.activation(out=w["junk"], in_=w["o"], func=AF.Square, accum_out=w["ss"]), reads=[k_("o")], writes=[k_("ss"), k_("junk")])
            rsqrt_col(Q, w["ss"], w["ss"], EPS, k_("ss"), k_("ss"), mul=1.0 / 128)
            Q.op("dve", lambda e: e.tensor_scalar(out=w["on"], in0=w["o"], scalar1=w["ss"], scalar2=None, op0=ALU.mult),
                 reads=[k_("o"), k_("ss")], writes=[k_("on")])
            Q.op("pe", lambda e: e.transpose(out=w["pT"][:, 0, :], in_=w["on"], identity=ident), reads=[k_("on"), "ident"], writes=[k_("pT")])
            oc = slice((n - 16) * 128, (n - 15) * 128)
            Q.op("act", lambda e: e.copy(out=w["junk"], in_=w["pT"][:, 0, :]), reads=[k_("pT")], writes=[k_("junk")])
            Q.op("dve", lambda e: e.scalar_tensor_tensor(out=OGb[hs][:, oc], in0=w["junk"], scalar=normw, in1=zs[hs][:, oc],
                                                         op0=ALU.mult, op1=ALU.mult),
                 reads=[k_("junk"), "normw", k_("zs")], writes=[k_("OGb")])

    class Rec:
        def __init__(self):
            self.items = []

        def op(self, *a, **kw):
            self.items.append((a, kw))

    def interleave(fn, grp, n):
        recs = []
        for hs in range(2):
            r = Rec()
            fn(grp, hs, n, r)
            recs.append(r.items)
        for k in range(max(len(r) for r in recs)):
            for r in recs:
                if k < len(r):
                    a, kw = r[k]
                    P.op(*a, **kw)

    for grp in groups:
        preprocess(grp)
        interleave(pre, grp, 0)
        for n in range(NT):
            if n + 1 < NT:
                interleave(pre, grp, n + 1)
            interleave(seq, grp, n)
        for hs in range(2):
            h = grp * 2 + hs
            P.dma(OG_d[h * 128:(h + 1) * 128, :], OGb[hs], reads=[K(hs, "OGb")])
    st.close()


def nsa_host_consts(half):
    import ml_dtypes
    bf = ml_dtypes.bfloat16
    c = {}
    n = np.arange(256)
    q = np.arange(2048)
    cm = np.where((16 * n[:, None] + 31) <= (OWN0 + q[None, :]), 0.0, NEG).astype(np.float32)
    c["cmask"] = np.ascontiguousarray(cm.reshape(2, 128, 2048).transpose(1, 0, 2)).astype(bf)
    nvalid = (n <= 254) & ((16 * n >= OWN0) if half == 0 else True)
    c["cbias"] = np.ascontiguousarray(np.where(nvalid, 0.0, NEG).astype(np.float32).reshape(2, 128).T)
    pos = np.arange(TL)
    kvalid = (pos >= OWN0) if half == 0 else np.ones(TL, bool)
    c["kbias"] = np.ascontiguousarray(np.where(kvalid, 0.0, NEG).astype(np.float32).reshape(32, 128).T)
    key = np.arange(128)
    dk = np.arange(4)
    qq = np.arange(512)
    rel = dk[None, :, None] * 128 + key[:, None, None]
    c["causm"] = np.where(rel <= qq[None, None, :], 0.0, NEG).astype(bf)
    c["winm"] = np.where(rel > qq[None, None, :], 0.0, NEG).astype(bf)
    j = np.arange(64)
    kt = np.arange(32)
    c["esel"] = (j[:, None, None] == (2 * kt[None, :, None] + key[None, None, :] // 64)).astype(bf)
    cmp_start = np.arange(255) * 16
    sel_start = np.arange(64) * 64
    ov = np.minimum(cmp_start[:, None] + 32, sel_start[None, :] + 64) - np.maximum(cmp_start[:, None], sel_start[None, :])
    sm = np.zeros((256, 65), np.float32)
    sm[:255, :64] = np.clip(ov, 0, None) / 16.0
    sm[:, 64] = 1.0
    c["selmap"] = np.ascontiguousarray(sm.reshape(2, 128, 65).transpose(1, 0, 2)).astype(bf)
    t = OWN0 + q
    cur = t // 64
    blk0 = 32 if half == 0 else 0
    forced = (j[None, :] == blk0) | (j[None, :] == cur[:, None]) | (j[None, :] == cur[:, None] - 1)
    causal = (j[None, :] * 64 <= t[:, None]) & (j[None, :] >= blk0)
    use_imp = causal & ~forced
    selm = use_imp.astype(np.float32)
    sela = np.where(forced, 1e6, np.where(causal, 0.0, -1e30)).astype(np.float32)
    c["selm"] = np.ascontiguousarray(selm.reshape(16, 128, 64).transpose(1, 0, 2))
    c["sela"] = np.ascontiguousarray(sela.reshape(16, 128, 64).transpose(1, 0, 2))
    r = np.arange(48)
    c["selrow"] = np.ascontiguousarray(np.broadcast_to((r[:, None, None] == r[None, :, None]), (48, 48, 128))).astype(bf)
    return c


NSA_CONST_SPECS = [("cmask", [128, 2, 2048], BF16), ("cbias", [128, 2], F32), ("kbias", [128, 32], F32),
                   ("causm", [128, 4, 512], BF16), ("winm", [128, 4, 512], BF16), ("esel", [64, 32, 128], BF16),
                   ("selmap", [128, 2, 65], BF16), ("selm", [128, 16, 64], F32), ("sela", [128, 16, 64], F32),
                   ("selrow", [48, 48, 128], BF16)]


def stage_nsa(nc, P, cst, PT_d, KD, w1k_d, w2k_d, pek_d, w1v_d, w2v_d, pev_d, ON_d, ngroups=4, nqc=4):
    st = Stage(nc, P)
    ident, ones = cst["ident"], cst["ones"]
    SCALE = float(128 ** -0.5)
    TINY = 1e-30
    C = {}
    for nm, shp, dt in NSA_CONST_SPECS:
        C[nm] = st.sb("c_" + nm, shp, dt)
        P.dma(C[nm], KD[nm], writes=["c_" + nm])
    gst = st.sb("gst", [48, 2048], F32)
    gsig = st.sb("gsig", [48, 2048], BF16)
    P.dma(gst, PT_d[C_NG:C_NG + 48, OWN0:TL], writes=["gst"])
    P.op("act", lambda e: e.activation(out=gsig, in_=gst, func=AF.Sigmoid), reads=["gst"], writes=["gsig"])
    pS = [st.ps(f"pS{i}", [128, 512], F32) for i in range(2)]
    pO = st.ps("pO", [128, 512], F32)
    pD = st.ps("pD", [128, 512], F32)
    pG = st.ps("pG", [128, 512], F32)
    pI = st.ps("pI", [128, 4, 128], F32)
    pTm = st.ps("pTm", [128, 4, 128], BF16)
    pO2 = st.ps("pO2", [128, 512], F32)
    pD2 = pI.rearrange("p a b -> p (a b)")
    ACC = [(pO, "pO", pD, "pD"), (pO2, "pO2", pD2, "pI")]
    cw_ = {}
    for tag, w1d, w2d, ped in (("k", w1k_d, w2k_d, pek_d), ("v", w1v_d, w2v_d, pev_d)):
        w1 = st.sb("w1" + tag, [128, 32, 128], BF16)
        w2 = st.sb("w2" + tag, [128, 128], BF16)
        peT = st.sb("peT" + tag, [128, 32], BF16)
        hb = st.sb("hb" + tag, [128, 1], F32)
        P.dma(w1, w1d.rearrange("l d e -> d l e"), writes=["w1" + tag], q="pool")
        P.dma(w2, w2d, writes=["w2" + tag], q="pool")
        P.dma(peT, ped, writes=["peT" + tag], q="pool")
        for l in range(32):
            P.op("pe", lambda e, l=l, w1=w1, peT=peT: e.matmul(pS[0][:, 0:1], lhsT=w1[:, l, :], rhs=peT[:, l:l + 1],
                                                              start=(l == 0), stop=(l == 31)),
                 reads=["w1" + tag, "peT" + tag], writes=["pS0"])
        P.op("act", lambda e, hb=hb: e.copy(out=hb, in_=pS[0][:, 0:1]), reads=["pS0"], writes=["hb" + tag])
        cw_[tag] = (w1, w2, hb)
    kcT = st.sb("kcT", [128, TL], BF16)
    vcT = st.sb("vcT", [128, TL], BF16)
    ksT = st.sb("ksT", [128, TL], BF16)
    kwT = st.sb("kwT", [128, TL], BF16)
    vtmp = st.sb("vtmp", [128, TL], BF16)
    vs_tok = st.sb("vs_tok", [128, 32, 128], BF16)
    vw_tok = st.sb("vw_tok", [128, 32, 128], BF16)
    qT = [st.sb(f"nq{i}", [128, 2048], BF16) for i in range(4)]
    hid = st.sb("hid", [128, 256], BF16)
    kcmpT = st.sb("kcmpT", [128, 256], BF16)
    vcmpT = st.sb("vcmpT", [128, 256], BF16)
    vcmp_tok = st.sb("vcmp_tok", [128, 2, 128], BF16)
    Pc = [[st.sb(f"Pc{h}_{j}", [128, 512], BF16) for j in range(2)] for h in range(4)]
    PTl = [st.sb(f"PTl{i}", [128, 512], BF16) for i in range(3)]
    accO = [st.sb(f"accO{h}", [128, 512], F32) for h in range(4)]
    accI = st.sb("accI", [128, 4, 64], F32)
    rr = st.sb("rr", [128, 512], F32)
    tmpo = st.sb("tmpo", [128, 512], F32)
    rc = st.sb("rc", [128, 1], F32)
    score = st.sb("score", [128, 64], F32)
    work = st.sb("work", [128, 64], F32)
    mx8 = st.sb("mx8", [128, 8], F32)
    madd = st.sb("madd", [128, 64], BF16)
    maddT = st.sb("maddT", [64, 512], BF16)
    osb = st.sb("osb", [128, 512], BF16)
    P.op("pool", lambda e: e.memset(hid, 0.0), writes=["hid"])

    def combine(h, first, aset=0):
        pO_, kO, pD_, kD = ACC[aset]
        P.op("dve", lambda e: e.tensor_scalar(out=rr, in0=pD_, scalar1=TINY, scalar2=None, op0=ALU.max), reads=[kD], writes=["rr"])
        P.op("dve", lambda e: e.reciprocal(out=rr, in_=rr), reads=["rr"], writes=["rr"])
        P.op("dve", lambda e: e.tensor_tensor(out=rr, in0=rr, in1=pG, op=ALU.mult), reads=["rr", "pG"], writes=["rr"])
        if first:
            P.op("dve", lambda e: e.tensor_tensor(out=accO[h], in0=pO_, in1=rr, op=ALU.mult), reads=[kO, "rr"], writes=[f"accO{h}"])
        else:
            P.op("dve", lambda e: e.tensor_tensor(out=tmpo, in0=pO_, in1=rr, op=ALU.mult), reads=[kO, "rr"], writes=["tmpo"])
            P.op("dve", lambda e: e.tensor_tensor(out=accO[h], in0=accO[h], in1=tmpo, op=ALU.add), reads=["tmpo", f"accO{h}"], writes=[f"accO{h}"])

    def compress(tag, srcT, srckey, dstT, dstkey):
        w1, w2, hb = cw_[tag]
        for l in range(32):
            v3 = srcT.rearrange("p (n s) -> p n s", s=16)
            rhs_ = v3[:, 0:255, l] if l < 16 else v3[:, 1:256, l - 16]
            P.op("pe", lambda e, l=l, rhs_=rhs_: e.matmul(pS[0][:, 0:255], lhsT=w1[:, l, :], rhs=rhs_,
                                               start=(l == 0), stop=(l == 31)),
                 reads=["w1" + tag, srckey], writes=["pS0"])
        P.op("act", lambda e: e.activation(out=hid[:, 0:255], in_=pS[0][:, 0:255], func=AF.Silu, bias=hb), reads=["pS0", "hb" + tag], writes=["hid"])
        P.op("pe", lambda e: e.matmul(pS[1][:, 0:256], lhsT=w2, rhs=hid, start=True, stop=True), reads=["w2" + tag, "hid"], writes=["pS1"])
        P.op("act", lambda e: e.copy(out=dstT, in_=pS[1][:, 0:256]), reads=["pS1"], writes=[dstkey])

    def to_tok(dst, dstkey, ntile, src, srckey):
        for g4 in range(0, ntile, 4):
            nn = min(4, ntile - g4)
            for j in range(nn):
                P.op("pe", lambda e, j=j, g4=g4: e.transpose(out=pTm[:, j, :], in_=src[:, (g4 + j) * 128:(g4 + j + 1) * 128], identity=ident),
                     reads=[srckey, "ident"], writes=["pTm"])
            P.op("act", lambda e, g4=g4, nn=nn: e.copy(out=dst[:, g4:g4 + nn, :], in_=pTm[:, 0:nn, :]), reads=["pTm"], writes=[dstkey])

    pcount = [0]
    scount = [0]

    def attend(h, qs, kT, kTkey, vtok, vkey, kts, maskfn, bias_fn, aset=0):
        n = len(kts)
        prev = None
        pO_, kO, pD_, kD = ACC[aset]

        def pv(pt, pi, kt, i):
            P.op("pe", lambda e: e.matmul(pO_, lhsT=vtok[:, kt, :], rhs=pt, start=(i == 0), stop=(i == n - 1)),
                 reads=[vkey, f"PTl{pi}"], writes=[kO])
            P.op("pe", lambda e: e.matmul(pD_, lhsT=ones, rhs=pt, start=(i == 0), stop=(i == n - 1)),
                 reads=["ones", f"PTl{pi}"], writes=[kD])

        for i, kt in enumerate(kts):
            sb_ = scount[0] % 2
            scount[0] += 1
            ps = pS[sb_]
            extra = maskfn(kt)
            P.op("pe", lambda e, qs=qs, kt=kt, ps=ps, extra=extra: e.matmul(ps, lhsT=kT[:, kt * 128:(kt + 1) * 128], rhs=qT[h][:, qs],
                                                                    start=True, stop=(len(extra) == 0)),
                 reads=[kTkey, f"nq{h}"], writes=[f"pS{sb_}"])
            for mi, (l_, r_, keys_) in enumerate(extra):
                P.op("pe", lambda e, ps=ps, l_=l_, r_=r_, last=(mi == len(extra) - 1): e.matmul(ps, lhsT=l_, rhs=r_, start=False, stop=last),
                     reads=keys_, writes=[f"pS{sb_}"])
            pi = pcount[0] % 3
            pcount[0] += 1
            pt = PTl[pi]
            P.op("act", lambda e, ps=ps, pt=pt, kt=kt: e.activation(out=pt, in_=ps, func=AF.Exp, scale=SCALE, bias=bias_fn(kt)),
                 reads=[f"pS{sb_}", "c_kbias", "c_cbias"], writes=[f"PTl{pi}"])
            if prev is not None:
                pv(*prev)
            prev = (pt, pi, kt, i)
        pv(*prev)

    for g in range(ngroups):
        rows = lambda jj: slice(C_NKV + jj * 512 + g * 128, C_NKV + jj * 512 + (g + 1) * 128)
        P.dma(kcT, PT_d[rows(0), 0:TL], writes=["kcT"], q="pool")
        P.dma(vcT, PT_d[rows(1), 0:TL], writes=["vcT"], q="pool")
        P.dma(ksT, PT_d[rows(2), 0:TL], writes=["ksT"], q="pool")
        P.dma(kwT, PT_d[rows(4), 0:TL], writes=["kwT"], q="pool")
        P.dma(vtmp, PT_d[rows(3), 0:TL], writes=["vtmp"], q="pool")
        to_tok(vs_tok, "vs_tok", 32, vtmp, "vtmp")
        P.dma(vtmp, PT_d[rows(5), 0:TL], writes=["vtmp"], q="pool")
        to_tok(vw_tok, "vw_tok", 32, vtmp, "vtmp")
        for hg in range(4):
            hq = g * 4 + hg
            P.dma(qT[hg], PT_d[C_NQ + hq * 128:C_NQ + (hq + 1) * 128, OWN0:TL], writes=[f"nq{hg}"], q="pool")
        compress("k", kcT, "kcT", kcmpT, "kcmpT")
        compress("v", vcT, "vcT", vcmpT, "vcmpT")
        to_tok(vcmp_tok, "vcmp_tok", 2, vcmpT, "vcmpT")
        for qc in range(nqc):
            qs = slice(qc * 512, (qc + 1) * 512)
            for hg in range(4):
                hq = g * 4 + hg
                for j in range(2):
                    sb_ = scount[0] % 2
                    scount[0] += 1
                    ps = pS[sb_]
                    P.op("pe", lambda e, qs=qs, j=j, ps=ps, hg=hg: e.matmul(ps, lhsT=kcmpT[:, j * 128:(j + 1) * 128], rhs=qT[hg][:, qs], start=True, stop=False),
                         reads=["kcmpT", f"nq{hg}"], writes=[f"pS{sb_}"])
                    P.op("pe", lambda e, qs=qs, j=j, ps=ps: e.matmul(ps, lhsT=ident, rhs=C["cmask"][:, j, qs], start=False, stop=True),
                         reads=["ident", "c_cmask"], writes=[f"pS{sb_}"])
                    P.op("act", lambda e, j=j, ps=ps, hg=hg: e.activation(out=Pc[hg][j], in_=ps, func=AF.Exp, scale=SCALE, bias=C["cbias"][:, j:j + 1]),
                         reads=[f"pS{sb_}", "c_cbias"], writes=[f"Pc{hg}_{j}"])
                for j in range(2):
                    P.op("pe", lambda e, j=j, hg=hg: e.matmul(pO, lhsT=vcmp_tok[:, j, :], rhs=Pc[hg][j], start=(j == 0), stop=(j == 1)),
                         reads=["vcmp_tok", f"Pc{hg}_{j}"], writes=["pO"])
                for j in range(2):
                    P.op("pe", lambda e, j=j, hg=hg: e.matmul(pD, lhsT=ones, rhs=Pc[hg][j], start=(j == 0), stop=(j == 1)),
                         reads=["ones", f"Pc{hg}_{j}"], writes=["pD"])
                P.op("pe", lambda e, qs=qs, hq=hq: e.matmul(pG, lhsT=C["selrow"][:, hq * 3 + 0, :], rhs=gsig[:, qs], start=True, stop=True),
                     reads=["c_selrow", "gsig"], writes=["pG"])
                combine(hg, True)
                for sub in range(4):
                    for j in range(2):
                        P.op("pe", lambda e, j=j, hg=hg, sub=sub: e.matmul(pI[:, sub, 0:65], lhsT=Pc[hg][j][:, sub * 128:(sub + 1) * 128],
                                                                         rhs=C["selmap"][:, j, :], start=(j == 0), stop=(j == 1)),
                             reads=["c_selmap", f"Pc{hg}_{j}"], writes=["pI"])
                    P.op("dve", lambda e, sub=sub: e.tensor_scalar(out=rc, in0=pI[:, sub, 64:65], scalar1=TINY, scalar2=None, op0=ALU.max),
                         reads=["pI"], writes=["rc"])
                    P.op("dve", lambda e: e.reciprocal(out=rc, in_=rc), reads=["rc"], writes=["rc"])
                    if hg == 0:
                        P.op("dve", lambda e, sub=sub: e.tensor_scalar(out=accI[:, sub, :], in0=pI[:, sub, 0:64], scalar1=rc, scalar2=None, op0=ALU.mult),
                             reads=["pI", "rc"], writes=["accI"])
                    else:
                        P.op("dve", lambda e, sub=sub: e.scalar_tensor_tensor(out=accI[:, sub, :], in0=pI[:, sub, 0:64], scalar=rc, in1=accI[:, sub, :],
                                                                             op0=ALU.mult, op1=ALU.add),
                             reads=["pI", "rc", "accI"], writes=["accI"])
            for sub in range(4):
                ts_ = qc * 4 + sub
                P.op("dve", lambda e, sub=sub, ts_=ts_: e.tensor_tensor(out=score, in0=accI[:, sub, :], in1=C["selm"][:, ts_, :], op=ALU.mult),
                     reads=["accI", "c_selm"], writes=["score"])
                P.op("dve", lambda e, ts_=ts_: e.tensor_tensor(out=score, in0=score, in1=C["sela"][:, ts_, :], op=ALU.add),
                     reads=["score", "c_sela"], writes=["score"])
                P.op("dve", lambda e: e.max(out=mx8, in_=score), reads=["score"], writes=["mx8"])
                P.op("dve", lambda e: e.match_replace(out=work, in_to_replace=mx8, in_values=score, imm_value=-3.0e38),
                     reads=["score", "mx8"], writes=["work"])
                P.op("dve", lambda e: e.max(out=mx8, in_=work), reads=["work"], writes=["mx8"])
                P.op("dve", lambda e: e.tensor_scalar(out=work, in0=score, scalar1=mx8[:, 7:8], scalar2=None, op0=ALU.is_ge),
                     reads=["score", "mx8"], writes=["work"])
                P.op("dve", lambda e: e.tensor_scalar(out=madd, in0=work, scalar1=1.0, scalar2=-NEG, op0=ALU.subtract, op1=ALU.mult),
                     reads=["work"], writes=["madd"])
                P.op("pe", lambda e: e.transpose(out=pTm[0:64, 0, :], in_=madd, identity=ident), reads=["madd", "ident"], writes=["pTm"])
                P.op("act", lambda e, sub=sub: e.copy(out=maddT[:, sub * 128:(sub + 1) * 128], in_=pTm[0:64, 0, :]), reads=["pTm"], writes=["maddT"])
            kt_diag0 = 16 + 4 * qc
            for hg in range(4):
                hq = g * 4 + hg

                def sel_mask(kt):
                    ex = [(C["esel"][:, kt, :], maddT, ["c_esel", "maddT"])]
                    if kt >= kt_diag0:
                        ex.append((ident, C["causm"][:, kt - kt_diag0, :], ["ident", "c_causm"]))
                    return ex

                def win_mask(kt):
                    if kt >= kt_diag0:
                        return [(ident, C["causm"][:, kt - kt_diag0, :], ["ident", "c_causm"])]
                    return [(ident, C["winm"][:, kt - (kt_diag0 - 4), :], ["ident", "c_winm"])]

                kb = lambda kt: C["kbias"][:, kt:kt + 1]
                attend(hg, qs, ksT, "ksT", vs_tok, "vs_tok", list(range(0, kt_diag0 + 4)), sel_mask, kb, aset=1)
                P.op("pe", lambda e, qs=qs, hq=hq: e.matmul(pG, lhsT=C["selrow"][:, hq * 3 + 1, :], rhs=gsig[:, qs], start=True, stop=True),
                     reads=["c_selrow", "gsig"], writes=["pG"])
                combine(hg, False, aset=1)
                attend(hg, qs, kwT, "kwT", vw_tok, "vw_tok", list(range(kt_diag0 - 4, kt_diag0 + 4)), win_mask, kb, aset=0)
                P.op("pe", lambda e, qs=qs, hq=hq: e.matmul(pG, lhsT=C["selrow"][:, hq * 3 + 2, :], rhs=gsig[:, qs], start=True, stop=True),
                     reads=["c_selrow", "gsig"], writes=["pG"])
                combine(hg, False)
                P.op("act", lambda e, hg=hg: e.copy(out=osb, in_=accO[hg]), reads=[f"accO{hg}"], writes=["osb"])
                P.dma(ON_d[hq * 128:(hq + 1) * 128, qs], osb, reads=["osb"])
    st.close()


def stage_merge(nc, P, cst, PT_d, OG_d, ON_d, wa_d, wb_d, wo_d, Hd, nblk=4):
    st = Stage(nc, P)
    ogT = st.sb("ogT", [128, 16, 512], BF16)
    onT = st.sb("onT", [128, 16, 512], BF16)
    mT = st.sb("mT", [128, 32, 512], BF16)
    wa = [st.sb(f"wa{i}", [128, 16, 256], BF16) for i in range(2)]
    wb = [st.sb(f"wb{i}", [128, 16, 256], BF16) for i in range(2)]
    ga = [st.sb(f"ga{i}", [128, 512], F32) for i in range(2)]
    gb = [st.sb(f"gb{i}", [128, 512], F32) for i in range(2)]
    m1 = st.sb("m1", [128, 512], F32)
    m2 = st.sb("m2", [128, 512], F32)
    wo = [st.sb(f"wo{i}", [128, 32, 256], BF16) for i in range(2)]
    hs = [st.sb(f"hs{i}", [128, 256], F32) for i in range(2)]
    pA = [st.ps(f"mpA{i}", [128, 512], F32) for i in range(2)]
    pB = [st.ps(f"mpB{i}", [128, 512], F32) for i in range(2)]
    pH = [st.ps(f"mpH{i}", [128, 512], F32) for i in range(2)]
    wc = 0
    gc_ = 0
    woc = 0
    hc = 0
    for tb in range(nblk):
        ts_ = slice(tb * 512, (tb + 1) * 512)
        P.dma(ogT, OG_d[:, ts_].rearrange("(c p) t -> p c t", p=128), writes=["ogT"])
        P.dma(onT, ON_d[:, ts_].rearrange("(c p) t -> p c t", p=128), writes=["onT"])
        for c2 in range(16):
            j = wc % 2
            wc += 1
            P.dma(wa[j], wa_d[:, c2 * 256:(c2 + 1) * 256].rearrange("(k p) n -> p k n", p=128), writes=[f"wa{j}"], q="pool")
            P.dma(wb[j], wb_d[:, c2 * 256:(c2 + 1) * 256].rearrange("(k p) n -> p k n", p=128), writes=[f"wb{j}"], q="pool")
            for sub in range(2):
                cc = c2 * 2 + sub
                i = gc_ % 2
                gc_ += 1
                P.dma(ga[i], PT_d[C_MG + cc * 128:C_MG + (cc + 1) * 128, OWN0 + tb * 512:OWN0 + (tb + 1) * 512], writes=[f"ga{i}"])
                P.dma(gb[i], PT_d[C_MG + 4096 + cc * 128:C_MG + 4096 + (cc + 1) * 128, OWN0 + tb * 512:OWN0 + (tb + 1) * 512], writes=[f"gb{i}"])
                P.op("act", lambda e, i=i: e.activation(out=ga[i], in_=ga[i], func=AF.Sigmoid), reads=[f"ga{i}"], writes=[f"ga{i}"])
                P.op("act", lambda e, i=i: e.activation(out=gb[i], in_=gb[i], func=AF.Sigmoid), reads=[f"gb{i}"], writes=[f"gb{i}"])
                for k in range(16):
                    P.op("pe", lambda e, k=k, i=i, j=j, sub=sub: e.matmul(pA[i], lhsT=wa[j][:, k, sub * 128:(sub + 1) * 128], rhs=ogT[:, k, :],
                                                                        start=(k == 0), stop=(k == 15)),
                         reads=[f"wa{j}", "ogT"], writes=[f"mpA{i}"])
                for k in range(16):
                    P.op("pe", lambda e, k=k, i=i, j=j, sub=sub: e.matmul(pB[i], lhsT=wb[j][:, k, sub * 128:(sub + 1) * 128], rhs=onT[:, k, :],
                                                                        start=(k == 0), stop=(k == 15)),
                         reads=[f"wb{j}", "onT"], writes=[f"mpB{i}"])
                P.op("dve", lambda e, i=i: e.tensor_tensor(out=m1, in0=pA[i], in1=ga[i], op=ALU.mult), reads=[f"mpA{i}", f"ga{i}"], writes=["m1"])
                P.op("dve", lambda e, i=i: e.tensor_tensor(out=m2, in0=pB[i], in1=gb[i], op=ALU.mult), reads=[f"mpB{i}", f"gb{i}"], writes=["m2"])
                P.op("dve", lambda e, cc=cc: e.tensor_tensor(out=mT[:, cc, :], in0=m1, in1=m2, op=ALU.add), reads=["m1", "m2"], writes=[f"mT{cc}"])
        for ct in range(16):
            j = woc % 2
            woc += 1
            P.dma(wo[j], wo_d[:, ct * 256:(ct + 1) * 256].rearrange("(k p) n -> p k n", p=128), writes=[f"wo{j}"], q="pool")
            for tt in range(4):
                i = hc % 2
                hc += 1
                for k in range(32):
                    P.op("pe", lambda e, k=k, i=i, j=j, tt=tt: e.matmul(pH[i][:, 0:256], lhsT=mT[:, k, tt * 128:(tt + 1) * 128], rhs=wo[j][:, k, :],
                                                                      start=(k == 0), stop=(k == 31)),
                         reads=[f"wo{j}", f"mT{k}"], writes=[f"mpH{i}"])
                evac(P, i, hs[i], pH[i][:, 0:256], [f"mpH{i}"], [f"hs{i}"])
                r0 = tb * 512 + tt * 128
                P.dma(Hd[r0:r0 + 128, ct * 256:(ct + 1) * 256], hs[i], reads=[f"hs{i}"])
    st.close()


CAP = 128


def stage_route(nc, P, cst, x_d, Hd, gffn_d, wr_d, br_d, eb_d, Xd, slots_i, wts):
    st = Stage(nc, P)
    identf, ones, onesf = cst["identf"], cst["ones"], cst["onesf"]
    xt = [st.sb(f"rx{i}", [128, D], F32) for i in range(2)]
    hd = [st.sb(f"rh{i}", [128, D], F32) for i in range(2)]
    hn2 = [st.sb(f"rhn{i}", [128, D], F32) for i in range(2)]
    hnb = [st.sb(f"rhnb{i}", [128, D], BF16) for i in range(2)]
    junk2 = [st.sb(f"rjunk{i}", [128, D], BF16) for i in range(2)]
    gf = st.sb("rgf", [128, D], F32)
    hnT2 = [st.sb(f"rhnT{i}", [128, 32, 128], F32) for i in range(2)]
    wr = st.sb("rwr", [128, 32, 72], F32)
    br = st.sb("rbr", [128, 72], F32)
    eb = st.sb("reb", [128, 64], F32)
    sut = st.sb("rsut", [128, 128], BF16)
    cnt = st.sb("rcnt", [128, 64], F32)
    zt = st.sb("rzt", [128, 2048], BF16)
    sm2 = [{}, {}]
    for i_ in range(2):
        for nm, w in (("ss", 1), ("lg", 72), ("mxg", 1), ("ohg", 8), ("eg", 8), ("sumg", 1), ("t3", 64), ("es", 8), ("mx8", 8),
                      ("oh1", 8), ("sel2", 8), ("oh2", 8), ("dd", 1), ("w1", 1), ("w2", 1), ("o1", 64), ("o2", 64), ("posf", 64),
                      ("tmp", 64), ("s1", 1), ("s2", 1)):
            sm2[i_][nm] = st.sb(f"r{i_}_" + nm, [128, w], F32)
    oh64_2 = [st.sb(f"r_oh64_{i_}", [128, 64], BF16) for i_ in range(2)]
    pT2 = [st.ps(f"rpT{i_}", [128, 4, 128], F32) for i_ in range(2)]
    pL2 = [st.ps(f"rpL{i_}", [128, 512], F32)[:, 0:72] for i_ in range(2)]
    pP2 = [st.ps(f"rpP{i_}", [128, 512], F32)[:, 0:64] for i_ in range(2)]
    pC2 = [st.ps(f"rpC{i_}", [128, 512], F32)[:, 0:64] for i_ in range(2)]

    P.dma(gf, gffn_d.partition_broadcast(128), writes=["rgf"])
    P.dma(wr, wr_d, writes=["rwr"])
    P.dma(br, br_d.partition_broadcast(128), writes=["rbr"])
    P.dma(eb, eb_d.partition_broadcast(128), writes=["reb"])
    P.op("pool", lambda e: e.memset(cnt, 0.0), writes=["rcnt"])
    P.op("pool", lambda e: e.affine_select(out=sut, in_=ones, pattern=[[1, 128]], compare_op=ALU.is_gt, fill=0.0,
                                           base=0, channel_multiplier=-1), reads=["ones"], writes=["rsut"])
    P.op("pool", lambda e: e.memset(zt, 0.0), writes=["rzt"])
    Xz = Xd.rearrange("(a p) (b n) -> a b p n", p=128, n=2048)
    zero_done = []
    for a in range(64):
        for b_ in range(2):
            P.dma(Xz[a, b_], zt, reads=["rzt"], writes=[f"Xz{a}_{b_}"])
            zero_done.append(f"Xz{a}_{b_}")

    def route_tile(tt, Q):
        i = tt % 2
        hn, junk, hnT, sm, oh64 = hn2[i], junk2[i], hnT2[i], sm2[i], oh64_2[i]
        pT, pL, pP, pC = pT2[i], pL2[i], pP2[i], pC2[i]

        def v(nm):
            return sm[nm]
        r0 = tt * 128
        Q.dma(xt[i], x_d[OWN0 + r0:OWN0 + r0 + 128, :], writes=[f"rx{i}"])
        Q.dma(hd[i], Hd[r0:r0 + 128, :], writes=[f"rh{i}"])
        Q.op("dve", lambda e, i=i: e.tensor_tensor(out=hd[i], in0=hd[i], in1=xt[i], op=ALU.add), reads=[f"rx{i}", f"rh{i}"], writes=[f"rh{i}"])
        Q.dma(Hd[r0:r0 + 128, :], hd[i], reads=[f"rh{i}"])
        Q.op("act", lambda e, i=i: e.activation(out=junk, in_=hd[i], func=AF.Square, accum_out=v("ss")), reads=[f"rh{i}"], writes=["rjunk", "r_ss"])
        rsqrt_col(Q, v("ss"), v("ss"), EPS, "r_ss", "r_ss", mul=1.0 / D)
        Q.op("dve", lambda e, i=i: e.scalar_tensor_tensor(out=hn, in0=hd[i], scalar=v("ss"), in1=gf, op0=ALU.mult, op1=ALU.mult),
             reads=[f"rh{i}", "r_ss", "rgf"], writes=["rhn"])
        Q.op("act", lambda e, i=i: e.copy(out=hnb[i], in_=hn), reads=["rhn"], writes=[f"rhnb{i}"])
        for g4 in range(8):
            for j in range(4):
                c = g4 * 4 + j
                Q.op("pe", lambda e, c=c, j=j: e.transpose(out=pT[:, j, :], in_=hn[:, c * 128:(c + 1) * 128], identity=identf),
                     reads=["rhn", "identf"], writes=["rpT"])
            Q.op("act", lambda e, g4=g4: e.copy(out=hnT[:, g4 * 4:(g4 + 1) * 4, :], in_=pT), reads=["rpT"], writes=[f"rhnT{g4}"])
        for c in range(32):
            Q.op("pe", lambda e, c=c: e.matmul(pL, lhsT=hnT[:, c, :], rhs=wr[:, c, :], start=(c == 0), stop=(c == 31)),
                 reads=[f"rhnT{c // 4}", "rwr"], writes=["rpL"])
        Q.op("dve", lambda e: e.tensor_tensor(out=v("lg"), in0=pL, in1=br, op=ALU.add), reads=["rpL", "rbr"], writes=["r_lg"])
        lgG = v("lg")[:, 0:8]
        le3 = v("lg")[:, 8:72].rearrange("p (g e) -> p g e", e=8)
        t3 = v("t3").rearrange("p (g e) -> p g e", e=8)
        o1 = v("o1").rearrange("p (g e) -> p g e", e=8)
        o2 = v("o2").rearrange("p (g e) -> p g e", e=8)
        D_ = "dve"
        Q.op(D_, lambda e: e.reduce_max(out=v("mxg"), in_=lgG, axis=AX.X), reads=["r_lg"], writes=["r_mxg"])
        Q.op(D_, lambda e: e.tensor_scalar(out=v("ohg"), in0=lgG, scalar1=v("mxg"), scalar2=None, op0=ALU.is_ge), reads=["r_lg", "r_mxg"], writes=["r_ohg"])
        Q.op(D_, lambda e: e.tensor_scalar(out=v("mxg"), in0=v("mxg"), scalar1=-1.0, scalar2=None, op0=ALU.mult), reads=["r_mxg", "r_ohg"], writes=["r_mxg"])
        Q.op("act", lambda e: e.activation(out=v("eg"), in_=lgG, func=AF.Exp, bias=v("mxg"), accum_out=v("sumg")), reads=["r_lg", "r_mxg"], writes=["r_eg", "r_sumg"])
        Q.op(D_, lambda e: e.reciprocal(out=v("sumg"), in_=v("sumg")), reads=["r_sumg"], writes=["r_sumg"])
        Q.op(D_, lambda e: e.tensor_tensor(out=t3, in0=le3, in1=v("ohg").unsqueeze(2).to_broadcast([128, 8, 8]), op=ALU.mult),
             reads=["r_lg", "r_ohg"], writes=["r_t3"])
        Q.op(D_, lambda e: e.tensor_reduce(out=v("es"), in_=t3.rearrange("p g e -> p e g"), axis=AX.X, op=ALU.add), reads=["r_t3"], writes=["r_es"])
        Q.op(D_, lambda e: e.max(out=v("mx8"), in_=v("es")), reads=["r_es"], writes=["r_mx8"])
        Q.op(D_, lambda e: e.tensor_scalar(out=v("oh1"), in0=v("es"), scalar1=v("mx8")[:, 0:1], scalar2=None, op0=ALU.is_ge), reads=["r_es", "r_mx8"], writes=["r_oh1"])
        Q.op(D_, lambda e: e.tensor_scalar(out=v("sel2"), in0=v("es"), scalar1=v("mx8")[:, 1:2], scalar2=None, op0=ALU.is_ge), reads=["r_es", "r_mx8"], writes=["r_sel2"])
        Q.op(D_, lambda e: e.tensor_tensor(out=v("oh2"), in0=v("sel2"), in1=v("oh1"), op=ALU.subtract), reads=["r_sel2", "r_oh1"], writes=["r_oh2"])
        Q.op(D_, lambda e: e.tensor_tensor(out=v("dd"), in0=v("mx8")[:, 1:2], in1=v("mx8")[:, 0:1], op=ALU.subtract), reads=["r_mx8"], writes=["r_dd"])
        Q.op("act", lambda e: e.activation(out=v("dd"), in_=v("dd"), func=AF.Exp), reads=["r_dd"], writes=["r_dd"])
        Q.op(D_, lambda e: e.tensor_scalar(out=v("w1"), in0=v("dd"), scalar1=1.0, scalar2=None, op0=ALU.add), reads=["r_dd"], writes=["r_w1"])
        Q.op(D_, lambda e: e.reciprocal(out=v("w1"), in_=v("w1")), reads=["r_w1"], writes=["r_w1"])
        Q.op(D_, lambda e: e.tensor_tensor(out=v("w2"), in0=v("dd"), in1=v("w1"), op=ALU.mult), reads=["r_dd", "r_w1"], writes=["r_w2"])
        Q.op(D_, lambda e, tt=tt: e.tensor_tensor(out=wts[:, tt, 0:1], in0=v("w1"), in1=v("sumg"), op=ALU.mult), reads=["r_w1", "r_sumg"], writes=[f"wts{tt}"])
        Q.op(D_, lambda e, tt=tt: e.tensor_tensor(out=wts[:, tt, 1:2], in0=v("w2"), in1=v("sumg"), op=ALU.mult), reads=["r_w2", "r_sumg"], writes=[f"wts{tt}"])
        for onm, src in (("o1", "oh1"), ("o2", "oh2")):
            o3 = o1 if onm == "o1" else o2
            Q.op(D_, lambda e, o3=o3: e.tensor_copy(out=o3, in_=v("ohg").unsqueeze(2).to_broadcast([128, 8, 8])), reads=["r_ohg"], writes=["r_" + onm])
            Q.op(D_, lambda e, o3=o3, src=src: e.tensor_tensor(out=o3, in0=o3, in1=v(src).unsqueeze(1).to_broadcast([128, 8, 8]), op=ALU.mult),
                 reads=["r_" + onm, "r_" + src], writes=["r_" + onm])
        Q.op(D_, lambda e: e.tensor_tensor(out=oh64, in0=v("o1"), in1=v("o2"), op=ALU.add), reads=["r_o1", "r_o2"], writes=["r_oh64"])
        Q.op("pe", lambda e: e.matmul(pP, lhsT=sut, rhs=oh64, start=True, stop=True), reads=["rsut", "r_oh64"], writes=["rpP"])
        Q.op("pe", lambda e: e.matmul(pC, lhsT=ones, rhs=oh64, start=True, stop=True), reads=["ones", "r_oh64"], writes=["rpC"])
        Q.op(D_, lambda e: e.tensor_tensor(out=v("posf"), in0=pP, in1=cnt, op=ALU.add), reads=["rpP", "rcnt"], writes=["r_posf"])
        Q.op(D_, lambda e: e.tensor_tensor(out=cnt, in0=cnt, in1=pC, op=ALU.add), reads=["rpC", "rcnt", "r_posf"], writes=["rcnt"])
        Q.op(D_, lambda e: e.tensor_scalar(out=v("posf"), in0=v("posf"), scalar1=float(CAP - 1), scalar2=None, op0=ALU.min), reads=["r_posf"], writes=["r_posf"])
        Q.op(D_, lambda e: e.tensor_tensor(out=v("posf"), in0=v("posf"), in1=eb, op=ALU.add), reads=["r_posf", "reb"], writes=["r_posf"])
        for k, (onm, snm) in enumerate((("o1", "s1"), ("o2", "s2"))):
            Q.op(D_, lambda e, onm=onm: e.tensor_tensor(out=v("tmp"), in0=v("posf"), in1=v(onm), op=ALU.mult), reads=["r_posf", "r_" + onm], writes=["r_tmp"])
            Q.op(D_, lambda e, snm=snm: e.reduce_sum(out=v(snm), in_=v("tmp"), axis=AX.X), reads=["r_tmp"], writes=["r_" + snm])
            Q.op(D_, lambda e, snm=snm, tt=tt, k=k: e.tensor_copy(out=slots_i[:, tt, k:k + 1], in_=v(snm)), reads=["r_" + snm], writes=[f"slot{tt}_{k}"])
            Q.op("pool", lambda e, tt=tt, k=k, i=i: e.indirect_dma_start(
                out=Xd, out_offset=bass.IndirectOffsetOnAxis(ap=slots_i[:, tt, k:k + 1], axis=0), in_=hnb[i], in_offset=None),
                reads=[f"slot{tt}_{k}", f"rhnb{i}"] + zero_done, writes=(), dma=True)

    SHARED = ("rgf", "rbr", "reb", "rsut", "rwr", "rcnt", "identf", "ones", "onesf", "wts", "slot", "rzt", "Xz")

    class LaneRec:
        def __init__(self, lane):
            self.items, self.lane = [], lane

        def _k(self, keys):
            return [k if k.startswith(SHARED) else f"{k}#{self.lane}" for k in keys]

        def op(self, eng, fn, reads=(), writes=(), dma=False):
            self.items.append(("op", (eng, fn, self._k(reads), self._k(writes)), dict(dma=dma)))

        def dma(self, out, in_, reads=(), writes=(), q="sp"):
            self.items.append(("dma", (out, in_, self._k(reads), self._k(writes)), dict(q=q)))

    streams = []
    for tt in range(16):
        r = LaneRec(tt % 2)
        route_tile(tt, r)
        streams.append(r.items)
    m = max(len(r) for r in streams)
    order = sorted((j * (m // 2) + k, j, k) for j, r in enumerate(streams) for k in range(len(r)))
    for _, j, k in order:
        kind, a, kw = streams[j][k]
        (P.op if kind == "op" else P.dma)(*a, **kw)
    st.close()


def stage_experts(nc, P, cst, Xd, wgu_d, wdn_d, Yd, nexp=64):
    st = Stage(nc, P)
    ident = cst["ident"]
    xe = [st.sb(f"xe{i}", [128, D], BF16) for i in range(2)]
    xeT = [st.sb(f"xeT{i}", [128, 32, 128], BF16) for i in range(2)]
    wg = [st.sb(f"wg{i}", [128, 8, 1536], BF16) for i in range(2)]
    wd = [st.sb(f"wd{i}", [128, 6, 2048], BF16) for i in range(2)]
    sg = st.sb("sg", [128, 768], F32)
    usb = st.sb("usb", [128, 256], F32)
    hh = st.sb("hh", [128, 768], BF16)
    hT = st.sb("hT", [128, 6, 128], BF16)
    ysb = [st.sb(f"ysb{i}", [128, D], F32) for i in range(2)]
    pXb = [st.ps(f"epX{i}", [128, 8, 128], BF16) for i in range(2)]
    pGU = [st.ps(f"epGU{i}", [128, 512], F32) for i in range(3)]
    pHT = st.ps("epHT", [128, 8, 128], BF16)
    pY = [st.ps(f"epY{i}", [128, 512], F32) for i in range(2)]
    gcnt = 0
    dcnt = 0
    for ex in range(nexp):
        i = ex % 2
        P.dma(xe[i], Xd[ex * 128:(ex + 1) * 128, :], writes=[f"xe{i}"])
        for g4 in range(8):
            hf = g4 % 2
            for j in range(4):
                c = g4 * 4 + j
                P.op("pe", lambda e, c=c, j=j, hf=hf, i=i: e.transpose(out=pXb[hf][:, j, :], in_=xe[i][:, c * 128:(c + 1) * 128], identity=ident),
                     reads=[f"xe{i}", "ident"], writes=[f"epX{hf}"])
            P.op("act", lambda e, g4=g4, hf=hf, i=i: e.copy(out=xeT[i][:, g4 * 4:(g4 + 1) * 4, :], in_=pXb[hf][:, 0:4, :]),
                 reads=[f"epX{hf}"], writes=[f"xeT{i}_{g4}", f"epX{hf}"])
        for g8 in range(4):
            j = gcnt % 2
            gcnt += 1
            P.dma(wg[j], wgu_d[ex, g8 * 1024:(g8 + 1) * 1024, :].rearrange("(c p) n -> p c n", p=128), writes=[f"wg{j}"], q="pool")
            for c8 in range(8):
                kc = g8 * 8 + c8
                for nt in range(3):
                    P.op("pe", lambda e, kc=kc, c8=c8, nt=nt, j=j, i=i: e.matmul(pGU[nt], lhsT=xeT[i][:, kc, :], rhs=wg[j][:, c8, nt * 512:(nt + 1) * 512],
                                                                              start=(kc == 0), stop=(kc == 31)),
                         reads=[f"xeT{i}_{kc // 4}", f"wg{j}"], writes=[f"epGU{nt}"])
        P.op("act", lambda e: e.activation(out=sg[:, 0:512], in_=pGU[0], func=AF.Silu), reads=["epGU0"], writes=["sg", "epGU0"])
        P.op("act", lambda e: e.activation(out=sg[:, 512:768], in_=pGU[1][:, 0:256], func=AF.Silu), reads=["epGU1"], writes=["sg", "epGU1"])
        P.op("act", lambda e: e.copy(out=usb, in_=pGU[1][:, 256:512]), reads=["epGU1"], writes=["usb", "epGU1"])
        P.op("dve", lambda e: e.tensor_tensor(out=hh[:, 256:768], in0=pGU[2], in1=sg[:, 256:768], op=ALU.mult), reads=["epGU2", "sg"], writes=["hh", "epGU2"])
        P.op("dve", lambda e: e.tensor_tensor(out=hh[:, 0:256], in0=usb, in1=sg[:, 0:256], op=ALU.mult), reads=["usb", "sg"], writes=["hh"])
        for fc in range(6):
            P.op("pe", lambda e, fc=fc: e.transpose(out=pHT[:, fc, :], in_=hh[:, fc * 128:(fc + 1) * 128], identity=ident), reads=["hh", "ident"], writes=["epHT"])
        P.op("act", lambda e: e.copy(out=hT, in_=pHT[:, 0:6, :]), reads=["epHT"], writes=["hT", "epHT"])
        yi = ex % 2
        for hf in range(2):
            j = dcnt % 2
            dcnt += 1
            P.dma(wd[j], wdn_d[ex, :, hf * 2048:(hf + 1) * 2048].rearrange("(c p) n -> p c n", p=128), writes=[f"wd{j}"], q="pool")
            for c4 in range(4):
                ct = hf * 4 + c4
                pb = ct % 2
                for fc in range(6):
                    P.op("pe", lambda e, fc=fc, c4=c4, pb=pb, j=j: e.matmul(pY[pb], lhsT=hT[:, fc, :], rhs=wd[j][:, fc, c4 * 512:(c4 + 1) * 512],
                                                                         start=(fc == 0), stop=(fc == 5)),
                         reads=["hT", f"wd{j}"], writes=[f"epY{pb}"])
                evac(P, pb, ysb[yi][:, ct * 512:(ct + 1) * 512], pY[pb], [f"epY{pb}"], [f"ysb{yi}_{ct}", f"epY{pb}"])
        P.dma(Yd[ex * 128:(ex + 1) * 128, :], ysb[yi], reads=[f"ysb{yi}_{ct}" for ct in range(8)])
    st.close()


def stage_final(nc, P, Hd, Yd, gfin_d, slots_i, wts, out_d):
    st = Stage(nc, P)
    ht = [st.sb(f"fh{i}", [128, D], F32) for i in range(2)]
    y1 = [st.sb(f"fy1{i}", [128, D], F32) for i in range(2)]
    y2 = [st.sb(f"fy2{i}", [128, D], F32) for i in range(2)]
    gf = st.sb("fgf", [128, D], F32)
    junk = st.sb("fjunk", [128, D], BF16)
    ss = st.sb("fss", [128, 2], F32)
    P.dma(gf, gfin_d.partition_broadcast(128), writes=["fgf"])
    for tt in range(16):
        i = tt % 2
        r0 = tt * 128
        P.dma(ht[i], Hd[r0:r0 + 128, :], writes=[f"fh{i}"])
        for k, yb in enumerate((y1, y2)):
            P.op("pool", lambda e, tt=tt, k=k, yb=yb, i=i: e.indirect_dma_start(
                out=yb[i], out_offset=None, in_=Yd, in_offset=bass.IndirectOffsetOnAxis(ap=slots_i[:, tt, k:k + 1], axis=0)),
                reads=(), writes=[f"fy{k}{i}"], dma=True)
        P.op("dve", lambda e, i=i, tt=tt: e.scalar_tensor_tensor(out=ht[i], in0=y1[i], scalar=wts[:, tt, 0:1], in1=ht[i], op0=ALU.mult, op1=ALU.add),
             reads=[f"fy0{i}", f"fh{i}"], writes=[f"fh{i}"])
        P.op("dve", lambda e, i=i, tt=tt: e.scalar_tensor_tensor(out=ht[i], in0=y2[i], scalar=wts[:, tt, 1:2], in1=ht[i], op0=ALU.mult, op1=ALU.add),
             reads=[f"fy1{i}", f"fh{i}"], writes=[f"fh{i}"])
        P.op("act", lambda e, i=i: e.activation(out=junk, in_=ht[i], func=AF.Square, accum_out=ss[:, i:i + 1]), reads=[f"fh{i}"], writes=["fjunk", f"fss{i}"])
        rsqrt_col(P, ss[:, i:i + 1], ss[:, i:i + 1], EPS, f"fss{i}", f"fss{i}", mul=1.0 / D)
        P.op("dve", lambda e, i=i: e.scalar_tensor_tensor(out=y1[i], in0=ht[i], scalar=ss[:, i:i + 1], in1=gf, op0=ALU.mult, op1=ALU.mult),
             reads=[f"fh{i}", f"fss{i}", "fgf"], writes=[f"fy0{i}"])
        P.dma(out_d[r0:r0 + 128, :], y1[i], reads=[f"fy0{i}"])
    st.close()


def stage_tail(nc, P, x_d, gfin_d, out_d):
    st = Stage(nc, P)
    xt = [st.sb(f"tx{i}", [128, D], F32) for i in range(2)]
    ot = [st.sb(f"to{i}", [128, D], F32) for i in range(2)]
    gf = st.sb("tgf", [128, D], F32)
    junk = st.sb("tjunk", [128, D], BF16)
    ss = st.sb("tss", [128, 2], F32)
    P.dma(gf, gfin_d.partition_broadcast(128), writes=["gf"])
    for t in range(16):
        i = t % 2
        r0 = OWN0 + t * 128
        P.dma(xt[i], x_d[r0:r0 + 128, :], writes=[f"tx{i}"])
        P.op("act", lambda e, i=i: e.activation(out=junk, in_=xt[i], func=AF.Square, accum_out=ss[:, i:i + 1]),
             reads=[f"tx{i}"], writes=["tjunk", f"tss{i}"])
        rsqrt_col(P, ss[:, i:i + 1], ss[:, i:i + 1], EPS, f"tss{i}", f"tss{i}", mul=1.0 / D)
        P.op("dve", lambda e, i=i: e.scalar_tensor_tensor(out=ot[i], in0=xt[i], scalar=ss[:, i:i + 1], in1=gf, op0=ALU.mult, op1=ALU.mult),
             reads=[f"tx{i}", f"tss{i}", "gf"], writes=[f"to{i}"])
        P.dma(out_d[t * 128:(t + 1) * 128, :], ot[i], reads=[f"to{i}"])
    st.close()


class SplitRows:
    def __init__(self, parts, split):
        self.parts, self.split = parts, split

    def __getitem__(self, key):
        rs, cs = key
        if rs.stop <= self.split:
            return self.parts[0][rs.start:rs.stop, cs]
        assert rs.start >= self.split
        return self.parts[1][rs.start - self.split:rs.stop - self.split, cs]


def build_program(debug=False):
    nc = bass.Bass("TRN2", target_bir_lowering=False)
    ext = lambda name, shape, dt=F32: nc.dram_tensor(name, shape, dt, kind="ExternalInput").ap()
    x_d = ext("xs", [TL, D])
    gmix_d = ext("g_mix", [D])
    win_d = ext("w_in", [D, NCOL])
    convw = ext("convw", [128, 48, 4])
    alog = ext("a_log", [16])
    dtbias = ext("dt_bias", [16])
    gnormw = ext("gdn_norm_w", [128])
    gmk_d = ext("gdn_lvl_masks", [128, 8, 128], BF16)
    KD = {nm: ext("k_" + nm, shp, dt) for nm, shp, dt in NSA_CONST_SPECS}
    w1k, w2k, pek = ext("cmp_w1_k", [32, 128, 128]), ext("cmp_w2_k", [128, 128]), ext("cmp_peT_k", [128, 32])
    w1v, w2v, pev = ext("cmp_w1_v", [32, 128, 128]), ext("cmp_w2_v", [128, 128]), ext("cmp_peT_v", [128, 32])
    wa_d, wb_d, wo_d = ext("w_branch_gdn", [2048, D]), ext("w_branch_nsa", [2048, D]), ext("w_out", [D, D])
    gffn_d = ext("g_ffn", [D])
    wr_d, br_d, eb_d = ext("w_router", [128, 32, 72]), ext("b_router", [72]), ext("ebase", [64])
    wgu_d, wdn_d = ext("w_gate_up", [64, D, 1536]), ext("w_down", [64, 768, D])
    gfin = ext("g_final", [D])
    out_d = nc.dram_tensor("out", [TL - OWN0, D], F32, kind="ExternalOutput").ap()
    PT_d = SplitRows([nc.dram_tensor("PT_scratch_a", [C_NKV, TL], F32).ap(),
                      nc.dram_tensor("PT_scratch_b", [NCOL - C_NKV, TL], F32).ap()], C_NKV)
    sk = "ExternalOutput" if debug else "Internal"
    OG_d = nc.dram_tensor("OG_scratch", [2048, 2048], BF16, kind=sk).ap()
    ON_d = nc.dram_tensor("ON_scratch", [2048, 2048], BF16, kind=sk).ap()
    Hd = nc.dram_tensor("H_scratch", [2048, D], F32, kind=sk).ap()
    Xd = nc.dram_tensor("X_scratch", [64 * CAP, D], BF16).ap()
    Yd = nc.dram_tensor("Y_scratch", [64 * CAP, D], F32).ap()
    P = Prog(nc)
    cst = make_consts(nc, P)
    ab_tok = nc.alloc_sbuf_tensor("ab_tok", [128, 32, 32], F32).ap()
    slots_i = nc.alloc_sbuf_tensor("slots_i", [128, 16, 2], I32).ap()
    wts = nc.alloc_sbuf_tensor("wts", [128, 16, 2], F32).ap()
    stage_proj(nc, P, cst, x_d, gmix_d, win_d, PT_d, ab_tok)
    stage_gdn(nc, P, cst, PT_d, ab_tok, convw, alog, dtbias, gnormw, gmk_d, OG_d)
    stage_nsa(nc, P, cst, PT_d, KD, w1k, w2k, pek, w1v, w2v, pev, ON_d)
    stage_merge(nc, P, cst, PT_d, OG_d, ON_d, wa_d, wb_d, wo_d, Hd)
    stage_route(nc, P, cst, x_d, Hd, gffn_d, wr_d, br_d, eb_d, Xd, slots_i, wts)
    stage_experts(nc, P, cst, Xd, wgu_d, wdn_d, Yd)
    stage_final(nc, P, Hd, Yd, gfin, slots_i, wts, out_d)
    P.emit()
    return nc


def gdn_level_masks():
    import ml_dtypes
    c = np.arange(128)[:, None]
    s_ = np.arange(128)[None, :]
    m = np.zeros((128, 8, 128), np.float32)
    for k in range(7):
        mk = (((c >> k) & 1) == 1) & (((s_ >> k) & 1) == 0) & ((c >> (k + 1)) == (s_ >> (k + 1)))
        m[:, k, :] = mk.T
        if k == 0:
            m[:, 7, :] = mk
    return m.astype(ml_dtypes.bfloat16)


def host_shared(inputs):
    f = lambda k: np.ascontiguousarray(np.asarray(inputs[k])[0])
    wr = np.concatenate([f("w_group"), f("w_expert")], axis=1)
    return {
        "g_mix": f("g_mix"), "w_in": f("w_in"),
        "convw": np.ascontiguousarray(f("gdn_conv_w").reshape(4, 48, 128).transpose(2, 1, 0)),
        "gdn_lvl_masks": gdn_level_masks(),
        "a_log": f("gdn_a_log"), "dt_bias": f("gdn_dt_bias"), "gdn_norm_w": f("gdn_norm_w"),
        "cmp_w1_k": f("cmp_w1_k"), "cmp_w2_k": f("cmp_w2_k"), "cmp_peT_k": np.ascontiguousarray(f("cmp_pe_k").T),
        "cmp_w1_v": f("cmp_w1_v"), "cmp_w2_v": f("cmp_w2_v"), "cmp_peT_v": np.ascontiguousarray(f("cmp_pe_v").T),
        "w_branch_gdn": f("w_branch_gdn"), "w_branch_nsa": f("w_branch_nsa"), "w_out": f("w_out"),
        "g_ffn": f("g_ffn"),
        "w_router": np.ascontiguousarray(wr.reshape(32, 128, 72).transpose(1, 0, 2)),
        "b_router": np.concatenate([f("b_group"), f("b_expert")]),
        "ebase": (np.arange(64) * CAP).astype(np.float32),
        "w_gate_up": f("w_gate_up"), "w_down": f("w_down"),
        "g_final": np.ascontiguousarray(np.asarray(inputs["g_final"])),
    }


def kernel(**inputs):
    x = np.asarray(inputs["x"], dtype=np.float32)
    B, T, _ = x.shape
    nc = build_program()
    shared = host_shared(inputs)
    consts = [nsa_host_consts(0), nsa_host_consts(1)]
    in_maps = []
    for c in range(8):
        b, half = c // 2, c % 2
        xs = np.zeros((TL, D), np.float32)
        if half == 0:
            xs[OWN0:] = x[b, :2048]
        else:
            xs[:] = x[b]
        m = dict(shared)
        m["xs"] = xs
        for k, v in consts[half].items():
            m["k_" + k] = v
        in_maps.append(m)
    res = run_bass_kernel_spmd(nc, in_maps, core_ids=list(range(8)))
    out = np.zeros((B, T, D), np.float32)
    for c in range(8):
        b, half = c // 2, c % 2
        out[b, half * 2048:(half + 1) * 2048] = res.results[c]["out"]
    return out
```

```python
from contextlib import ExitStack
import numpy as np
import concourse.bass as bass
import concourse.mybir as mybir
from concourse.bass_utils import run_bass_kernel_spmd

F32 = mybir.dt.float32
BF16 = mybir.dt.bfloat16
I32 = mybir.dt.int32
AF = mybir.ActivationFunctionType
ALU = mybir.AluOpType
AX = mybir.AxisListType

SEM_LIM = 24000
D = 4096
TL = 4096
OWN0 = 2048
NCOL = 21584
EPS = 1e-6
C_QKV, C_Z, C_A, C_B, C_NQ, C_NKV, C_NG, C_MG = 0, 6144, 8192, 8208, 8224, 10272, 13344, 13392
NEG = -30000.0


class Prog:
    ENGS = ("pe", "act", "dve", "pool", "sp")
    NDMA = {"sp": 16, "pool": 8, "act": 4}

    def __init__(self, nc):
        self.nc = nc
        self.ops = []
        self.last_w = {}
        self.readers = {}
        self.dma_count = {"sp": 0, "pool": 0, "act": 0}
        self.dma_ops = {"sp": [], "pool": [], "act": []}
        self.last_on = {}
        self.barrier_idx = None

    def op(self, eng, fn, reads=(), writes=(), dma=False):
        idx = len(self.ops)
        deps = set()
        if self.barrier_idx is not None:
            deps.add(self.barrier_idx)
        for r in reads:
            w = self.last_w.get(r)
            if w is not None:
                deps.add(w)
        for w_ in writes:
            w = self.last_w.get(w_)
            if w is not None:
                deps.add(w)
            for r in self.readers.get(w_, ()):
                deps.add(r)
        o = dict(eng=eng, fn=fn, deps=deps, dma=dma, idx=idx, marked=False)
        if dma:
            n = self.dma_count[eng]
            self.dma_count[eng] = n + 1
            nd = self.NDMA[eng]
            o["dma_n"] = n
            lst = self.dma_ops[eng]
            if n >= nd:
                deps.add(lst[n - nd])
            lst.append(idx)
        else:
            self.last_on[eng] = idx
        deps.discard(idx)
        self.ops.append(o)
        for r in reads:
            self.readers.setdefault(r, []).append(idx)
        for w_ in writes:
            self.last_w[w_] = idx
            self.readers[w_] = []
        return idx

    def dma(self, out, in_, reads=(), writes=(), q="sp", **kw):
        return self.op(q, lambda e: e.dma_start(out=out, in_=in_, **kw), reads, writes, dma=True)

    def barrier(self):
        deps = set(self.last_on.values())
        for q, lst in self.dma_ops.items():
            deps.update(lst[-self.NDMA[q]:])
        idx = self.op("sp", lambda e: e.nop(), (), ())
        self.ops[idx]["deps"] |= deps
        self.ops[idx]["deps"].discard(idx)
        self.barrier_idx = idx
        self.last_w = {}
        self.readers = {}

    def emit(self):
        nc = self.nc
        ops = self.ops
        for o in ops:
            for d in o["deps"]:
                p = ops[d]
                if p["eng"] == "pe" and o["eng"] == "pe" and not p["dma"] and not o["dma"]:
                    continue
                p["marked"] = True
        cnt = {e: 0 for e in self.ENGS}
        for o in ops:
            if o["dma"]:
                nd = self.NDMA[o["eng"]]
                o["tok"] = ("dma_" + o["eng"], o["dma_n"] % nd, 16 * (o["dma_n"] // nd + 1))
            elif o["marked"]:
                m = cnt[o["eng"]]
                cnt[o["eng"]] = m + 1
                o["tok"] = (o["eng"], m // SEM_LIM, m % SEM_LIM + 1)
            else:
                o["tok"] = None
        sems = {}
        for e in self.ENGS:
            for j in range(cnt[e] // SEM_LIM + 1):
                sems[(e, j)] = nc.alloc_semaphore(name=f"s_{e}_{j}")
        for q, nd in self.NDMA.items():
            if self.dma_count[q] > 0:
                for j in range(nd):
                    sems[("dma_" + q, j)] = nc.alloc_semaphore(name=f"d_{q}_{j}")
        final_dma = {}
        for o in ops:
            if o["dma"]:
                t = o["tok"]
                final_dma[(t[0], t[1])] = max(final_dma.get((t[0], t[1]), 0), t[2])

        def run_engine(ename, eobj, is_last_waiter=False):
            waited = {}
            for o in ops:
                if o["eng"] != ename:
                    continue
                for d in sorted(o["deps"]):
                    p = ops[d]
                    if p["eng"] == "pe" and ename == "pe" and not p["dma"] and not o["dma"]:
                        continue
                    t = p["tok"]
                    key = (t[0], t[1])
                    if waited.get(key, 0) >= t[2]:
                        continue
                    eobj.wait_ge(sems[key], t[2])
                    waited[key] = t[2]
                ins = o["fn"](eobj)
                t = o["tok"]
                if t is not None:
                    ins.then_inc(sems[(t[0], t[1])], 16 if o["dma"] else 1)
            if is_last_waiter:
                for key, v in final_dma.items():
                    if waited.get(key, 0) < v:
                        eobj.wait_ge(sems[key], v)

        with nc.Block() as block:
            @block.sync
            def _(e):
                run_engine("sp", e, True)

            @block.tensor
            def _(e):
                run_engine("pe", e)

            @block.scalar
            def _(e):
                run_engine("act", e)

            @block.vector
            def _(e):
                run_engine("dve", e)

            @block.gpsimd
            def _(e):
                run_engine("pool", e)


class Stage:
    def __init__(self, nc, P):
        self.nc, self.P = nc, P
        self.es = ExitStack()

    def sb(self, name, shape, dt):
        return self.es.enter_context(self.nc.sbuf_tensor(name, shape, dt)).ap()

    def ps(self, name, shape, dt=F32):
        return self.es.enter_context(self.nc.psum_tensor(name, shape, dt)).ap()

    def close(self):
        self.P.barrier()
        self.es.close()


def evac(P, k, out, in_, reads, writes):
    if k % 2 == 0:
        return P.op("act", lambda e: e.copy(out=out, in_=in_), reads, writes)
    return P.op("dve", lambda e: e.tensor_copy(out=out, in_=in_), reads, writes)


def rsqrt_col(P, out, in_, add, rkey, wkey, mul=1.0):
    P.op("dve", lambda e: e.tensor_scalar(out=out, in0=in_, scalar1=float(mul), scalar2=float(add), op0=ALU.mult, op1=ALU.add),
         reads=[rkey], writes=[wkey])
    P.op("act", lambda e: e.sqrt(out=out, in_=out), reads=[wkey], writes=[wkey])
    P.op("dve", lambda e: e.reciprocal(out=out, in_=out), reads=[wkey], writes=[wkey])


def make_consts(nc, P):
    c = {}
    c["identf"] = nc.alloc_sbuf_tensor("identf", [128, 128], F32).ap()
    c["ident"] = nc.alloc_sbuf_tensor("ident", [128, 128], BF16).ap()
    c["onesf"] = nc.alloc_sbuf_tensor("onesf", [128, 128], F32).ap()
    c["ones"] = nc.alloc_sbuf_tensor("ones", [128, 128], BF16).ap()
    P.op("pool", lambda e: e.memset(c["identf"], 0.0), writes=["identf"])
    P.op("pool", lambda e: e.affine_select(out=c["identf"], in_=c["identf"], pattern=[[-1, 128]],
                                           compare_op=ALU.not_equal, fill=1.0, base=0, channel_multiplier=1),
         reads=["identf"], writes=["identf"])
    P.op("dve", lambda e: e.tensor_copy(out=c["ident"], in_=c["identf"]), reads=["identf"], writes=["ident"])
    P.op("pool", lambda e: e.memset(c["onesf"], 1.0), writes=["onesf"])
    P.op("pool", lambda e: e.memset(c["ones"], 1.0), writes=["ones"])
    return c


def col_tiles(prefix, tb=1):
    if prefix and tb == 0:
        groups = [(C_QKV + 2048, 4096), (C_NKV, 2048)]
    elif prefix:
        groups = [(C_QKV + 2048, 4096), (C_NKV, 3072)]
    else:
        groups = [(C_QKV, 6144), (C_Z, 2048), (C_NQ, 2048), (C_NKV, 3072), (C_NG, 48), (C_MG, 8192)]
    out = []
    for c0, n in groups:
        o = 0
        while o < n:
            w = min(256, n - o)
            out.append((c0 + o, w))
            o += w
    return out


def stage_proj(nc, P, cst, x_d, gmix_d, win_d, PT_d, ab_tok, nblocks=4):
    st = Stage(nc, P)
    xt = [st.sb(f"xt{i}", [128, D], F32) for i in range(2)]
    xb = [st.sb(f"xb{i}", [128, D], BF16) for i in range(2)]
    gm = st.sb("gm", [128, D], F32)
    junk = st.sb("junk", [128, D], BF16)
    xnT = st.sb("xnT", [128, 32, 1024], BF16)
    wt = [st.sb(f"wt{i}", [128, 32, 256], BF16) for i in range(2)]
    og = [st.sb(f"og{i}", [128, 1024], F32) for i in range(2)]
    ss = st.sb("ss", [128, 2], F32)
    rs = st.sb("rs", [128, 2], F32)
    wab = st.sb("wab", [128, 32, 32], BF16)
    ptp = [st.ps(f"ptp{i}", [128, 4, 128], BF16) for i in range(2)]
    pp = [st.ps(f"pp{i}", [128, 1024], F32) for i in range(2)]
    pab = st.ps("pab", [128, 32], F32)

    P.dma(gm, gmix_d.partition_broadcast(128), writes=["gm"])
    P.op("dve", lambda e: e.tensor_scalar(out=gm, in0=gm, scalar1=float(np.sqrt(D)), scalar2=None, op0=ALU.mult),
         reads=["gm"], writes=["gm"])
    P.dma(wab, win_d[:, C_A:C_A + 32].rearrange("(c p) n -> p c n", p=128), writes=["wab"], q="pool")

    wcount = 0
    ocount = 0
    tcount = 0
    for tb in range(4 - nblocks, 4):
        prefix = tb < 2
        for t in range(8):
            i = tcount % 2
            tcount += 1
            r0 = tb * 1024 + t * 128
            P.dma(xt[i], x_d[r0:r0 + 128, :], writes=[f"xt{i}"])
            P.op("act", lambda e, i=i: e.activation(out=junk, in_=xt[i], func=AF.Square, accum_out=ss[:, i:i + 1]),
                 reads=[f"xt{i}"], writes=["junk", f"ss{i}"])
            rsqrt_col(P, rs[:, i:i + 1], ss[:, i:i + 1], float(D * EPS), f"ss{i}", f"rs{i}")
            P.op("dve", lambda e, i=i: e.scalar_tensor_tensor(out=xb[i], in0=xt[i], scalar=rs[:, i:i + 1], in1=gm,
                                                              op0=ALU.mult, op1=ALU.mult),
                 reads=[f"xt{i}", f"rs{i}", "gm"], writes=[f"xb{i}"])
            for g in range(8):
                pj = g % 2
                for j in range(4):
                    c = g * 4 + j
                    P.op("pe", lambda e, c=c, j=j, pj=pj, i=i: e.transpose(out=ptp[pj][:, j, :], in_=xb[i][:, c * 128:(c + 1) * 128],
                                                                         identity=cst["ident"]),
                         reads=[f"xb{i}", "ident"], writes=[f"ptp{pj}"])
                evac(P, g, xnT[:, g * 4:(g + 1) * 4, t * 128:(t + 1) * 128], ptp[pj], [f"ptp{pj}"], [f"xnT{t}_{g}"])
        for t in range(8):
            for c in range(32):
                P.op("pe", lambda e, c=c, t=t: e.matmul(pab, lhsT=xnT[:, c, t * 128:(t + 1) * 128], rhs=wab[:, c, :],
                                                        start=(c == 0), stop=(c == 31)),
                     reads=[f"xnT{t}_{c // 4}", "wab"], writes=["pab"])
            evac(P, t, ab_tok[:, tb * 8 + t, :], pab, ["pab"], [f"ab{tb * 8 + t}"])
        for (c0, ncw) in col_tiles(prefix, tb):
            j = wcount % 2
            wcount += 1
            P.dma(wt[j][:, :, :ncw], win_d[:, c0:c0 + ncw].rearrange("(c p) n -> p c n", p=128),
                  writes=[f"wt{j}"], q="pool")
            for sub in range(0, ncw, 128):
                m = min(128, ncw - sub)
                k = ocount % 2
                ocount += 1
                for half in range(2):
                    for c in range(32):
                        P.op("pe", lambda e, c=c, j=j, k=k, half=half, sub=sub, m=m: e.matmul(
                            pp[k][:m, half * 512:(half + 1) * 512], lhsT=wt[j][:, c, sub:sub + m],
                            rhs=xnT[:, c, half * 512:(half + 1) * 512], start=(c == 0), stop=(c == 31)),
                            reads=[f"wt{j}"] + [f"xnT{t}_{c // 4}" for t in range(half * 4, half * 4 + 4)],
                            writes=[f"pp{k}_{half}"])
                    evac(P, half, og[k][:m, half * 512:(half + 1) * 512], pp[k][:m, half * 512:(half + 1) * 512],
                         [f"pp{k}_{half}"], [f"og{k}_{half}"])
                P.dma(PT_d[c0 + sub:c0 + sub + m, tb * 1024:(tb + 1) * 1024], og[k][:m, :],
                      reads=[f"og{k}_0", f"og{k}_1"])
        if tb == 1:
            for o in range(0, 2048, 256):
                c0 = C_QKV + o
                j = wcount % 2
                wcount += 1
                P.dma(wt[j], win_d[:, c0:c0 + 256].rearrange("(c p) n -> p c n", p=128), writes=[f"wt{j}"], q="pool")
                for sub in (0, 128):
                    k = ocount % 2
                    ocount += 1
                    for c in range(32):
                        P.op("pe", lambda e, c=c, j=j, k=k, sub=sub: e.matmul(
                            pp[k][:, 0:128], lhsT=wt[j][:, c, sub:sub + 128], rhs=xnT[:, c, 896:1024], start=(c == 0), stop=(c == 31)),
                            reads=[f"wt{j}", f"xnT7_{c // 4}"], writes=[f"pp{k}_0"])
                    evac(P, 0, og[k][:, 0:128], pp[k][:, 0:128], [f"pp{k}_0"], [f"og{k}_0"])
                    P.dma(PT_d[c0 + sub:c0 + sub + 128, tb * 1024 + 896:(tb + 1) * 1024], og[k][:, 0:128], reads=[f"og{k}_0"])
    st.close()


def stage_gdn(nc, P, cst, PT_d, ab_tok, convw_d, alog_d, dtb_d, normw_d, gmk_d, OG_d, groups=range(8), dbg=None):
    st = Stage(nc, P)
    identf, ident, onesf, ones = cst["identf"], cst["ident"], cst["onesf"], cst["ones"]
    NT = 32
    sc = {}
    for nm in ("g", "gc", "ngc", "beta", "nbeta", "be1", "e1", "e2", "dch"):
        sc[nm] = st.sb("sc_" + nm, [128, NT * 16], F32)
    dtb = st.sb("dtb", [128, 16], F32)
    nA = st.sb("nA", [128, 16], F32)
    normw = st.sb("normw", [128, 1], F32)
    cw = st.sb("cw", [128, 48, 4], F32)
    zeros = st.sb("zeros", [128, 128], F32)
    Tri = st.sb("Tri", [128, 128], F32)
    TriN = st.sb("TriN", [128, 128], F32)
    maskU = st.sb("maskU", [128, 128], F32)
    maskL = st.sb("maskL", [128, 128], F32)
    gmk = st.sb("gmk", [128, 8, 128], BF16)
    P.dma(gmk, gmk_d, writes=["gmk"])
    xr = st.sb("xr", [128, TL], F32)
    yy = st.sb("yy", [128, TL], F32)
    sq = st.sb("sq", [128, TL], BF16)
    rt = [st.sb(f"rt{i}", [128, 512], F32) for i in range(2)]
    qkv = [[st.sb(f"qkv{hs}_{w}", [128, TL], BF16) for w in range(3)] for hs in range(2)]
    zs = [st.sb(f"zs{hs}", [128, 2048], F32) for hs in range(2)]
    OGb = [st.sb(f"OGb{hs}", [128, 2048], BF16) for hs in range(2)]
    pbig = st.ps("pbig", [128, 512], F32)
    pbig2 = st.ps("pbig2", [128, 512], F32)

    P.dma(dtb, dtb_d.partition_broadcast(128), writes=["dtb"])
    P.dma(nA, alog_d.partition_broadcast(128), writes=["nA"])
    P.dma(normw, normw_d.rearrange("(c o) -> c o", o=1), writes=["normw"])
    P.dma(cw, convw_d, writes=["cw"])
    P.op("pool", lambda e: e.memset(zeros, 0.0), writes=["zeros"])
    P.op("pool", lambda e: e.affine_select(out=Tri, in_=onesf, pattern=[[1, 128]], compare_op=ALU.is_ge, fill=0.0,
                                           base=0, channel_multiplier=-1), reads=["onesf"], writes=["Tri"])
    P.op("dve", lambda e: e.tensor_scalar(out=TriN, in0=Tri, scalar1=-1.0, scalar2=None, op0=ALU.mult),
         reads=["Tri"], writes=["TriN"])
    P.op("pool", lambda e: e.affine_select(out=maskU, in_=zeros, pattern=[[1, 128]], compare_op=ALU.is_ge, fill=NEG,
                                           base=0, channel_multiplier=-1), reads=["zeros"], writes=["maskU"])
    P.op("pool", lambda e: e.affine_select(out=maskL, in_=zeros, pattern=[[-1, 128]], compare_op=ALU.is_gt, fill=NEG,
                                           base=0, channel_multiplier=1), reads=["zeros"], writes=["maskL"])
    if dbg == 1:
        st.close()
        return
    g3 = sc["g"].rearrange("p (t h) -> p t h", h=16)
    b3 = sc["beta"].rearrange("p (t h) -> p t h", h=16)
    abk = [f"ab{t}" for t in range(NT)]
    P.op("act", lambda e: e.activation(out=nA, in_=nA, func=AF.Exp), reads=["nA"], writes=["nA"])
    P.op("dve", lambda e: e.tensor_scalar(out=nA, in0=nA, scalar1=-1.0, scalar2=None, op0=ALU.mult), reads=["nA"], writes=["nA"])
    P.op("dve", lambda e: e.tensor_tensor(out=g3, in0=ab_tok[:, :, 0:16], in1=dtb.unsqueeze(1).to_broadcast([128, NT, 16]),
                                          op=ALU.add), reads=abk + ["dtb"], writes=["g"])
    P.op("act", lambda e: e.activation(out=sc["g"], in_=sc["g"], func=AF.Exp), reads=["g"], writes=["g"])
    P.op("act", lambda e: e.activation(out=sc["g"], in_=sc["g"], func=AF.Ln, bias=1.0), reads=["g"], writes=["g"])
    P.op("dve", lambda e: e.tensor_tensor(out=g3, in0=g3, in1=nA.unsqueeze(1).to_broadcast([128, NT, 16]), op=ALU.mult),
         reads=["g", "nA"], writes=["g"])
    P.op("act", lambda e: e.activation(out=b3, in_=ab_tok[:, :, 16:32], func=AF.Sigmoid), reads=abk, writes=["beta"])
    if dbg == 2:
        st.close()
        return
    P.op("pe", lambda e: e.matmul(pbig, lhsT=Tri, rhs=sc["g"], start=True, stop=True), reads=["Tri", "g"], writes=["pbig"])
    P.op("pe", lambda e: e.matmul(pbig2, lhsT=onesf, rhs=sc["g"], start=True, stop=True), reads=["onesf", "g"], writes=["pbig2"])
    P.op("act", lambda e: e.copy(out=sc["gc"], in_=pbig), reads=["pbig"], writes=["gc"])
    P.op("act", lambda e: e.activation(out=sc["e1"], in_=pbig, func=AF.Exp), reads=["pbig"], writes=["e1"])
    if dbg == 3:
        st.close()
        return
    P.op("dve", lambda e: e.tensor_scalar(out=sc["ngc"], in0=sc["gc"], scalar1=-1.0, scalar2=None, op0=ALU.mult), reads=["gc"], writes=["ngc"])
    P.op("act", lambda e: e.copy(out=sc["e2"], in_=pbig2), reads=["pbig2"], writes=["e2"])
    P.op("dve", lambda e: e.tensor_tensor(out=sc["e2"], in0=sc["e2"], in1=sc["gc"], op=ALU.subtract), reads=["e2", "gc"], writes=["e2"])
    if dbg == 5:
        st.close()
        return
    P.op("act", lambda e: e.activation(out=sc["e2"], in_=sc["e2"], func=AF.Exp), reads=["e2"], writes=["e2"])
    if dbg == 6:
        st.close()
        return
    P.op("act", lambda e: e.activation(out=sc["dch"], in_=pbig2, func=AF.Exp), reads=["pbig2"], writes=["dch"])
    P.op("dve", lambda e: e.tensor_tensor(out=sc["be1"], in0=sc["beta"], in1=sc["e1"], op=ALU.mult), reads=["beta", "e1"], writes=["be1"])
    P.op("dve", lambda e: e.tensor_scalar(out=sc["nbeta"], in0=sc["beta"], scalar1=-1.0, scalar2=None, op0=ALU.mult),
         reads=["beta"], writes=["nbeta"])
    SCK = ["g", "gc", "ngc", "beta", "nbeta", "be1", "e1", "e2", "dch"]
    if dbg == 4:
        st.close()
        return

    W = []
    for hs in range(2):
        w = {}
        if hs == 0:
            w["pA"] = pbig[:, 0:128]
            w["pB"] = pbig2[:, 0:128]
        else:
            w["pA"] = st.ps(f"pA{hs}", [128, 512], F32)[:, 0:128]
            w["pB"] = st.ps(f"pB{hs}", [128, 512], F32)[:, 0:128]
        w["pC"] = st.ps(f"pC{hs}", [128, 512], F32)[:, 0:256]
        w["pT"] = st.ps(f"pT{hs}", [128, 8, 128], BF16)[:, 0:2, :]
        w["Gb"] = st.sb(f"Gb{hs}", [128, 128], F32)
        w["E"] = st.sb(f"E{hs}", [128, 128], F32)
        w["ET"] = st.sb(f"ET{hs}", [128, 128], F32)
        w["X"] = [st.sb(f"X{hs}_{i}", [128, 128], BF16) for i in range(2)]
        w["XT"] = [st.sb(f"XT{hs}_{i}", [128, 128], BF16) for i in range(2)]
        w["Yb"] = st.sb(f"Yb{hs}", [128, 256], BF16)
        w["Pm"] = st.sb(f"Pm{hs}", [128, 128], BF16)
        w["NkTs"] = [st.sb(f"NkT{hs}_{l}", [128, 128], BF16) for l in range(6)]
        for pb in range(2):
            w[f"Y{pb}"] = st.sb(f"Y{hs}_{pb}", [128, 256], F32)
            w[f"attnT{pb}"] = st.sb(f"attnT{hs}_{pb}", [128, 128], BF16)
            w[f"kout{pb}"] = st.sb(f"kout{hs}_{pb}", [128, 128], BF16)
            w[f"wT{pb}"] = st.sb(f"wT{hs}_{pb}", [128, 128], BF16)
        w["S"] = st.sb(f"S{hs}", [128, 128], F32)
        w["Sb"] = st.sb(f"Sb{hs}", [128, 128], BF16)
        w["vnew"] = st.sb(f"vnew{hs}", [128, 128], BF16)
        w["oS"] = st.sb(f"oS{hs}", [128, 128], F32)
        w["o"] = st.sb(f"o{hs}", [128, 128], F32)
        w["on"] = st.sb(f"on{hs}", [128, 128], BF16)
        w["junk"] = st.sb(f"gjunk{hs}", [128, 128], BF16)
        w["ss"] = st.sb(f"gss{hs}", [128, 1], F32)
        W.append(w)

    def K(hs, nm):
        if hs == 0 and nm == "pA":
            return "pbig"
        if hs == 0 and nm == "pB":
            return "pbig2"
        return f"{nm}@{hs}"

    def preprocess(grp):
        for hs in range(2):
            h = grp * 2 + hs
            for wi, coff in enumerate((0, 2048, 4096)):
                ct = (coff + h * 128) // 128
                row0 = C_QKV + coff + h * 128
                dst = qkv[hs][wi]
                lo = OWN0 - 3 if wi == 0 else 0
                P.dma(xr[:, lo:], PT_d[row0:row0 + 128, lo:TL], writes=["xr"])
                eng = "dve"
                P.op(eng, lambda e, ct=ct, lo=lo: e.tensor_scalar(out=yy[:, lo:], in0=xr[:, lo:], scalar1=cw[:, ct, 3:4], scalar2=None, op0=ALU.mult),
                     reads=["xr", "cw"], writes=["yy"])
                for sh in (1, 2, 3):
                    P.op(eng, lambda e, ct=ct, sh=sh, lo=lo: e.scalar_tensor_tensor(out=yy[:, lo + sh:], in0=xr[:, lo:TL - sh],
                                                                                 scalar=cw[:, ct, 3 - sh:4 - sh], in1=yy[:, lo + sh:],
                                                                                 op0=ALU.mult, op1=ALU.add),
                         reads=["xr", "cw", "yy"], writes=["yy"])
                if wi == 2:
                    P.op("act", lambda e, dst=dst: e.activation(out=dst, in_=yy, func=AF.Silu), reads=["yy"], writes=[K(hs, f"qkv{wi}")])
                    continue
                c0 = OWN0 if wi == 0 else 0
                P.op("act", lambda e, c0=c0: e.activation(out=yy[:, c0:], in_=yy[:, c0:], func=AF.Silu), reads=["yy"], writes=["yy"])
                P.op("pool", lambda e, c0=c0: e.tensor_tensor(out=sq[:, c0:], in0=yy[:, c0:], in1=yy[:, c0:], op=ALU.mult), reads=["yy"], writes=["sq"])
                scale = float(128 ** -0.5) if wi == 0 else 1.0
                for ch in range(c0 // 512, 8):
                    cs = slice(ch * 512, (ch + 1) * 512)
                    r = rt[ch % 2]
                    P.op("pe", lambda e, cs=cs: e.matmul(pbig, lhsT=ones, rhs=sq[:, cs], start=True, stop=True),
                         reads=["ones", "sq"], writes=["pbig"])
                    P.op("act", lambda e, r=r: e.activation(out=r, in_=pbig, func=AF.Ln, bias=float(EPS)), reads=["pbig"], writes=[f"rt{ch % 2}"])
                    P.op("act", lambda e, r=r: e.activation(out=r, in_=r, func=AF.Exp, scale=-0.5), reads=[f"rt{ch % 2}"], writes=[f"rt{ch % 2}"])
                    P.op("dve", lambda e, r=r, cs=cs, dst=dst, scale=scale: e.scalar_tensor_tensor(
                        out=dst[:, cs], in0=yy[:, cs], scalar=scale, in1=r, op0=ALU.mult, op1=ALU.mult),
                        reads=["yy", f"rt{ch % 2}"], writes=[K(hs, f"qkv{wi}")])
            P.dma(zs[hs], PT_d[C_Z + h * 128:C_Z + (h + 1) * 128, OWN0:TL], writes=[K(hs, "zs")])
            P.op("act", lambda e, hs=hs: e.activation(out=zs[hs], in_=zs[hs], func=AF.Silu), reads=[K(hs, "zs")], writes=[K(hs, "zs")])
            P.op("pool", lambda e, hs=hs: e.memset(W[hs]["S"], 0.0), writes=[K(hs, "S")])
            P.op("pool", lambda e, hs=hs: e.memset(W[hs]["Sb"], 0.0), writes=[K(hs, "Sb")])

    def pre(grp, hs, n, Q):
        h = grp * 2 + hs
        w = W[hs]
        pb = n % 2
        col = n * 16 + h
        cs = slice(n * 128, (n + 1) * 128)
        qT, kT, vT = (qkv[hs][i][:, cs] for i in range(3))
        kq, kk, kv = (K(hs, f"qkv{i}") for i in range(3))
        sv = lambda nm: sc[nm][:, col:col + 1]
        k_ = lambda nm: K(hs, nm)
        Q.op("dve", lambda e: e.tensor_scalar(out=w["Gb"], in0=onesf, scalar1=sv("g"), scalar2=None, op0=ALU.mult),
             reads=["onesf", "g"], writes=[k_("Gb")])
        if n >= 16:
            Q.op("pe", lambda e: e.matmul(w["pA"], lhsT=w["Gb"], rhs=Tri, start=True, stop=False), reads=[k_("Gb"), "Tri"], writes=[k_("pA")])
            Q.op("pe", lambda e: e.matmul(w["pA"], lhsT=identf, rhs=maskU, start=False, stop=True), reads=["identf", "maskU"], writes=[k_("pA")])
        Q.op("pe", lambda e: e.matmul(w["pB"], lhsT=w["Gb"], rhs=TriN, start=True, stop=False), reads=[k_("Gb"), "TriN"], writes=[k_("pB")])
        Q.op("pe", lambda e: e.matmul(w["pB"], lhsT=identf, rhs=maskL, start=False, stop=True), reads=["identf", "maskL"], writes=[k_("pB")])
        if n >= 16:
            Q.op("act", lambda e: e.activation(out=w["ET"], in_=w["pA"], func=AF.Exp, bias=sv("ngc")), reads=[k_("pA"), "ngc"], writes=[k_("ET")])
        Q.op("act", lambda e: e.activation(out=w["E"], in_=w["pB"], func=AF.Exp, bias=sv("gc")), reads=[k_("pB"), "gc"], writes=[k_("E")])
        Q.op("pe", lambda e: e.matmul(w["pA"], lhsT=kT, rhs=kT, start=True, stop=True), reads=[kk], writes=[k_("pA")])
        Q.op("dve", lambda e: e.scalar_tensor_tensor(out=w["X"][0], in0=w["pA"], scalar=sv("nbeta"), in1=w["E"], op0=ALU.mult, op1=ALU.mult),
             reads=[k_("pA"), "nbeta", k_("E")], writes=[k_("X0")])
        if n >= 16:
            Q.op("pe", lambda e: e.matmul(w["pB"], lhsT=kT, rhs=qT, start=True, stop=True), reads=[kk, kq], writes=[k_("pB")])
            Q.op("dve", lambda e: e.tensor_tensor(out=w[f"attnT{pb}"], in0=w["pB"], in1=w["ET"], op=ALU.mult),
                 reads=[k_("pB"), k_("ET")], writes=[k_(f"attnT{pb}")])
        Q.op("pe", lambda e: e.transpose(out=w["pT"][:, 0, :], in_=vT, identity=ident), reads=[kv, "ident"], writes=[k_("pT")])
        Q.op("pe", lambda e: e.transpose(out=w["pT"][:, 1, :], in_=kT, identity=ident), reads=[kk, "ident"], writes=[k_("pT")])
        Y = w[f"Y{pb}"]
        Q.op("act", lambda e: e.activation(out=Y[:, 0:128], in_=w["pT"][:, 0, :], func=AF.Copy, scale=sv("beta")),
             reads=[k_("pT"), "beta"], writes=[k_(f"Y{pb}")])
        Q.op("act", lambda e: e.activation(out=Y[:, 128:256], in_=w["pT"][:, 1, :], func=AF.Copy, scale=sv("be1")),
             reads=[k_("pT"), "be1"], writes=[k_(f"Y{pb}")])
        Q.op("act", lambda e: e.activation(out=w[f"kout{pb}"], in_=w["pT"][:, 1, :], func=AF.Copy, scale=sv("e2")),
             reads=[k_("pT"), "e2"], writes=[k_(f"kout{pb}")])
        Q.op("act", lambda e: e.copy(out=w["Yb"], in_=Y), reads=[k_(f"Y{pb}")], writes=[k_("Yb")])
        Q.op("pe", lambda e: e.transpose(out=w["pT"][:, 0, :], in_=w["X"][0], identity=ident), reads=[k_("X0"), "ident"], writes=[k_("pT")])
        Q.op("act", lambda e: e.copy(out=w["XT"][0], in_=w["pT"][:, 0, :]), reads=[k_("pT")], writes=[k_("XT0")])
        Am, Bm, Pm = w["X"][1], w["XT"][1], w["Pm"]
        Q.op("pool", lambda e: e.tensor_tensor(out=Am, in0=w["X"][0], in1=gmk[:, 7, :], op=ALU.mult), reads=[k_("X0"), "gmk"], writes=[k_("Am")])
        Q.op("pool", lambda e: e.tensor_tensor(out=Am, in0=Am, in1=ident, op=ALU.add), reads=[k_("Am"), "ident"], writes=[k_("Am")])
        Q.op("pool", lambda e: e.tensor_tensor(out=Bm, in0=w["XT"][0], in1=gmk[:, 0, :], op=ALU.mult), reads=[k_("XT0"), "gmk"], writes=[k_("Bm")])
        Q.op("pool", lambda e: e.tensor_tensor(out=Bm, in0=Bm, in1=ident, op=ALU.add), reads=[k_("Bm"), "ident"], writes=[k_("Bm")])
        for lvl in range(1, 7):
            Q.op("pool", lambda e, lvl=lvl: e.tensor_tensor(out=w["NkTs"][lvl - 1], in0=w["XT"][0], in1=gmk[:, lvl, :], op=ALU.mult),
                 reads=[k_("XT0"), "gmk"], writes=[k_(f"NkT{lvl}")])
        for lvl in range(1, 7):
            NkT_l = w["NkTs"][lvl - 1]
            Q.op("pe", lambda e, NkT_l=NkT_l: e.matmul(w["pA"], lhsT=NkT_l, rhs=Am, start=True, stop=True), reads=[k_(f"NkT{lvl}"), k_("Am")], writes=[k_("pA")])
            if lvl % 2 == 0:
                Q.op("act", lambda e: e.copy(out=Pm, in_=w["pA"]), reads=[k_("pA")], writes=[k_("Pm")])
            else:
                Q.op("dve", lambda e: e.tensor_copy(out=Pm, in_=w["pA"]), reads=[k_("pA")], writes=[k_("Pm")])
            Q.op("pe", lambda e: e.matmul(w["pB"], lhsT=Bm, rhs=Pm, start=True, stop=False), reads=[k_("Bm"), k_("Pm")], writes=[k_("pB")])
            Q.op("pe", lambda e: e.matmul(w["pB"], lhsT=ident, rhs=Am, start=False, stop=True), reads=["ident", k_("Am")], writes=[k_("pB")])
            Q.op("pe", lambda e: e.matmul(w["pC"][:, 0:128], lhsT=Pm, rhs=Bm, start=True, stop=False), reads=[k_("Bm"), k_("Pm")], writes=[k_("pC")])
            Q.op("pe", lambda e: e.matmul(w["pC"][:, 0:128], lhsT=ident, rhs=Bm, start=False, stop=True), reads=["ident", k_("Bm")], writes=[k_("pC")])
            Q.op("act", lambda e: e.copy(out=Am, in_=w["pB"]), reads=[k_("pB")], writes=[k_("Am")])
            Q.op("dve", lambda e: e.tensor_copy(out=Bm, in_=w["pC"][:, 0:128]), reads=[k_("pC")], writes=[k_("Bm")])
        Q.op("pe", lambda e: e.matmul(w["pC"], lhsT=Bm, rhs=w["Yb"], start=True, stop=True), reads=[k_("Bm"), k_("Yb")], writes=[k_("pC")])
        Q.op("dve", lambda e: e.tensor_copy(out=Y, in_=w["pC"]), reads=[k_("pC")], writes=[k_(f"Y{pb}")])
        Q.op("act", lambda e: e.copy(out=w["Yb"], in_=Y), reads=[k_(f"Y{pb}")], writes=[k_("Yb")])
        Q.op("pe", lambda e: e.transpose(out=w["pT"][:, 1, :], in_=w["Yb"][:, 128:256], identity=ident), reads=[k_("Yb"), "ident"], writes=[k_("pT")])
        Q.op("act", lambda e: e.copy(out=w[f"wT{pb}"], in_=w["pT"][:, 1, :]), reads=[k_("pT")], writes=[k_(f"wT{pb}")])

    def seq(grp, hs, n, Q):
        h = grp * 2 + hs
        w = W[hs]
        pb = n % 2
        col = n * 16 + h
        cs = slice(n * 128, (n + 1) * 128)
        qT = qkv[hs][0][:, cs]
        kq = K(hs, "qkv0")
        sv = lambda nm: sc[nm][:, col:col + 1]
        k_ = lambda nm: K(hs, nm)
        Y = w[f"Y{pb}"]
        Q.op("pe", lambda e: e.matmul(w["pA"], lhsT=w[f"wT{pb}"], rhs=w["Sb"], start=True, stop=True),
             reads=[k_(f"wT{pb}"), k_("Sb")], writes=[k_("pA")])
        Q.op("dve", lambda e: e.tensor_tensor(out=w["vnew"], in0=Y[:, 0:128], in1=w["pA"], op=ALU.subtract),
             reads=[k_("pA"), k_(f"Y{pb}")], writes=[k_("vnew")])
        if n >= 16:
            Q.op("pe", lambda e: e.matmul(w["pB"], lhsT=qT, rhs=w["Sb"], start=True, stop=True), reads=[kq, k_("Sb")], writes=[k_("pB")])
            Q.op("act", lambda e: e.activation(out=w["oS"], in_=w["pB"], func=AF.Copy, scale=sv("e1")), reads=[k_("pB"), "e1"], writes=[k_("oS")])
            Q.op("pe", lambda e: e.matmul(w["pC"][:, 0:128], lhsT=w[f"attnT{pb}"], rhs=w["vnew"], start=True, stop=True),
                 reads=[k_(f"attnT{pb}"), k_("vnew")], writes=[k_("pC")])
            Q.op("dve", lambda e: e.tensor_tensor(out=w["o"], in0=w["pC"][:, 0:128], in1=w["oS"], op=ALU.add),
                 reads=[k_("pC"), k_("oS")], writes=[k_("o")])
        Q.op("pe", lambda e: e.matmul(w["pC"][:, 128:256], lhsT=w[f"kout{pb}"], rhs=w["vnew"], start=True, stop=True),
             reads=[k_(f"kout{pb}"), k_("vnew")], writes=[k_("pC")])
        Q.op("dve", lambda e: e.scalar_tensor_tensor(out=w["S"], in0=w["S"], scalar=sv("dch"), in1=w["pC"][:, 128:256], op0=ALU.mult, op1=ALU.add),
             reads=[k_("pC"), "dch", k_("S")], writes=[k_("S")])
        Q.op("act", lambda e: e.copy(out=w["Sb"], in_=w["S"]), reads=[k_("S")], writes=[k_("Sb")])
        if n >= 16:
            Q.op("act", lambda e: e.activation(out=w["junk"], in_=w["o"], func=AF.Square, accum_out=w["ss"]), reads=[k_("o")], writes=[k_("ss"), k_("junk")])
            rsqrt_col(Q, w["ss"], w["ss"], EPS, k_("ss"), k_("ss"), mul=1.0 / 128)
            Q.op("dve", lambda e: e.tensor_scalar(out=w["on"], in0=w["o"], scalar1=w["ss"], scalar2=None, op0=ALU.mult),
                 reads=[k_("o"), k_("ss")], writes=[k_("on")])
            Q.op("pe", lambda e: e.transpose(out=w["pT"][:, 0, :], in_=w["on"], identity=ident), reads=[k_("on"), "ident"], writes=[k_("pT")])
            oc = slice((n - 16) * 128, (n - 15) * 128)
            Q.op("act", lambda e: e.copy(out=w["junk"], in_=w["pT"][:, 0, :]), reads=[k_("pT")], writes=[k_("junk")])
            Q.op("dve", lambda e: e.scalar_tensor_tensor(out=OGb[hs][:, oc], in0=w["junk"], scalar=normw, in1=zs[hs][:, oc],
                                                         op0=ALU.mult, op1=ALU.mult),
                 reads=[k_("junk"), "normw", k_("zs")], writes=[k_("OGb")])

    class Rec:
        def __init__(self):
            self.items = []

        def op(self, *a, **kw):
            self.items.append((a, kw))

    def interleave(fn, grp, n):
        recs = []
        for hs in range(2):
            r = Rec()
            fn(grp, hs, n, r)
            recs.append(r.items)
        for k in range(max(len(r) for r in recs)):
            for r in recs:
                if k < len(r):
                    a, kw = r[k]
                    P.op(*a, **kw)

    for grp in groups:
        preprocess(grp)
        interleave(pre, grp, 0)
        for n in range(NT):
            if n + 1 < NT:
                interleave(pre, grp, n + 1)
            interleave(seq, grp, n)
        for hs in range(2):
            h = grp * 2 + hs
            P.dma(OG_d[h * 128:(h + 1) * 128, :], OGb[hs], reads=[K(hs, "OGb")])
    st.close()


def nsa_host_consts(half):
    import ml_dtypes
    bf = ml_dtypes.bfloat16
    c = {}
    n = np.arange(256)
    q = np.arange(2048)
    cm = np.where((16 * n[:, None] + 31) <= (OWN0 + q[None, :]), 0.0, NEG).astype(np.float32)
    c["cmask"] = np.ascontiguousarray(cm.reshape(2, 128, 2048).transpose(1, 0, 2)).astype(bf)
    nvalid = (n <= 254) & ((16 * n >= OWN0) if half == 0 else True)
    c["cbias"] = np.ascontiguousarray(np.where(nvalid, 0.0, NEG).astype(np.float32).reshape(2, 128).T)
    pos = np.arange(TL)
    kvalid = (pos >= OWN0) if half == 0 else np.ones(TL, bool)
    c["kbias"] = np.ascontiguousarray(np.where(kvalid, 0.0, NEG).astype(np.float32).reshape(32, 128).T)
    key = np.arange(128)
    dk = np.arange(4)
    qq = np.arange(512)
    rel = dk[None, :, None] * 128 + key[:, None, None]
    c["causm"] = np.where(rel <= qq[None, None, :], 0.0, NEG).astype(bf)
    c["winm"] = np.where(rel > qq[None, None, :], 0.0, NEG).astype(bf)
    j = np.arange(64)
    kt = np.arange(32)
    c["esel"] = (j[:, None, None] == (2 * kt[None, :, None] + key[None, None, :] // 64)).astype(bf)
    cmp_start = np.arange(255) * 16
    sel_start = np.arange(64) * 64
    ov = np.minimum(cmp_start[:, None] + 32, sel_start[None, :] + 64) - np.maximum(cmp_start[:, None], sel_start[None, :])
    sm = np.zeros((256, 65), np.float32)
    sm[:255, :64] = np.clip(ov, 0, None) / 16.0
    sm[:, 64] = 1.0
    c["selmap"] = np.ascontiguousarray(sm.reshape(2, 128, 65).transpose(1, 0, 2)).astype(bf)
    t = OWN0 + q
    cur = t // 64
    blk0 = 32 if half == 0 else 0
    forced = (j[None, :] == blk0) | (j[None, :] == cur[:, None]) | (j[None, :] == cur[:, None] - 1)
    causal = (j[None, :] * 64 <= t[:, None]) & (j[None, :] >= blk0)
    use_imp = causal & ~forced
    selm = use_imp.astype(np.float32)
    sela = np.where(forced, 1e6, np.where(causal, 0.0, -1e30)).astype(np.float32)
    c["selm"] = np.ascontiguousarray(selm.reshape(16, 128, 64).transpose(1, 0, 2))
    c["sela"] = np.ascontiguousarray(sela.reshape(16, 128, 64).transpose(1, 0, 2))
    r = np.arange(48)
    c["selrow"] = np.ascontiguousarray(np.broadcast_to((r[:, None, None] == r[None, :, None]), (48, 48, 128))).astype(bf)
    return c


NSA_CONST_SPECS = [("cmask", [128, 2, 2048], BF16), ("cbias", [128, 2], F32), ("kbias", [128, 32], F32),
                   ("causm", [128, 4, 512], BF16), ("winm", [128, 4, 512], BF16), ("esel", [64, 32, 128], BF16),
                   ("selmap", [128, 2, 65], BF16), ("selm", [128, 16, 64], F32), ("sela", [128, 16, 64], F32),
                   ("selrow", [48, 48, 128], BF16)]


def stage_nsa(nc, P, cst, PT_d, KD, w1k_d, w2k_d, pek_d, w1v_d, w2v_d, pev_d, ON_d, ngroups=4, nqc=4):
    st = Stage(nc, P)
    ident, ones = cst["ident"], cst["ones"]
    SCALE = float(128 ** -0.5)
    TINY = 1e-30
    C = {}
    for nm, shp, dt in NSA_CONST_SPECS:
        C[nm] = st.sb("c_" + nm, shp, dt)
        P.dma(C[nm], KD[nm], writes=["c_" + nm])
    gst = st.sb("gst", [48, 2048], F32)
    gsig = st.sb("gsig", [48, 2048], BF16)
    P.dma(gst, PT_d[C_NG:C_NG + 48, OWN0:TL], writes=["gst"])
    P.op("act", lambda e: e.activation(out=gsig, in_=gst, func=AF.Sigmoid), reads=["gst"], writes=["gsig"])
    pS = [st.ps(f"pS{i}", [128, 512], F32) for i in range(2)]
    pO = st.ps("pO", [128, 512], F32)
    pD = st.ps("pD", [128, 512], F32)
    pG = st.ps("pG", [128, 512], F32)
    pI = st.ps("pI", [128, 4, 128], F32)
    pTm = st.ps("pTm", [128, 4, 128], BF16)
    pO2 = st.ps("pO2", [128, 512], F32)
    pD2 = pI.rearrange("p a b -> p (a b)")
    ACC = [(pO, "pO", pD, "pD"), (pO2, "pO2", pD2, "pI")]
    cw_ = {}
    for tag, w1d, w2d, ped in (("k", w1k_d, w2k_d, pek_d), ("v", w1v_d, w2v_d, pev_d)):
        w1 = st.sb("w1" + tag, [128, 32, 128], BF16)
        w2 = st.sb("w2" + tag, [128, 128], BF16)
        peT = st.sb("peT" + tag, [128, 32], BF16)
        hb = st.sb("hb" + tag, [128, 1], F32)
        P.dma(w1, w1d.rearrange("l d e -> d l e"), writes=["w1" + tag], q="pool")
        P.dma(w2, w2d, writes=["w2" + tag], q="pool")
        P.dma(peT, ped, writes=["peT" + tag], q="pool")
        for l in range(32):
            P.op("pe", lambda e, l=l, w1=w1, peT=peT: e.matmul(pS[0][:, 0:1], lhsT=w1[:, l, :], rhs=peT[:, l:l + 1],
                                                              start=(l == 0), stop=(l == 31)),
                 reads=["w1" + tag, "peT" + tag], writes=["pS0"])
        P.op("act", lambda e, hb=hb: e.copy(out=hb, in_=pS[0][:, 0:1]), reads=["pS0"], writes=["hb" + tag])
        cw_[tag] = (w1, w2, hb)
    kcT = st.sb("kcT", [128, TL], BF16)
    vcT = st.sb("vcT", [128, TL], BF16)
    ksT = st.sb("ksT", [128, TL], BF16)
    kwT = st.sb("kwT", [128, TL], BF16)
    vtmp = st.sb("vtmp", [128, TL], BF16)
    vs_tok = st.sb("vs_tok", [128, 32, 128], BF16)
    vw_tok = st.sb("vw_tok", [128, 32, 128], BF16)
    qT = [st.sb(f"nq{i}", [128, 2048], BF16) for i in range(4)]
    hid = st.sb("hid", [128, 256], BF16)
    kcmpT = st.sb("kcmpT", [128, 256], BF16)
    vcmpT = st.sb("vcmpT", [128, 256], BF16)
    vcmp_tok = st.sb("vcmp_tok", [128, 2, 128], BF16)
    Pc = [[st.sb(f"Pc{h}_{j}", [128, 512], BF16) for j in range(2)] for h in range(4)]
    PTl = [st.sb(f"PTl{i}", [128, 512], BF16) for i in range(3)]
    accO = [st.sb(f"accO{h}", [128, 512], F32) for h in range(4)]
    accI = st.sb("accI", [128, 4, 64], F32)
    rr = st.sb("rr", [128, 512], F32)
    tmpo = st.sb("tmpo", [128, 512], F32)
    rc = st.sb("rc", [128, 1], F32)
    score = st.sb("score", [128, 64], F32)
    work = st.sb("work", [128, 64], F32)
    mx8 = st.sb("mx8", [128, 8], F32)
    madd = st.sb("madd", [128, 64], BF16)
    maddT = st.sb("maddT", [64, 512], BF16)
    osb = st.sb("osb", [128, 512], BF16)
    P.op("pool", lambda e: e.memset(hid, 0.0), writes=["hid"])

    def combine(h, first, aset=0):
        pO_, kO, pD_, kD = ACC[aset]
        P.op("dve", lambda e: e.tensor_scalar(out=rr, in0=pD_, scalar1=TINY, scalar2=None, op0=ALU.max), reads=[kD], writes=["rr"])
        P.op("dve", lambda e: e.reciprocal(out=rr, in_=rr), reads=["rr"], writes=["rr"])
        P.op("dve", lambda e: e.tensor_tensor(out=rr, in0=rr, in1=pG, op=ALU.mult), reads=["rr", "pG"], writes=["rr"])
        if first:
            P.op("dve", lambda e: e.tensor_tensor(out=accO[h], in0=pO_, in1=rr, op=ALU.mult), reads=[kO, "rr"], writes=[f"accO{h}"])
        else:
            P.op("dve", lambda e: e.tensor_tensor(out=tmpo, in0=pO_, in1=rr, op=ALU.mult), reads=[kO, "rr"], writes=["tmpo"])
            P.op("dve", lambda e: e.tensor_tensor(out=accO[h], in0=accO[h], in1=tmpo, op=ALU.add), reads=["tmpo", f"accO{h}"], writes=[f"accO{h}"])

    def compress(tag, srcT, srckey, dstT, dstkey):
        w1, w2, hb = cw_[tag]
        for l in range(32):
            v3 = srcT.rearrange("p (n s) -> p n s", s=16)
            rhs_ = v3[:, 0:255, l] if l < 16 else v3[:, 1:256, l - 16]
            P.op("pe", lambda e, l=l, rhs_=rhs_: e.matmul(pS[0][:, 0:255], lhsT=w1[:, l, :], rhs=rhs_,
                                               start=(l == 0), stop=(l == 31)),
                 reads=["w1" + tag, srckey], writes=["pS0"])
        P.op("act", lambda e: e.activation(out=hid[:, 0:255], in_=pS[0][:, 0:255], func=AF.Silu, bias=hb), reads=["pS0", "hb" + tag], writes=["hid"])
        P.op("pe", lambda e: e.matmul(pS[1][:, 0:256], lhsT=w2, rhs=hid, start=True, stop=True), reads=["w2" + tag, "hid"], writes=["pS1"])
        P.op("act", lambda e: e.copy(out=dstT, in_=pS[1][:, 0:256]), reads=["pS1"], writes=[dstkey])

    def to_tok(dst, dstkey, ntile, src, srckey):
        for g4 in range(0, ntile, 4):
            nn = min(4, ntile - g4)
            for j in range(nn):
                P.op("pe", lambda e, j=j, g4=g4: e.transpose(out=pTm[:, j, :], in_=src[:, (g4 + j) * 128:(g4 + j + 1) * 128], identity=ident),
                     reads=[srckey, "ident"], writes=["pTm"])
            P.op("act", lambda e, g4=g4, nn=nn: e.copy(out=dst[:, g4:g4 + nn, :], in_=pTm[:, 0:nn, :]), reads=["pTm"], writes=[dstkey])

    pcount = [0]
    scount = [0]

    def attend(h, qs, kT, kTkey, vtok, vkey, kts, maskfn, bias_fn, aset=0):
        n = len(kts)
        prev = None
        pO_, kO, pD_, kD = ACC[aset]

        def pv(pt, pi, kt, i):
            P.op("pe", lambda e: e.matmul(pO_, lhsT=vtok[:, kt, :], rhs=pt, start=(i == 0), stop=(i == n - 1)),
                 reads=[vkey, f"PTl{pi}"], writes=[kO])
            P.op("pe", lambda e: e.matmul(pD_, lhsT=ones, rhs=pt, start=(i == 0), stop=(i == n - 1)),
                 reads=["ones", f"PTl{pi}"], writes=[kD])

        for i, kt in enumerate(kts):
            sb_ = scount[0] % 2
            scount[0] += 1
            ps = pS[sb_]
            extra = maskfn(kt)
            P.op("pe", lambda e, qs=qs, kt=kt, ps=ps, extra=extra: e.matmul(ps, lhsT=kT[:, kt * 128:(kt + 1) * 128], rhs=qT[h][:, qs],
                                                                    start=True, stop=(len(extra) == 0)),
                 reads=[kTkey, f"nq{h}"], writes=[f"pS{sb_}"])
            for mi, (l_, r_, keys_) in enumerate(extra):
                P.op("pe", lambda e, ps=ps, l_=l_, r_=r_, last=(mi == len(extra) - 1): e.matmul(ps, lhsT=l_, rhs=r_, start=False, stop=last),
                     reads=keys_, writes=[f"pS{sb_}"])
            pi = pcount[0] % 3
            pcount[0] += 1
            pt = PTl[pi]
            P.op("act", lambda e, ps=ps, pt=pt, kt=kt: e.activation(out=pt, in_=ps, func=AF.Exp, scale=SCALE, bias=bias_fn(kt)),
                 reads=[f"pS{sb_}", "c_kbias", "c_cbias"], writes=[f"PTl{pi}"])
            if prev is not None:
                pv(*prev)
            prev = (pt, pi, kt, i)
        pv(*prev)

    for g in range(ngroups):
        rows = lambda jj: slice(C_NKV + jj * 512 + g * 128, C_NKV + jj * 512 + (g + 1) * 128)
        P.dma(kcT, PT_d[rows(0), 0:TL], writes=["kcT"], q="pool")
        P.dma(vcT, PT_d[rows(1), 0:TL], writes=["vcT"], q="pool")
        P.dma(ksT, PT_d[rows(2), 0:TL], writes=["ksT"], q="pool")
        P.dma(kwT, PT_d[rows(4), 0:TL], writes=["kwT"], q="pool")
        P.dma(vtmp, PT_d[rows(3), 0:TL], writes=["vtmp"], q="pool")
        to_tok(vs_tok, "vs_tok", 32, vtmp, "vtmp")
        P.dma(vtmp, PT_d[rows(5), 0:TL], writes=["vtmp"], q="pool")
        to_tok(vw_tok, "vw_tok", 32, vtmp, "vtmp")
        for hg in range(4):
            hq = g * 4 + hg
            P.dma(qT[hg], PT_d[C_NQ + hq * 128:C_NQ + (hq + 1) * 128, OWN0:TL], writes=[f"nq{hg}"], q="pool")
        compress("k", kcT, "kcT", kcmpT, "kcmpT")
        compress("v", vcT, "vcT", vcmpT, "vcmpT")
        to_tok(vcmp_tok, "vcmp_tok", 2, vcmpT, "vcmpT")
        for qc in range(nqc):
            qs = slice(qc * 512, (qc + 1) * 512)
            for hg in range(4):
                hq = g * 4 + hg
                for j in range(2):
                    sb_ = scount[0] % 2
                    scount[0] += 1
                    ps = pS[sb_]
                    P.op("pe", lambda e, qs=qs, j=j, ps=ps, hg=hg: e.matmul(ps, lhsT=kcmpT[:, j * 128:(j + 1) * 128], rhs=qT[hg][:, qs], start=True, stop=False),
                         reads=["kcmpT", f"nq{hg}"], writes=[f"pS{sb_}"])
                    P.op("pe", lambda e, qs=qs, j=j, ps=ps: e.matmul(ps, lhsT=ident, rhs=C["cmask"][:, j, qs], start=False, stop=True),
                         reads=["ident", "c_cmask"], writes=[f"pS{sb_}"])
                    P.op("act", lambda e, j=j, ps=ps, hg=hg: e.activation(out=Pc[hg][j], in_=ps, func=AF.Exp, scale=SCALE, bias=C["cbias"][:, j:j + 1]),
                         reads=[f"pS{sb_}", "c_cbias"], writes=[f"Pc{hg}_{j}"])
                for j in range(2):
                    P.op("pe", lambda e, j=j, hg=hg: e.matmul(pO, lhsT=vcmp_tok[:, j, :], rhs=Pc[hg][j], start=(j == 0), stop=(j == 1)),
                         reads=["vcmp_tok", f"Pc{hg}_{j}"], writes=["pO"])
                for j in range(2):
                    P.op("pe", lambda e, j=j, hg=hg: e.matmul(pD, lhsT=ones, rhs=Pc[hg][j], start=(j == 0), stop=(j == 1)),
                         reads=["ones", f"Pc{hg}_{j}"], writes=["pD"])
                P.op("pe", lambda e, qs=qs, hq=hq: e.matmul(pG, lhsT=C["selrow"][:, hq * 3 + 0, :], rhs=gsig[:, qs], start=True, stop=True),
                     reads=["c_selrow", "gsig"], writes=["pG"])
                combine(hg, True)
                for sub in range(4):
                    for j in range(2):
                        P.op("pe", lambda e, j=j, hg=hg, sub=sub: e.matmul(pI[:, sub, 0:65], lhsT=Pc[hg][j][:, sub * 128:(sub + 1) * 128],
                                                                         rhs=C["selmap"][:, j, :], start=(j == 0), stop=(j == 1)),
                             reads=["c_selmap", f"Pc{hg}_{j}"], writes=["pI"])
                    P.op("dve", lambda e, sub=sub: e.tensor_scalar(out=rc, in0=pI[:, sub, 64:65], scalar1=TINY, scalar2=None, op0=ALU.max),
                         reads=["pI"], writes=["rc"])
                    P.op("dve", lambda e: e.reciprocal(out=rc, in_=rc), reads=["rc"], writes=["rc"])
                    if hg == 0:
                        P.op("dve", lambda e, sub=sub: e.tensor_scalar(out=accI[:, sub, :], in0=pI[:, sub, 0:64], scalar1=rc, scalar2=None, op0=ALU.mult),
                             reads=["pI", "rc"], writes=["accI"])
                    else:
                        P.op("dve", lambda e, sub=sub: e.scalar_tensor_tensor(out=accI[:, sub, :], in0=pI[:, sub, 0:64], scalar=rc, in1=accI[:, sub, :],
                                                                             op0=ALU.mult, op1=ALU.add),
                             reads=["pI", "rc", "accI"], writes=["accI"])
            for sub in range(4):
                ts_ = qc * 4 + sub
                P.op("dve", lambda e, sub=sub, ts_=ts_: e.tensor_tensor(out=score, in0=accI[:, sub, :], in1=C["selm"][:, ts_, :], op=ALU.mult),
                     reads=["accI", "c_selm"], writes=["score"])
                P.op("dve", lambda e, ts_=ts_: e.tensor_tensor(out=score, in0=score, in1=C["sela"][:, ts_, :], op=ALU.add),
                     reads=["score", "c_sela"], writes=["score"])
                P.op("dve", lambda e: e.max(out=mx8, in_=score), reads=["score"], writes=["mx8"])
                P.op("dve", lambda e: e.match_replace(out=work, in_to_replace=mx8, in_values=score, imm_value=-3.0e38),
                     reads=["score", "mx8"], writes=["work"])
                P.op("dve", lambda e: e.max(out=mx8, in_=work), reads=["work"], writes=["mx8"])
                P.op("dve", lambda e: e.tensor_scalar(out=work, in0=score, scalar1=mx8[:, 7:8], scalar2=None, op0=ALU.is_ge),
                     reads=["score", "mx8"], writes=["work"])
                P.op("dve", lambda e: e.tensor_scalar(out=madd, in0=work, scalar1=1.0, scalar2=-NEG, op0=ALU.subtract, op1=ALU.mult),
                     reads=["work"], writes=["madd"])
                P.op("pe", lambda e: e.transpose(out=pTm[0:64, 0, :], in_=madd, identity=ident), reads=["madd", "ident"], writes=["pTm"])
                P.op("act", lambda e, sub=sub: e.copy(out=maddT[:, sub * 128:(sub + 1) * 128], in_=pTm[0:64, 0, :]), reads=["pTm"], writes=["maddT"])
            kt_diag0 = 16 + 4 * qc
            for hg in range(4):
                hq = g * 4 + hg

                def sel_mask(kt):
                    ex = [(C["esel"][:, kt, :], maddT, ["c_esel", "maddT"])]
                    if kt >= kt_diag0:
                        ex.append((ident, C["causm"][:, kt - kt_diag0, :], ["ident", "c_causm"]))
                    return ex

                def win_mask(kt):
                    if kt >= kt_diag0:
                        return [(ident, C["causm"][:, kt - kt_diag0, :], ["ident", "c_causm"])]
                    return [(ident, C["winm"][:, kt - (kt_diag0 - 4), :], ["ident", "c_winm"])]

                kb = lambda kt: C["kbias"][:, kt:kt + 1]
                attend(hg, qs, ksT, "ksT", vs_tok, "vs_tok", list(range(0, kt_diag0 + 4)), sel_mask, kb, aset=1)
                P.op("pe", lambda e, qs=qs, hq=hq: e.matmul(pG, lhsT=C["selrow"][:, hq * 3 + 1, :], rhs=gsig[:, qs], start=True, stop=True),
                     reads=["c_selrow", "gsig"], writes=["pG"])
                combine(hg, False, aset=1)
                attend(hg, qs, kwT, "kwT", vw_tok, "vw_tok", list(range(kt_diag0 - 4, kt_diag0 + 4)), win_mask, kb, aset=0)
                P.op("pe", lambda e, qs=qs, hq=hq: e.matmul(pG, lhsT=C["selrow"][:, hq * 3 + 2, :], rhs=gsig[:, qs], start=True, stop=True),
                     reads=["c_selrow", "gsig"], writes=["pG"])
                combine(hg, False)
                P.op("act", lambda e, hg=hg: e.copy(out=osb, in_=accO[hg]), reads=[f"accO{hg}"], writes=["osb"])
                P.dma(ON_d[hq * 128:(hq + 1) * 128, qs], osb, reads=["osb"])
    st.close()


def stage_merge(nc, P, cst, PT_d, OG_d, ON_d, wa_d, wb_d, wo_d, Hd, nblk=4):
    st = Stage(nc, P)
    ogT = st.sb("ogT", [128, 16, 512], BF16)
    onT = st.sb("onT", [128, 16, 512], BF16)
    mT = st.sb("mT", [128, 32, 512], BF16)
    wa = [st.sb(f"wa{i}", [128, 16, 256], BF16) for i in range(2)]
    wb = [st.sb(f"wb{i}", [128, 16, 256], BF16) for i in range(2)]
    ga = [st.sb(f"ga{i}", [128, 512], F32) for i in range(2)]
    gb = [st.sb(f"gb{i}", [128, 512], F32) for i in range(2)]
    m1 = st.sb("m1", [128, 512], F32)
    m2 = st.sb("m2", [128, 512], F32)
    wo = [st.sb(f"wo{i}", [128, 32, 256], BF16) for i in range(2)]
    hs = [st.sb(f"hs{i}", [128, 256], F32) for i in range(2)]
    pA = [st.ps(f"mpA{i}", [128, 512], F32) for i in range(2)]
    pB = [st.ps(f"mpB{i}", [128, 512], F32) for i in range(2)]
    pH = [st.ps(f"mpH{i}", [128, 512], F32) for i in range(2)]
    wc = 0
    gc_ = 0
    woc = 0
    hc = 0
    for tb in range(nblk):
        ts_ = slice(tb * 512, (tb + 1) * 512)
        P.dma(ogT, OG_d[:, ts_].rearrange("(c p) t -> p c t", p=128), writes=["ogT"])
        P.dma(onT, ON_d[:, ts_].rearrange("(c p) t -> p c t", p=128), writes=["onT"])
        for c2 in range(16):
            j = wc % 2
            wc += 1
            P.dma(wa[j], wa_d[:, c2 * 256:(c2 + 1) * 256].rearrange("(k p) n -> p k n", p=128), writes=[f"wa{j}"], q="pool")
            P.dma(wb[j], wb_d[:, c2 * 256:(c2 + 1) * 256].rearrange("(k p) n -> p k n", p=128), writes=[f"wb{j}"], q="pool")
            for sub in range(2):
                cc = c2 * 2 + sub
                i = gc_ % 2
                gc_ += 1
                P.dma(ga[i], PT_d[C_MG + cc * 128:C_MG + (cc + 1) * 128, OWN0 + tb * 512:OWN0 + (tb + 1) * 512], writes=[f"ga{i}"])
                P.dma(gb[i], PT_d[C_MG + 4096 + cc * 128:C_MG + 4096 + (cc + 1) * 128, OWN0 + tb * 512:OWN0 + (tb + 1) * 512], writes=[f"gb{i}"])
                P.op("act", lambda e, i=i: e.activation(out=ga[i], in_=ga[i], func=AF.Sigmoid), reads=[f"ga{i}"], writes=[f"ga{i}"])
                P.op("act", lambda e, i=i: e.activation(out=gb[i], in_=gb[i], func=AF.Sigmoid), reads=[f"gb{i}"], writes=[f"gb{i}"])
                for k in range(16):
                    P.op("pe", lambda e, k=k, i=i, j=j, sub=sub: e.matmul(pA[i], lhsT=wa[j][:, k, sub * 128:(sub + 1) * 128], rhs=ogT[:, k, :],
                                                                        start=(k == 0), stop=(k == 15)),
                         reads=[f"wa{j}", "ogT"], writes=[f"mpA{i}"])
                for k in range(16):
                    P.op("pe", lambda e, k=k, i=i, j=j, sub=sub: e.matmul(pB[i], lhsT=wb[j][:, k, sub * 128:(sub + 1) * 128], rhs=onT[:, k, :],
                                                                        start=(k == 0), stop=(k == 15)),
                         reads=[f"wb{j}", "onT"], writes=[f"mpB{i}"])
                P.op("dve", lambda e, i=i: e.tensor_tensor(out=m1, in0=pA[i], in1=ga[i], op=ALU.mult), reads=[f"mpA{i}", f"ga{i}"], writes=["m1"])
                P.op("dve", lambda e, i=i: e.tensor_tensor(out=m2, in0=pB[i], in1=gb[i], op=ALU.mult), reads=[f"mpB{i}", f"gb{i}"], writes=["m2"])
                P.op("dve", lambda e, cc=cc: e.tensor_tensor(out=mT[:, cc, :], in0=m1, in1=m2, op=ALU.add), reads=["m1", "m2"], writes=[f"mT{cc}"])
        for ct in range(16):
            j = woc % 2
            woc += 1
            P.dma(wo[j], wo_d[:, ct * 256:(ct + 1) * 256].rearrange("(k p) n -> p k n", p=128), writes=[f"wo{j}"], q="pool")
            for tt in range(4):
                i = hc % 2
                hc += 1
                for k in range(32):
                    P.op("pe", lambda e, k=k, i=i, j=j, tt=tt: e.matmul(pH[i][:, 0:256], lhsT=mT[:, k, tt * 128:(tt + 1) * 128], rhs=wo[j][:, k, :],
                                                                      start=(k == 0), stop=(k == 31)),
                         reads=[f"wo{j}", f"mT{k}"], writes=[f"mpH{i}"])
                evac(P, i, hs[i], pH[i][:, 0:256], [f"mpH{i}"], [f"hs{i}"])
                r0 = tb * 512 + tt * 128
                P.dma(Hd[r0:r0 + 128, ct * 256:(ct + 1) * 256], hs[i], reads=[f"hs{i}"])
    st.close()


CAP = 128


def stage_route(nc, P, cst, x_d, Hd, gffn_d, wr_d, br_d, eb_d, Xd, slots_i, wts):
    st = Stage(nc, P)
    identf, ones, onesf = cst["identf"], cst["ones"], cst["onesf"]
    xt = [st.sb(f"rx{i}", [128, D], F32) for i in range(2)]
    hd = [st.sb(f"rh{i}", [128, D], F32) for i in range(2)]
    hn2 = [st.sb(f"rhn{i}", [128, D], F32) for i in range(2)]
    hnb = [st.sb(f"rhnb{i}", [128, D], BF16) for i in range(2)]
    junk2 = [st.sb(f"rjunk{i}", [128, D], BF16) for i in range(2)]
    gf = st.sb("rgf", [128, D], F32)
    hnT2 = [st.sb(f"rhnT{i}", [128, 32, 128], F32) for i in range(2)]
    wr = st.sb("rwr", [128, 32, 72], F32)
    br = st.sb("rbr", [128, 72], F32)
    eb = st.sb("reb", [128, 64], F32)
    sut = st.sb("rsut", [128, 128], BF16)
    cnt = st.sb("rcnt", [128, 64], F32)
    zt = st.sb("rzt", [128, 2048], BF16)
    sm2 = [{}, {}]
    for i_ in range(2):
        for nm, w in (("ss", 1), ("lg", 72), ("mxg", 1), ("ohg", 8), ("eg", 8), ("sumg", 1), ("t3", 64), ("es", 8), ("mx8", 8),
                      ("oh1", 8), ("sel2", 8), ("oh2", 8), ("dd", 1), ("w1", 1), ("w2", 1), ("o1", 64), ("o2", 64), ("posf", 64),
                      ("tmp", 64), ("s1", 1), ("s2", 1)):
            sm2[i_][nm] = st.sb(f"r{i_}_" + nm, [128, w], F32)
    oh64_2 = [st.sb(f"r_oh64_{i_}", [128, 64], BF16) for i_ in range(2)]
    pT2 = [st.ps(f"rpT{i_}", [128, 4, 128], F32) for i_ in range(2)]
    pL2 = [st.ps(f"rpL{i_}", [128, 512], F32)[:, 0:72] for i_ in range(2)]
    pP2 = [st.ps(f"rpP{i_}", [128, 512], F32)[:, 0:64] for i_ in range(2)]
    pC2 = [st.ps(f"rpC{i_}", [128, 512], F32)[:, 0:64] for i_ in range(2)]

    P.dma(gf, gffn_d.partition_broadcast(128), writes=["rgf"])
    P.dma(wr, wr_d, writes=["rwr"])
    P.dma(br, br_d.partition_broadcast(128), writes=["rbr"])
    P.dma(eb, eb_d.partition_broadcast(128), writes=["reb"])
    P.op("pool", lambda e: e.memset(cnt, 0.0), writes=["rcnt"])
    P.op("pool", lambda e: e.affine_select(out=sut, in_=ones, pattern=[[1, 128]], compare_op=ALU.is_gt, fill=0.0,
                                           base=0, channel_multiplier=-1), reads=["ones"], writes=["rsut"])
    P.op("pool", lambda e: e.memset(zt, 0.0), writes=["rzt"])
    Xz = Xd.rearrange("(a p) (b n) -> a b p n", p=128, n=2048)
    zero_done = []
    for a in range(64):
        for b_ in range(2):
            P.dma(Xz[a, b_], zt, reads=["rzt"], writes=[f"Xz{a}_{b_}"])
            zero_done.append(f"Xz{a}_{b_}")

    def route_tile(tt, Q):
        i = tt % 2
        hn, junk, hnT, sm, oh64 = hn2[i], junk2[i], hnT2[i], sm2[i], oh64_2[i]
        pT, pL, pP, pC = pT2[i], pL2[i], pP2[i], pC2[i]

        def v(nm):
            return sm[nm]
        r0 = tt * 128
        Q.dma(xt[i], x_d[OWN0 + r0:OWN0 + r0 + 128, :], writes=[f"rx{i}"])
        Q.dma(hd[i], Hd[r0:r0 + 128, :], writes=[f"rh{i}"])
        Q.op("dve", lambda e, i=i: e.tensor_tensor(out=hd[i], in0=hd[i], in1=xt[i], op=ALU.add), reads=[f"rx{i}", f"rh{i}"], writes=[f"rh{i}"])
        Q.dma(Hd[r0:r0 + 128, :], hd[i], reads=[f"rh{i}"])
        Q.op("act", lambda e, i=i: e.activation(out=junk, in_=hd[i], func=AF.Square, accum_out=v("ss")), reads=[f"rh{i}"], writes=["rjunk", "r_ss"])
        rsqrt_col(Q, v("ss"), v("ss"), EPS, "r_ss", "r_ss", mul=1.0 / D)
        Q.op("dve", lambda e, i=i: e.scalar_tensor_tensor(out=hn, in0=hd[i], scalar=v("ss"), in1=gf, op0=ALU.mult, op1=ALU.mult),
             reads=[f"rh{i}", "r_ss", "rgf"], writes=["rhn"])
        Q.op("act", lambda e, i=i: e.copy(out=hnb[i], in_=hn), reads=["rhn"], writes=[f"rhnb{i}"])
        for g4 in range(8):
            for j in range(4):
                c = g4 * 4 + j
                Q.op("pe", lambda e, c=c, j=j: e.transpose(out=pT[:, j, :], in_=hn[:, c * 128:(c + 1) * 128], identity=identf),
                     reads=["rhn", "identf"], writes=["rpT"])
            Q.op("act", lambda e, g4=g4: e.copy(out=hnT[:, g4 * 4:(g4 + 1) * 4, :], in_=pT), reads=["rpT"], writes=[f"rhnT{g4}"])
        for c in range(32):
            Q.op("pe", lambda e, c=c: e.matmul(pL, lhsT=hnT[:, c, :], rhs=wr[:, c, :], start=(c == 0), stop=(c == 31)),
                 reads=[f"rhnT{c // 4}", "rwr"], writes=["rpL"])
        Q.op("dve", lambda e: e.tensor_tensor(out=v("lg"), in0=pL, in1=br, op=ALU.add), reads=["rpL", "rbr"], writes=["r_lg"])
        lgG = v("lg")[:, 0:8]
        le3 = v("lg")[:, 8:72].rearrange("p (g e) -> p g e", e=8)
        t3 = v("t3").rearrange("p (g e) -> p g e", e=8)
        o1 = v("o1").rearrange("p (g e) -> p g e", e=8)
        o2 = v("o2").rearrange("p (g e) -> p g e", e=8)
        D_ = "dve"
        Q.op(D_, lambda e: e.reduce_max(out=v("mxg"), in_=lgG, axis=AX.X), reads=["r_lg"], writes=["r_mxg"])
        Q.op(D_, lambda e: e.tensor_scalar(out=v("ohg"), in0=lgG, scalar1=v("mxg"), scalar2=None, op0=ALU.is_ge), reads=["r_lg", "r_mxg"], writes=["r_ohg"])
        Q.op(D_, lambda e: e.tensor_scalar(out=v("mxg"), in0=v("mxg"), scalar1=-1.0, scalar2=None, op0=ALU.mult), reads=["r_mxg", "r_ohg"], writes=["r_mxg"])
        Q.op("act", lambda e: e.activation(out=v("eg"), in_=lgG, func=AF.Exp, bias=v("mxg"), accum_out=v("sumg")), reads=["r_lg", "r_mxg"], writes=["r_eg", "r_sumg"])
        Q.op(D_, lambda e: e.reciprocal(out=v("sumg"), in_=v("sumg")), reads=["r_sumg"], writes=["r_sumg"])
        Q.op(D_, lambda e: e.tensor_tensor(out=t3, in0=le3, in1=v("ohg").unsqueeze(2).to_broadcast([128, 8, 8]), op=ALU.mult),
             reads=["r_lg", "r_ohg"], writes=["r_t3"])
        Q.op(D_, lambda e: e.tensor_reduce(out=v("es"), in_=t3.rearrange("p g e -> p e g"), axis=AX.X, op=ALU.add), reads=["r_t3"], writes=["r_es"])
        Q.op(D_, lambda e: e.max(out=v("mx8"), in_=v("es")), reads=["r_es"], writes=["r_mx8"])
        Q.op(D_, lambda e: e.tensor_scalar(out=v("oh1"), in0=v("es"), scalar1=v("mx8")[:, 0:1], scalar2=None, op0=ALU.is_ge), reads=["r_es", "r_mx8"], writes=["r_oh1"])
        Q.op(D_, lambda e: e.tensor_scalar(out=v("sel2"), in0=v("es"), scalar1=v("mx8")[:, 1:2], scalar2=None, op0=ALU.is_ge), reads=["r_es", "r_mx8"], writes=["r_sel2"])
        Q.op(D_, lambda e: e.tensor_tensor(out=v("oh2"), in0=v("sel2"), in1=v("oh1"), op=ALU.subtract), reads=["r_sel2", "r_oh1"], writes=["r_oh2"])
        Q.op(D_, lambda e: e.tensor_tensor(out=v("dd"), in0=v("mx8")[:, 1:2], in1=v("mx8")[:, 0:1], op=ALU.subtract), reads=["r_mx8"], writes=["r_dd"])
        Q.op("act", lambda e: e.activation(out=v("dd"), in_=v("dd"), func=AF.Exp), reads=["r_dd"], writes=["r_dd"])
        Q.op(D_, lambda e: e.tensor_scalar(out=v("w1"), in0=v("dd"), scalar1=1.0, scalar2=None, op0=ALU.add), reads=["r_dd"], writes=["r_w1"])
        Q.op(D_, lambda e: e.reciprocal(out=v("w1"), in_=v("w1")), reads=["r_w1"], writes=["r_w1"])
        Q.op(D_, lambda e: e.tensor_tensor(out=v("w2"), in0=v("dd"), in1=v("w1"), op=ALU.mult), reads=["r_dd", "r_w1"], writes=["r_w2"])
        Q.op(D_, lambda e, tt=tt: e.tensor_tensor(out=wts[:, tt, 0:1], in0=v("w1"), in1=v("sumg"), op=ALU.mult), reads=["r_w1", "r_sumg"], writes=[f"wts{tt}"])
        Q.op(D_, lambda e, tt=tt: e.tensor_tensor(out=wts[:, tt, 1:2], in0=v("w2"), in1=v("sumg"), op=ALU.mult), reads=["r_w2", "r_sumg"], writes=[f"wts{tt}"])
        for onm, src in (("o1", "oh1"), ("o2", "oh2")):
            o3 = o1 if onm == "o1" else o2
            Q.op(D_, lambda e, o3=o3: e.tensor_copy(out=o3, in_=v("ohg").unsqueeze(2).to_broadcast([128, 8, 8])), reads=["r_ohg"], writes=["r_" + onm])
            Q.op(D_, lambda e, o3=o3, src=src: e.tensor_tensor(out=o3, in0=o3, in1=v(src).unsqueeze(1).to_broadcast([128, 8, 8]), op=ALU.mult),
                 reads=["r_" + onm, "r_" + src], writes=["r_" + onm])
        Q.op(D_, lambda e: e.tensor_tensor(out=oh64, in0=v("o1"), in1=v("o2"), op=ALU.add), reads=["r_o1", "r_o2"], writes=["r_oh64"])
        Q.op("pe", lambda e: e.matmul(pP, lhsT=sut, rhs=oh64, start=True, stop=True), reads=["rsut", "r_oh64"], writes=["rpP"])
        Q.op("pe", lambda e: e.matmul(pC, lhsT=ones, rhs=oh64, start=True, stop=True), reads=["ones", "r_oh64"], writes=["rpC"])
        Q.op(D_, lambda e: e.tensor_tensor(out=v("posf"), in0=pP, in1=cnt, op=ALU.add), reads=["rpP", "rcnt"], writes=["r_posf"])
        Q.op(D_, lambda e: e.tensor_tensor(out=cnt, in0=cnt, in1=pC, op=ALU.add), reads=["rpC", "rcnt", "r_posf"], writes=["rcnt"])
        Q.op(D_, lambda e: e.tensor_scalar(out=v("posf"), in0=v("posf"), scalar1=float(CAP - 1), scalar2=None, op0=ALU.min), reads=["r_posf"], writes=["r_posf"])
        Q.op(D_, lambda e: e.tensor_tensor(out=v("posf"), in0=v("posf"), in1=eb, op=ALU.add), reads=["r_posf", "reb"], writes=["r_posf"])
        for k, (onm, snm) in enumerate((("o1", "s1"), ("o2", "s2"))):
            Q.op(D_, lambda e, onm=onm: e.tensor_tensor(out=v("tmp"), in0=v("posf"), in1=v(onm), op=ALU.mult), reads=["r_posf", "r_" + onm], writes=["r_tmp"])
            Q.op(D_, lambda e, snm=snm: e.reduce_sum(out=v(snm), in_=v("tmp"), axis=AX.X), reads=["r_tmp"], writes=["r_" + snm])
            Q.op(D_, lambda e, snm=snm, tt=tt, k=k: e.tensor_copy(out=slots_i[:, tt, k:k + 1], in_=v(snm)), reads=["r_" + snm], writes=[f"slot{tt}_{k}"])
            Q.op("pool", lambda e, tt=tt, k=k, i=i: e.indirect_dma_start(
                out=Xd, out_offset=bass.IndirectOffsetOnAxis(ap=slots_i[:, tt, k:k + 1], axis=0), in_=hnb[i], in_offset=None),
                reads=[f"slot{tt}_{k}", f"rhnb{i}"] + zero_done, writes=(), dma=True)

    SHARED = ("rgf", "rbr", "reb", "rsut", "rwr", "rcnt", "identf", "ones", "onesf", "wts", "slot", "rzt", "Xz")

    class LaneRec:
        def __init__(self, lane):
            self.items, self.lane = [], lane

        def _k(self, keys):
            return [k if k.startswith(SHARED) else f"{k}#{self.lane}" for k in keys]

        def op(self, eng, fn, reads=(), writes=(), dma=False):
            self.items.append(("op", (eng, fn, self._k(reads), self._k(writes)), dict(dma=dma)))

        def dma(self, out, in_, reads=(), writes=(), q="sp"):
            self.items.append(("dma", (out, in_, self._k(reads), self._k(writes)), dict(q=q)))

    streams = []
    for tt in range(16):
        r = LaneRec(tt % 2)
        route_tile(tt, r)
        streams.append(r.items)
    m = max(len(r) for r in streams)
    order = sorted((j * (m // 2) + k, j, k) for j, r in enumerate(streams) for k in range(len(r)))
    for _, j, k in order:
        kind, a, kw = streams[j][k]
        (P.op if kind == "op" else P.dma)(*a, **kw)
    st.close()


def stage_experts(nc, P, cst, Xd, wgu_d, wdn_d, Yd, nexp=64):
    st = Stage(nc, P)
    ident = cst["ident"]
    xe = [st.sb(f"xe{i}", [128, D], BF16) for i in range(2)]
    xeT = [st.sb(f"xeT{i}", [128, 32, 128], BF16) for i in range(2)]
    wg = [st.sb(f"wg{i}", [128, 8, 1536], BF16) for i in range(2)]
    wd = [st.sb(f"wd{i}", [128, 6, 2048], BF16) for i in range(2)]
    sg = st.sb("sg", [128, 768], F32)
    usb = st.sb("usb", [128, 256], F32)
    hh = st.sb("hh", [128, 768], BF16)
    hT = st.sb("hT", [128, 6, 128], BF16)
    ysb = [st.sb(f"ysb{i}", [128, D], F32) for i in range(2)]
    pXb = [st.ps(f"epX{i}", [128, 8, 128], BF16) for i in range(2)]
    pGU = [st.ps(f"epGU{i}", [128, 512], F32) for i in range(3)]
    pHT = st.ps("epHT", [128, 8, 128], BF16)
    pY = [st.ps(f"epY{i}", [128, 512], F32) for i in range(2)]
    gcnt = 0
    dcnt = 0
    for ex in range(nexp):
        i = ex % 2
        P.dma(xe[i], Xd[ex * 128:(ex + 1) * 128, :], writes=[f"xe{i}"])
        for g4 in range(8):
            hf = g4 % 2
            for j in range(4):
                c = g4 * 4 + j
                P.op("pe", lambda e, c=c, j=j, hf=hf, i=i: e.transpose(out=pXb[hf][:, j, :], in_=xe[i][:, c * 128:(c + 1) * 128], identity=ident),
                     reads=[f"xe{i}", "ident"], writes=[f"epX{hf}"])
            P.op("act", lambda e, g4=g4, hf=hf, i=i: e.copy(out=xeT[i][:, g4 * 4:(g4 + 1) * 4, :], in_=pXb[hf][:, 0:4, :]),
                 reads=[f"epX{hf}"], writes=[f"xeT{i}_{g4}", f"epX{hf}"])
        for g8 in range(4):
            j = gcnt % 2
            gcnt += 1
            P.dma(wg[j], wgu_d[ex, g8 * 1024:(g8 + 1) * 1024, :].rearrange("(c p) n -> p c n", p=128), writes=[f"wg{j}"], q="pool")
            for c8 in range(8):
                kc = g8 * 8 + c8
                for nt in range(3):
                    P.op("pe", lambda e, kc=kc, c8=c8, nt=nt, j=j, i=i: e.matmul(pGU[nt], lhsT=xeT[i][:, kc, :], rhs=wg[j][:, c8, nt * 512:(nt + 1) * 512],
                                                                              start=(kc == 0), stop=(kc == 31)),
                         reads=[f"xeT{i}_{kc // 4}", f"wg{j}"], writes=[f"epGU{nt}"])
        P.op("act", lambda e: e.activation(out=sg[:, 0:512], in_=pGU[0], func=AF.Silu), reads=["epGU0"], writes=["sg", "epGU0"])
        P.op("act", lambda e: e.activation(out=sg[:, 512:768], in_=pGU[1][:, 0:256], func=AF.Silu), reads=["epGU1"], writes=["sg", "epGU1"])
        P.op("act", lambda e: e.copy(out=usb, in_=pGU[1][:, 256:512]), reads=["epGU1"], writes=["usb", "epGU1"])
        P.op("dve", lambda e: e.tensor_tensor(out=hh[:, 256:768], in0=pGU[2], in1=sg[:, 256:768], op=ALU.mult), reads=["epGU2", "sg"], writes=["hh", "epGU2"])
        P.op("dve", lambda e: e.tensor_tensor(out=hh[:, 0:256], in0=usb, in1=sg[:, 0:256], op=ALU.mult), reads=["usb", "sg"], writes=["hh"])
        for fc in range(6):
            P.op("pe", lambda e, fc=fc: e.transpose(out=pHT[:, fc, :], in_=hh[:, fc * 128:(fc + 1) * 128], identity=ident), reads=["hh", "ident"], writes=["epHT"])
        P.op("act", lambda e: e.copy(out=hT, in_=pHT[:, 0:6, :]), reads=["epHT"], writes=["hT", "epHT"])
        yi = ex % 2
        for hf in range(2):
            j = dcnt % 2
            dcnt += 1
            P.dma(wd[j], wdn_d[ex, :, hf * 2048:(hf + 1) * 2048].rearrange("(c p) n -> p c n", p=128), writes=[f"wd{j}"], q="pool")
            for c4 in range(4):
                ct = hf * 4 + c4
                pb = ct % 2
                for fc in range(6):
                    P.op("pe", lambda e, fc=fc, c4=c4, pb=pb, j=j: e.matmul(pY[pb], lhsT=hT[:, fc, :], rhs=wd[j][:, fc, c4 * 512:(c4 + 1) * 512],
                                                                         start=(fc == 0), stop=(fc == 5)),
                         reads=["hT", f"wd{j}"], writes=[f"epY{pb}"])
                evac(P, pb, ysb[yi][:, ct * 512:(ct + 1) * 512], pY[pb], [f"epY{pb}"], [f"ysb{yi}_{ct}", f"epY{pb}"])
        P.dma(Yd[ex * 128:(ex + 1) * 128, :], ysb[yi], reads=[f"ysb{yi}_{ct}" for ct in range(8)])
    st.close()


def stage_final(nc, P, Hd, Yd, gfin_d, slots_i, wts, out_d):
    st = Stage(nc, P)
    ht = [st.sb(f"fh{i}", [128, D], F32) for i in range(2)]
    y1 = [st.sb(f"fy1{i}", [128, D], F32) for i in range(2)]
    y2 = [st.sb(f"fy2{i}", [128, D], F32) for i in range(2)]
    gf = st.sb("fgf", [128, D], F32)
    junk = st.sb("fjunk", [128, D], BF16)
    ss = st.sb("fss", [128, 2], F32)
    P.dma(gf, gfin_d.partition_broadcast(128), writes=["fgf"])
    for tt in range(16):
        i = tt % 2
        r0 = tt * 128
        P.dma(ht[i], Hd[r0:r0 + 128, :], writes=[f"fh{i}"])
        for k, yb in enumerate((y1, y2)):
            P.op("pool", lambda e, tt=tt, k=k, yb=yb, i=i: e.indirect_dma_start(
                out=yb[i], out_offset=None, in_=Yd, in_offset=bass.IndirectOffsetOnAxis(ap=slots_i[:, tt, k:k + 1], axis=0)),
                reads=(), writes=[f"fy{k}{i}"], dma=True)
        P.op("dve", lambda e, i=i, tt=tt: e.scalar_tensor_tensor(out=ht[i], in0=y1[i], scalar=wts[:, tt, 0:1], in1=ht[i], op0=ALU.mult, op1=ALU.add),
             reads=[f"fy0{i}", f"fh{i}"], writes=[f"fh{i}"])
        P.op("dve", lambda e, i=i, tt=tt: e.scalar_tensor_tensor(out=ht[i], in0=y2[i], scalar=wts[:, tt, 1:2], in1=ht[i], op0=ALU.mult, op1=ALU.add),
             reads=[f"fy1{i}", f"fh{i}"], writes=[f"fh{i}"])
        P.op("act", lambda e, i=i: e.activation(out=junk, in_=ht[i], func=AF.Square, accum_out=ss[:, i:i + 1]), reads=[f"fh{i}"], writes=["fjunk", f"fss{i}"])
        rsqrt_col(P, ss[:, i:i + 1], ss[:, i:i + 1], EPS, f"fss{i}", f"fss{i}", mul=1.0 / D)
        P.op("dve", lambda e, i=i: e.scalar_tensor_tensor(out=y1[i], in0=ht[i], scalar=ss[:, i:i + 1], in1=gf, op0=ALU.mult, op1=ALU.mult),
             reads=[f"fh{i}", f"fss{i}", "fgf"], writes=[f"fy0{i}"])
        P.dma(out_d[r0:r0 + 128, :], y1[i], reads=[f"fy0{i}"])
    st.close()


def stage_tail(nc, P, x_d, gfin_d, out_d):
    st = Stage(nc, P)
    xt = [st.sb(f"tx{i}", [128, D], F32) for i in range(2)]
    ot = [st.sb(f"to{i}", [128, D], F32) for i in range(2)]
    gf = st.sb("tgf", [128, D], F32)
    junk = st.sb("tjunk", [128, D], BF16)
    ss = st.sb("tss", [128, 2], F32)
    P.dma(gf, gfin_d.partition_broadcast(128), writes=["gf"])
    for t in range(16):
        i = t % 2
        r0 = OWN0 + t * 128
        P.dma(xt[i], x_d[r0:r0 + 128, :], writes=[f"tx{i}"])
        P.op("act", lambda e, i=i: e.activation(out=junk, in_=xt[i], func=AF.Square, accum_out=ss[:, i:i + 1]),
             reads=[f"tx{i}"], writes=["tjunk", f"tss{i}"])
        rsqrt_col(P, ss[:, i:i + 1], ss[:, i:i + 1], EPS, f"tss{i}", f"tss{i}", mul=1.0 / D)
        P.op("dve", lambda e, i=i: e.scalar_tensor_tensor(out=ot[i], in0=xt[i], scalar=ss[:, i:i + 1], in1=gf, op0=ALU.mult, op1=ALU.mult),
             reads=[f"tx{i}", f"tss{i}", "gf"], writes=[f"to{i}"])
        P.dma(out_d[t * 128:(t + 1) * 128, :], ot[i], reads=[f"to{i}"])
    st.close()


class SplitRows:
    def __init__(self, parts, split):
        self.parts, self.split = parts, split

    def __getitem__(self, key):
        rs, cs = key
        if rs.stop <= self.split:
            return self.parts[0][rs.start:rs.stop, cs]
        assert rs.start >= self.split
        return self.parts[1][rs.start - self.split:rs.stop - self.split, cs]


def build_program(debug=False):
    nc = bass.Bass("TRN2", target_bir_lowering=False)
    ext = lambda name, shape, dt=F32: nc.dram_tensor(name, shape, dt, kind="ExternalInput").ap()
    x_d = ext("xs", [TL, D])
    gmix_d = ext("g_mix", [D])
    win_d = ext("w_in", [D, NCOL])
    convw = ext("convw", [128, 48, 4])
    alog = ext("a_log", [16])
    dtbias = ext("dt_bias", [16])
    gnormw = ext("gdn_norm_w", [128])
    gmk_d = ext("gdn_lvl_masks", [128, 8, 128], BF16)
    KD = {nm: ext("k_" + nm, shp, dt) for nm, shp, dt in NSA_CONST_SPECS}
    w1k, w2k, pek = ext("cmp_w1_k", [32, 128, 128]), ext("cmp_w2_k", [128, 128]), ext("cmp_peT_k", [128, 32])
    w1v, w2v, pev = ext("cmp_w1_v", [32, 128, 128]), ext("cmp_w2_v", [128, 128]), ext("cmp_peT_v", [128, 32])
    wa_d, wb_d, wo_d = ext("w_branch_gdn", [2048, D]), ext("w_branch_nsa", [2048, D]), ext("w_out", [D, D])
    gffn_d = ext("g_ffn", [D])
    wr_d, br_d, eb_d = ext("w_router", [128, 32, 72]), ext("b_router", [72]), ext("ebase", [64])
    wgu_d, wdn_d = ext("w_gate_up", [64, D, 1536]), ext("w_down", [64, 768, D])
    gfin = ext("g_final", [D])
    out_d = nc.dram_tensor("out", [TL - OWN0, D], F32, kind="ExternalOutput").ap()
    PT_d = SplitRows([nc.dram_tensor("PT_scratch_a", [C_NKV, TL], F32).ap(),
                      nc.dram_tensor("PT_scratch_b", [NCOL - C_NKV, TL], F32).ap()], C_NKV)
    sk = "ExternalOutput" if debug else "Internal"
    OG_d = nc.dram_tensor("OG_scratch", [2048, 2048], BF16, kind=sk).ap()
    ON_d = nc.dram_tensor("ON_scratch", [2048, 2048], BF16, kind=sk).ap()
    Hd = nc.dram_tensor("H_scratch", [2048, D], F32, kind=sk).ap()
    Xd = nc.dram_tensor("X_scratch", [64 * CAP, D], BF16).ap()
    Yd = nc.dram_tensor("Y_scratch", [64 * CAP, D], F32).ap()
    P = Prog(nc)
    cst = make_consts(nc, P)
    ab_tok = nc.alloc_sbuf_tensor("ab_tok", [128, 32, 32], F32).ap()
    slots_i = nc.alloc_sbuf_tensor("slots_i", [128, 16, 2], I32).ap()
    wts = nc.alloc_sbuf_tensor("wts", [128, 16, 2], F32).ap()
    stage_proj(nc, P, cst, x_d, gmix_d, win_d, PT_d, ab_tok)
    stage_gdn(nc, P, cst, PT_d, ab_tok, convw, alog, dtbias, gnormw, gmk_d, OG_d)
    stage_nsa(nc, P, cst, PT_d, KD, w1k, w2k, pek, w1v, w2v, pev, ON_d)
    stage_merge(nc, P, cst, PT_d, OG_d, ON_d, wa_d, wb_d, wo_d, Hd)
    stage_route(nc, P, cst, x_d, Hd, gffn_d, wr_d, br_d, eb_d, Xd, slots_i, wts)
    stage_experts(nc, P, cst, Xd, wgu_d, wdn_d, Yd)
    stage_final(nc, P, Hd, Yd, gfin, slots_i, wts, out_d)
    P.emit()
    return nc


def gdn_level_masks():
    import ml_dtypes
    c = np.arange(128)[:, None]
    s_ = np.arange(128)[None, :]
    m = np.zeros((128, 8, 128), np.float32)
    for k in range(7):
        mk = (((c >> k) & 1) == 1) & (((s_ >> k) & 1) == 0) & ((c >> (k + 1)) == (s_ >> (k + 1)))
        m[:, k, :] = mk.T
        if k == 0:
            m[:, 7, :] = mk
    return m.astype(ml_dtypes.bfloat16)


def host_shared(inputs):
    f = lambda k: np.ascontiguousarray(np.asarray(inputs[k])[0])
    wr = np.concatenate([f("w_group"), f("w_expert")], axis=1)
    return {
        "g_mix": f("g_mix"), "w_in": f("w_in"),
        "convw": np.ascontiguousarray(f("gdn_conv_w").reshape(4, 48, 128).transpose(2, 1, 0)),
        "gdn_lvl_masks": gdn_level_masks(),
        "a_log": f("gdn_a_log"), "dt_bias": f("gdn_dt_bias"), "gdn_norm_w": f("gdn_norm_w"),
        "cmp_w1_k": f("cmp_w1_k"), "cmp_w2_k": f("cmp_w2_k"), "cmp_peT_k": np.ascontiguousarray(f("cmp_pe_k").T),
        "cmp_w1_v": f("cmp_w1_v"), "cmp_w2_v": f("cmp_w2_v"), "cmp_peT_v": np.ascontiguousarray(f("cmp_pe_v").T),
        "w_branch_gdn": f("w_branch_gdn"), "w_branch_nsa": f("w_branch_nsa"), "w_out": f("w_out"),
        "g_ffn": f("g_ffn"),
        "w_router": np.ascontiguousarray(wr.reshape(32, 128, 72).transpose(1, 0, 2)),
        "b_router": np.concatenate([f("b_group"), f("b_expert")]),
        "ebase": (np.arange(64) * CAP).astype(np.float32),
        "w_gate_up": f("w_gate_up"), "w_down": f("w_down"),
        "g_final": np.ascontiguousarray(np.asarray(inputs["g_final"])),
    }


def kernel(**inputs):
    x = np.asarray(inputs["x"], dtype=np.float32)
    B, T, _ = x.shape
    nc = build_program()
    shared = host_shared(inputs)
    consts = [nsa_host_consts(0), nsa_host_consts(1)]
    in_maps = []
    for c in range(8):
        b, half = c // 2, c % 2
        xs = np.zeros((TL, D), np.float32)
        if half == 0:
            xs[OWN0:] = x[b, :2048]
        else:
            xs[:] = x[b]
        m = dict(shared)
        m["xs"] = xs
        for k, v in consts[half].items():
            m["k_" + k] = v
        in_maps.append(m)
    res = run_bass_kernel_spmd(nc, in_maps, core_ids=list(range(8)))
    out = np.zeros((B, T, D), np.float32)
    for c in range(8):
        b, half = c // 2, c % 2
        out[b, half * 2048:(half + 1) * 2048] = res.results[c]["out"]
    return out
```

```python
from contextlib import ExitStack
import numpy as np
import concourse.bass as bass
import concourse.mybir as mybir
from concourse.bass_utils import run_bass_kernel_spmd

F32 = mybir.dt.float32
BF16 = mybir.dt.bfloat16
I32 = mybir.dt.int32
AF = mybir.ActivationFunctionType
ALU = mybir.AluOpType
AX = mybir.AxisListType

SEM_LIM = 24000
D = 4096
TL = 4096
OWN0 = 2048
NCOL = 21584
EPS = 1e-6
C_QKV, C_Z, C_A, C_B, C_NQ, C_NKV, C_NG, C_MG = 0, 6144, 8192, 8208, 8224, 10272, 13344, 13392
NEG = -30000.0


class Prog:
    ENGS = ("pe", "act", "dve", "pool", "sp")
    NDMA = {"sp": 16, "pool": 8, "act": 4}

    def __init__(self, nc):
        self.nc = nc
        self.ops = []
        self.last_w = {}
        self.readers = {}
        self.dma_count = {"sp": 0, "pool": 0, "act": 0}
        self.dma_ops = {"sp": [], "pool": [], "act": []}
        self.last_on = {}
        self.barrier_idx = None

    def op(self, eng, fn, reads=(), writes=(), dma=False):
        idx = len(self.ops)
        deps = set()
        if self.barrier_idx is not None:
            deps.add(self.barrier_idx)
        for r in reads:
            w = self.last_w.get(r)
            if w is not None:
                deps.add(w)
        for w_ in writes:
            w = self.last_w.get(w_)
            if w is not None:
                deps.add(w)
            for r in self.readers.get(w_, ()):
                deps.add(r)
        o = dict(eng=eng, fn=fn, deps=deps, dma=dma, idx=idx, marked=False)
        if dma:
            n = self.dma_count[eng]
            self.dma_count[eng] = n + 1
            nd = self.NDMA[eng]
            o["dma_n"] = n
            lst = self.dma_ops[eng]
            if n >= nd:
                deps.add(lst[n - nd])
            lst.append(idx)
        else:
            self.last_on[eng] = idx
        deps.discard(idx)
        self.ops.append(o)
        for r in reads:
            self.readers.setdefault(r, []).append(idx)
        for w_ in writes:
            self.last_w[w_] = idx
            self.readers[w_] = []
        return idx

    def dma(self, out, in_, reads=(), writes=(), q="sp", **kw):
        return self.op(q, lambda e: e.dma_start(out=out, in_=in_, **kw), reads, writes, dma=True)

    def barrier(self):
        deps = set(self.last_on.values())
        for q, lst in self.dma_ops.items():
            deps.update(lst[-self.NDMA[q]:])
        idx = self.op("sp", lambda e: e.nop(), (), ())
        self.ops[idx]["deps"] |= deps
        self.ops[idx]["deps"].discard(idx)
        self.barrier_idx = idx
        self.last_w = {}
        self.readers = {}

    def emit(self):
        nc = self.nc
        ops = self.ops
        for o in ops:
            for d in o["deps"]:
                p = ops[d]
                if p["eng"] == "pe" and o["eng"] == "pe" and not p["dma"] and not o["dma"]:
                    continue
                p["marked"] = True
        cnt = {e: 0 for e in self.ENGS}
        for o in ops:
            if o["dma"]:
                nd = self.NDMA[o["eng"]]
                o["tok"] = ("dma_" + o["eng"], o["dma_n"] % nd, 16 * (o["dma_n"] // nd + 1))
            elif o["marked"]:
                m = cnt[o["eng"]]
                cnt[o["eng"]] = m + 1
                o["tok"] = (o["eng"], m // SEM_LIM, m % SEM_LIM + 1)
            else:
                o["tok"] = None
        sems = {}
        for e in self.ENGS:
            for j in range(cnt[e] // SEM_LIM + 1):
                sems[(e, j)] = nc.alloc_semaphore(name=f"s_{e}_{j}")
        for q, nd in self.NDMA.items():
            if self.dma_count[q] > 0:
                for j in range(nd):
                    sems[("dma_" + q, j)] = nc.alloc_semaphore(name=f"d_{q}_{j}")
        final_dma = {}
        for o in ops:
            if o["dma"]:
                t = o["tok"]
                final_dma[(t[0], t[1])] = max(final_dma.get((t[0], t[1]), 0), t[2])

        def run_engine(ename, eobj, is_last_waiter=False):
            waited = {}
            for o in ops:
                if o["eng"] != ename:
                    continue
                for d in sorted(o["deps"]):
                    p = ops[d]
                    if p["eng"] == "pe" and ename == "pe" and not p["dma"] and not o["dma"]:
                        continue
                    t = p["tok"]
                    key = (t[0], t[1])
                    if waited.get(key, 0) >= t[2]:
                        continue
                    eobj.wait_ge(sems[key], t[2])
                    waited[key] = t[2]
                ins = o["fn"](eobj)
                t = o["tok"]
                if t is not None:
                    ins.then_inc(sems[(t[0], t[1])], 16 if o["dma"] else 1)
            if is_last_waiter:
                for key, v in final_dma.items():
                    if waited.get(key, 0) < v:
                        eobj.wait_ge(sems[key], v)

        with nc.Block() as block:
            @block.sync
            def _(e):
                run_engine("sp", e, True)

            @block.tensor
            def _(e):
                run_engine("pe", e)

            @block.scalar
            def _(e):
                run_engine("act", e)

            @block.vector
            def _(e):
                run_engine("dve", e)

            @block.gpsimd
            def _(e):
                run_engine("pool", e)


class Stage:
    def __init__(self, nc, P):
        self.nc, self.P = nc, P
        self.es = ExitStack()

    def sb(self, name, shape, dt):
        return self.es.enter_context(self.nc.sbuf_tensor(name, shape, dt)).ap()

    def ps(self, name, shape, dt=F32):
        return self.es.enter_context(self.nc.psum_tensor(name, shape, dt)).ap()

    def close(self):
        self.P.barrier()
        self.es.close()


def evac(P, k, out, in_, reads, writes):
    if k % 2 == 0:
        return P.op("act", lambda e: e.copy(out=out, in_=in_), reads, writes)
    return P.op("dve", lambda e: e.tensor_copy(out=out, in_=in_), reads, writes)


def rsqrt_col(P, out, in_, add, rkey, wkey, mul=1.0):
    P.op("dve", lambda e: e.tensor_scalar(out=out, in0=in_, scalar1=float(mul), scalar2=float(add), op0=ALU.mult, op1=ALU.add),
         reads=[rkey], writes=[wkey])
    P.op("act", lambda e: e.sqrt(out=out, in_=out), reads=[wkey], writes=[wkey])
    P.op("dve", lambda e: e.reciprocal(out=out, in_=out), reads=[wkey], writes=[wkey])


def make_consts(nc, P):
    c = {}
    c["identf"] = nc.alloc_sbuf_tensor("identf", [128, 128], F32).ap()
    c["ident"] = nc.alloc_sbuf_tensor("ident", [128, 128], BF16).ap()
    c["onesf"] = nc.alloc_sbuf_tensor("onesf", [128, 128], F32).ap()
    c["ones"] = nc.alloc_sbuf_tensor("ones", [128, 128], BF16).ap()
    P.op("pool", lambda e: e.memset(c["identf"], 0.0), writes=["identf"])
    P.op("pool", lambda e: e.affine_select(out=c["identf"], in_=c["identf"], pattern=[[-1, 128]],
                                           compare_op=ALU.not_equal, fill=1.0, base=0, channel_multiplier=1),
         reads=["identf"], writes=["identf"])
    P.op("dve", lambda e: e.tensor_copy(out=c["ident"], in_=c["identf"]), reads=["identf"], writes=["ident"])
    P.op("pool", lambda e: e.memset(c["onesf"], 1.0), writes=["onesf"])
    P.op("pool", lambda e: e.memset(c["ones"], 1.0), writes=["ones"])
    return c


def col_tiles(prefix, tb=1):
    if prefix and tb == 0:
        groups = [(C_QKV + 2048, 4096), (C_NKV, 2048)]
    elif prefix:
        groups = [(C_QKV + 2048, 4096), (C_NKV, 3072)]
    else:
        groups = [(C_QKV, 6144), (C_Z, 2048), (C_NQ, 2048), (C_NKV, 3072), (C_NG, 48), (C_MG, 8192)]
    out = []
    for c0, n in groups:
        o = 0
        while o < n:
            w = min(256, n - o)
            out.append((c0 + o, w))
            o += w
    return out


def stage_proj(nc, P, cst, x_d, gmix_d, win_d, PT_d, ab_tok, nblocks=4):
    st = Stage(nc, P)
    xt = [st.sb(f"xt{i}", [128, D], F32) for i in range(2)]
    xb = [st.sb(f"xb{i}", [128, D], BF16) for i in range(2)]
    gm = st.sb("gm", [128, D], F32)
    junk = st.sb("junk", [128, D], BF16)
    xnT = st.sb("xnT", [128, 32, 1024], BF16)
    wt = [st.sb(f"wt{i}", [128, 32, 256], BF16) for i in range(2)]
    og = [st.sb(f"og{i}", [128, 1024], F32) for i in range(2)]
    ss = st.sb("ss", [128, 2], F32)
    rs = st.sb("rs", [128, 2], F32)
    wab = st.sb("wab", [128, 32, 32], BF16)
    ptp = [st.ps(f"ptp{i}", [128, 4, 128], BF16) for i in range(2)]
    pp = [st.ps(f"pp{i}", [128, 1024], F32) for i in range(2)]
    pab = st.ps("pab", [128, 32], F32)

    P.dma(gm, gmix_d.partition_broadcast(128), writes=["gm"])
    P.op("dve", lambda e: e.tensor_scalar(out=gm, in0=gm, scalar1=float(np.sqrt(D)), scalar2=None, op0=ALU.mult),
         reads=["gm"], writes=["gm"])
    P.dma(wab, win_d[:, C_A:C_A + 32].rearrange("(c p) n -> p c n", p=128), writes=["wab"], q="pool")

    wcount = 0
    ocount = 0
    tcount = 0
    for tb in range(4 - nblocks, 4):
        prefix = tb < 2
        for t in range(8):
            i = tcount % 2
            tcount += 1
            r0 = tb * 1024 + t * 128
            P.dma(xt[i], x_d[r0:r0 + 128, :], writes=[f"xt{i}"])
            P.op("act", lambda e, i=i: e.activation(out=junk, in_=xt[i], func=AF.Square, accum_out=ss[:, i:i + 1]),
                 reads=[f"xt{i}"], writes=["junk", f"ss{i}"])
            rsqrt_col(P, rs[:, i:i + 1], ss[:, i:i + 1], float(D * EPS), f"ss{i}", f"rs{i}")
            P.op("dve", lambda e, i=i: e.scalar_tensor_tensor(out=xb[i], in0=xt[i], scalar=rs[:, i:i + 1], in1=gm,
                                                              op0=ALU.mult, op1=ALU.mult),
                 reads=[f"xt{i}", f"rs{i}", "gm"], writes=[f"xb{i}"])
            for g in range(8):
                pj = g % 2
                for j in range(4):
                    c = g * 4 + j
                    P.op("pe", lambda e, c=c, j=j, pj=pj, i=i: e.transpose(out=ptp[pj][:, j, :], in_=xb[i][:, c * 128:(c + 1) * 128],
                                                                         identity=cst["ident"]),
                         reads=[f"xb{i}", "ident"], writes=[f"ptp{pj}"])
                evac(P, g, xnT[:, g * 4:(g + 1) * 4, t * 128:(t + 1) * 128], ptp[pj], [f"ptp{pj}"], [f"xnT{t}_{g}"])
        for t in range(8):
            for c in range(32):
                P.op("pe", lambda e, c=c, t=t: e.matmul(pab, lhsT=xnT[:, c, t * 128:(t + 1) * 128], rhs=wab[:, c, :],
                                                        start=(c == 0), stop=(c == 31)),
                     reads=[f"xnT{t}_{c // 4}", "wab"], writes=["pab"])
            evac(P, t, ab_tok[:, tb * 8 + t, :], pab, ["pab"], [f"ab{tb * 8 + t}"])
        for (c0, ncw) in col_tiles(prefix, tb):
            j = wcount % 2
            wcount += 1
            P.dma(wt[j][:, :, :ncw], win_d[:, c0:c0 + ncw].rearrange("(c p) n -> p c n", p=128),
                  writes=[f"wt{j}"], q="pool")
            for sub in range(0, ncw, 128):
                m = min(128, ncw - sub)
                k = ocount % 2
                ocount += 1
                halves = (1,) if (tb == 1 and C_NKV + 2048 <= c0 < C_NKV + 3072) else (0, 1)
                for half in halves:
                    for c in range(32):
                        P.op("pe", lambda e, c=c, j=j, k=k, half=half, sub=sub, m=m: e.matmul(
                            pp[k][:m, half * 512:(half + 1) * 512], lhsT=wt[j][:, c, sub:sub + m],
                            rhs=xnT[:, c, half * 512:(half + 1) * 512], start=(c == 0), stop=(c == 31)),
                            reads=[f"wt{j}"] + [f"xnT{t}_{c // 4}" for t in range(half * 4, half * 4 + 4)],
                            writes=[f"pp{k}_{half}"])
                    evac(P, half, og[k][:m, half * 512:(half + 1) * 512], pp[k][:m, half * 512:(half + 1) * 512],
                         [f"pp{k}_{half}"], [f"og{k}_{half}"])
                if halves == (1,):
                    P.dma(PT_d[c0 + sub:c0 + sub + m, tb * 1024 + 512:(tb + 1) * 1024], og[k][:m, 512:1024],
                          reads=[f"og{k}_1"])
                else:
                    P.dma(PT_d[c0 + sub:c0 + sub + m, tb * 1024:(tb + 1) * 1024], og[k][:m, :],
                          reads=[f"og{k}_0", f"og{k}_1"])
        if tb == 1:
            for o in range(0, 2048, 256):
                c0 = C_QKV + o
                j = wcount % 2
                wcount += 1
                P.dma(wt[j], win_d[:, c0:c0 + 256].rearrange("(c p) n -> p c n", p=128), writes=[f"wt{j}"], q="pool")
                for sub in (0, 128):
                    k = ocount % 2
                    ocount += 1
                    for c in range(32):
                        P.op("pe", lambda e, c=c, j=j, k=k, sub=sub: e.matmul(
                            pp[k][:, 0:128], lhsT=wt[j][:, c, sub:sub + 128], rhs=xnT[:, c, 896:1024], start=(c == 0), stop=(c == 31)),
                            reads=[f"wt{j}", f"xnT7_{c // 4}"], writes=[f"pp{k}_0"])
                    evac(P, 0, og[k][:, 0:128], pp[k][:, 0:128], [f"pp{k}_0"], [f"og{k}_0"])
                    P.dma(PT_d[c0 + sub:c0 + sub + 128, tb * 1024 + 896:(tb + 1) * 1024], og[k][:, 0:128], reads=[f"og{k}_0"])
    st.close()


def stage_gdn(nc, P, cst, PT_d, ab_tok, convw_d, alog_d, dtb_d, normw_d, gmk_d, OG_d, groups=range(8), dbg=None):
    st = Stage(nc, P)
    identf, ident, onesf, ones = cst["identf"], cst["ident"], cst["onesf"], cst["ones"]
    NT = 32
    sc = {}
    for nm in ("g", "gc", "ngc", "beta", "nbeta", "be1", "e1", "e2", "dch"):
        sc[nm] = st.sb("sc_" + nm, [128, NT * 16], F32)
    dtb = st.sb("dtb", [128, 16], F32)
    nA = st.sb("nA", [128, 16], F32)
    normw = st.sb("normw", [128, 1], F32)
    cw = st.sb("cw", [128, 48, 4], F32)
    zeros = st.sb("zeros", [128, 128], F32)
    Tri = st.sb("Tri", [128, 128], F32)
    TriN = st.sb("TriN", [128, 128], F32)
    maskU = st.sb("maskU", [128, 128], F32)
    maskL = st.sb("maskL", [128, 128], F32)
    gmk = st.sb("gmk", [128, 8, 128], BF16)
    P.dma(gmk, gmk_d, writes=["gmk"])
    xr = st.sb("xr", [128, TL], F32)
    yy = st.sb("yy", [128, TL], F32)
    sq = st.sb("sq", [128, TL], BF16)
    rt = [st.sb(f"rt{i}", [128, 512], F32) for i in range(2)]
    qkv = [[st.sb(f"qkv{hs}_{w}", [128, TL], BF16) for w in range(3)] for hs in range(2)]
    zs = [st.sb(f"zs{hs}", [128, 2048], F32) for hs in range(2)]
    OGb = [st.sb(f"OGb{hs}", [128, 2048], BF16) for hs in range(2)]
    pbig = st.ps("pbig", [128, 512], F32)
    pbig2 = st.ps("pbig2", [128, 512], F32)

    P.dma(dtb, dtb_d.partition_broadcast(128), writes=["dtb"])
    P.dma(nA, alog_d.partition_broadcast(128), writes=["nA"])
    P.dma(normw, normw_d.rearrange("(c o) -> c o", o=1), writes=["normw"])
    P.dma(cw, convw_d, writes=["cw"])
    P.op("pool", lambda e: e.memset(zeros, 0.0), writes=["zeros"])
    P.op("pool", lambda e: e.affine_select(out=Tri, in_=onesf, pattern=[[1, 128]], compare_op=ALU.is_ge, fill=0.0,
                                           base=0, channel_multiplier=-1), reads=["onesf"], writes=["Tri"])
    P.op("dve", lambda e: e.tensor_scalar(out=TriN, in0=Tri, scalar1=-1.0, scalar2=None, op0=ALU.mult),
         reads=["Tri"], writes=["TriN"])
    P.op("pool", lambda e: e.affine_select(out=maskU, in_=zeros, pattern=[[1, 128]], compare_op=ALU.is_ge, fill=NEG,
                                           base=0, channel_multiplier=-1), reads=["zeros"], writes=["maskU"])
    P.op("pool", lambda e: e.affine_select(out=maskL, in_=zeros, pattern=[[-1, 128]], compare_op=ALU.is_gt, fill=NEG,
                                           base=0, channel_multiplier=1), reads=["zeros"], writes=["maskL"])
    if dbg == 1:
        st.close()
        return
    g3 = sc["g"].rearrange("p (t h) -> p t h", h=16)
    b3 = sc["beta"].rearrange("p (t h) -> p t h", h=16)
    abk = [f"ab{t}" for t in range(NT)]
    P.op("act", lambda e: e.activation(out=nA, in_=nA, func=AF.Exp), reads=["nA"], writes=["nA"])
    P.op("dve", lambda e: e.tensor_scalar(out=nA, in0=nA, scalar1=-1.0, scalar2=None, op0=ALU.mult), reads=["nA"], writes=["nA"])
    P.op("dve", lambda e: e.tensor_tensor(out=g3, in0=ab_tok[:, :, 0:16], in1=dtb.unsqueeze(1).to_broadcast([128, NT, 16]),
                                          op=ALU.add), reads=abk + ["dtb"], writes=["g"])
    P.op("act", lambda e: e.activation(out=sc["g"], in_=sc["g"], func=AF.Exp), reads=["g"], writes=["g"])
    P.op("act", lambda e: e.activation(out=sc["g"], in_=sc["g"], func=AF.Ln, bias=1.0), reads=["g"], writes=["g"])
    P.op("dve", lambda e: e.tensor_tensor(out=g3, in0=g3, in1=nA.unsqueeze(1).to_broadcast([128, NT, 16]), op=ALU.mult),
         reads=["g", "nA"], writes=["g"])
    P.op("act", lambda e: e.activation(out=b3, in_=ab_tok[:, :, 16:32], func=AF.Sigmoid), reads=abk, writes=["beta"])
    if dbg == 2:
        st.close()
        return
    P.op("pe", lambda e: e.matmul(pbig, lhsT=Tri, rhs=sc["g"], start=True, stop=True), reads=["Tri", "g"], writes=["pbig"])
    P.op("pe", lambda e: e.matmul(pbig2, lhsT=onesf, rhs=sc["g"], start=True, stop=True), reads=["onesf", "g"], writes=["pbig2"])
    P.op("act", lambda e: e.copy(out=sc["gc"], in_=pbig), reads=["pbig"], writes=["gc"])
    P.op("act", lambda e: e.activation(out=sc["e1"], in_=pbig, func=AF.Exp), reads=["pbig"], writes=["e1"])
    if dbg == 3:
        st.close()
        return
    P.op("dve", lambda e: e.tensor_scalar(out=sc["ngc"], in0=sc["gc"], scalar1=-1.0, scalar2=None, op0=ALU.mult), reads=["gc"], writes=["ngc"])
    P.op("act", lambda e: e.copy(out=sc["e2"], in_=pbig2), reads=["pbig2"], writes=["e2"])
    P.op("dve", lambda e: e.tensor_tensor(out=sc["e2"], in0=sc["e2"], in1=sc["gc"], op=ALU.subtract), reads=["e2", "gc"], writes=["e2"])
    if dbg == 5:
        st.close()
        return
    P.op("act", lambda e: e.activation(out=sc["e2"], in_=sc["e2"], func=AF.Exp), reads=["e2"], writes=["e2"])
    if dbg == 6:
        st.close()
        return
    P.op("act", lambda e: e.activation(out=sc["dch"], in_=pbig2, func=AF.Exp), reads=["pbig2"], writes=["dch"])
    P.op("dve", lambda e: e.tensor_tensor(out=sc["be1"], in0=sc["beta"], in1=sc["e1"], op=ALU.mult), reads=["beta", "e1"], writes=["be1"])
    P.op("dve", lambda e: e.tensor_scalar(out=sc["nbeta"], in0=sc["beta"], scalar1=-1.0, scalar2=None, op0=ALU.mult),
         reads=["beta"], writes=["nbeta"])
    SCK = ["g", "gc", "ngc", "beta", "nbeta", "be1", "e1", "e2", "dch"]
    if dbg == 4:
        st.close()
        return

    W = []
    for hs in range(2):
        w = {}
        if hs == 0:
            w["pA"] = pbig[:, 0:128]
            w["pB"] = pbig2[:, 0:128]
        else:
            w["pA"] = st.ps(f"pA{hs}", [128, 512], F32)[:, 0:128]
            w["pB"] = st.ps(f"pB{hs}", [128, 512], F32)[:, 0:128]
        w["pC"] = st.ps(f"pC{hs}", [128, 512], F32)[:, 0:256]
        w["pT"] = st.ps(f"pT{hs}", [128, 8, 128], BF16)[:, 0:2, :]
        w["Gb"] = st.sb(f"Gb{hs}", [128, 128], F32)
        w["E"] = st.sb(f"E{hs}", [128, 128], F32)
        w["ET"] = st.sb(f"ET{hs}", [128, 128], F32)
        w["X"] = [st.sb(f"X{hs}_{i}", [128, 128], BF16) for i in range(2)]
        w["XT"] = [st.sb(f"XT{hs}_{i}", [128, 128], BF16) for i in range(2)]
        w["Yb"] = st.sb(f"Yb{hs}", [128, 256], BF16)
        w["Pm"] = st.sb(f"Pm{hs}", [128, 128], BF16)
        w["NkTs"] = [st.sb(f"NkT{hs}_{l}", [128, 128], BF16) for l in range(6)]
        for pb in range(2):
            w[f"Y{pb}"] = st.sb(f"Y{hs}_{pb}", [128, 256], F32)
            w[f"attnT{pb}"] = st.sb(f"attnT{hs}_{pb}", [128, 128], BF16)
            w[f"kout{pb}"] = st.sb(f"kout{hs}_{pb}", [128, 128], BF16)
            w[f"wT{pb}"] = st.sb(f"wT{hs}_{pb}", [128, 128], BF16)
        w["S"] = st.sb(f"S{hs}", [128, 128], F32)
        w["Sb"] = st.sb(f"Sb{hs}", [128, 128], BF16)
        w["vnew"] = st.sb(f"vnew{hs}", [128, 128], BF16)
        w["oS"] = st.sb(f"oS{hs}", [128, 128], F32)
        w["o"] = st.sb(f"o{hs}", [128, 128], F32)
        w["on"] = st.sb(f"on{hs}", [128, 128], BF16)
        w["junk"] = st.sb(f"gjunk{hs}", [128, 128], BF16)
        w["ss"] = st.sb(f"gss{hs}", [128, 1], F32)
        W.append(w)

    def K(hs, nm):
        if hs == 0 and nm == "pA":
            return "pbig"
        if hs == 0 and nm == "pB":
            return "pbig2"
        return f"{nm}@{hs}"

    def preprocess(grp):
        for hs in range(2):
            h = grp * 2 + hs
            for wi, coff in enumerate((0, 2048, 4096)):
                ct = (coff + h * 128) // 128
                row0 = C_QKV + coff + h * 128
                dst = qkv[hs][wi]
                lo = OWN0 - 3 if wi == 0 else 0
                P.dma(xr[:, lo:], PT_d[row0:row0 + 128, lo:TL], writes=["xr"])
                eng = "dve"
                P.op(eng, lambda e, ct=ct, lo=lo: e.tensor_scalar(out=yy[:, lo:], in0=xr[:, lo:], scalar1=cw[:, ct, 3:4], scalar2=None, op0=ALU.mult),
                     reads=["xr", "cw"], writes=["yy"])
                for sh in (1, 2, 3):
                    P.op(eng, lambda e, ct=ct, sh=sh, lo=lo: e.scalar_tensor_tensor(out=yy[:, lo + sh:], in0=xr[:, lo:TL - sh],
                                                                                 scalar=cw[:, ct, 3 - sh:4 - sh], in1=yy[:, lo + sh:],
                                                                                 op0=ALU.mult, op1=ALU.add),
                         reads=["xr", "cw", "yy"], writes=["yy"])
                if wi == 2:
                    P.op("act", lambda e, dst=dst: e.activation(out=dst, in_=yy, func=AF.Silu), reads=["yy"], writes=[K(hs, f"qkv{wi}")])
                    continue
                c0 = OWN0 if wi == 0 else 0
                P.op("act", lambda e, c0=c0: e.activation(out=yy[:, c0:], in_=yy[:, c0:], func=AF.Silu), reads=["yy"], writes=["yy"])
                P.op("pool", lambda e, c0=c0: e.tensor_tensor(out=sq[:, c0:], in0=yy[:, c0:], in1=yy[:, c0:], op=ALU.mult), reads=["yy"], writes=["sq"])
                scale = float(128 ** -0.5) if wi == 0 else 1.0
                for ch in range(c0 // 512, 8):
                    cs = slice(ch * 512, (ch + 1) * 512)
                    r = rt[ch % 2]
                    P.op("pe", lambda e, cs=cs: e.matmul(pbig, lhsT=ones, rhs=sq[:, cs], start=True, stop=True),
                         reads=["ones", "sq"], writes=["pbig"])
                    P.op("act", lambda e, r=r: e.activation(out=r, in_=pbig, func=AF.Ln, bias=float(EPS)), reads=["pbig"], writes=[f"rt{ch % 2}"])
                    P.op("act", lambda e, r=r: e.activation(out=r, in_=r, func=AF.Exp, scale=-0.5), reads=[f"rt{ch % 2}"], writes=[f"rt{ch % 2}"])
                    P.op("dve", lambda e, r=r, cs=cs, dst=dst, scale=scale: e.scalar_tensor_tensor(
                        out=dst[:, cs], in0=yy[:, cs], scalar=scale, in1=r, op0=ALU.mult, op1=ALU.mult),
                        reads=["yy", f"rt{ch % 2}"], writes=[K(hs, f"qkv{wi}")])
            P.dma(zs[hs], PT_d[C_Z + h * 128:C_Z + (h + 1) * 128, OWN0:TL], writes=[K(hs, "zs")])
            P.op("act", lambda e, hs=hs: e.activation(out=zs[hs], in_=zs[hs], func=AF.Silu), reads=[K(hs, "zs")], writes=[K(hs, "zs")])
            P.op("pool", lambda e, hs=hs: e.memset(W[hs]["S"], 0.0), writes=[K(hs, "S")])
            P.op("pool", lambda e, hs=hs: e.memset(W[hs]["Sb"], 0.0), writes=[K(hs, "Sb")])

    def pre(grp, hs, n, Q):
        h = grp * 2 + hs
        w = W[hs]
        pb = n % 2
        col = n * 16 + h
        cs = slice(n * 128, (n + 1) * 128)
        qT, kT, vT = (qkv[hs][i][:, cs] for i in range(3))
        kq, kk, kv = (K(hs, f"qkv{i}") for i in range(3))
        sv = lambda nm: sc[nm][:, col:col + 1]
        k_ = lambda nm: K(hs, nm)
        Q.op("dve", lambda e: e.tensor_scalar(out=w["Gb"], in0=onesf, scalar1=sv("g"), scalar2=None, op0=ALU.mult),
             reads=["onesf", "g"], writes=[k_("Gb")])
        if n >= 16:
            Q.op("pe", lambda e: e.matmul(w["pA"], lhsT=w["Gb"], rhs=Tri, start=True, stop=False), reads=[k_("Gb"), "Tri"], writes=[k_("pA")])
            Q.op("pe", lambda e: e.matmul(w["pA"], lhsT=identf, rhs=maskU, start=False, stop=True), reads=["identf", "maskU"], writes=[k_("pA")])
        Q.op("pe", lambda e: e.matmul(w["pB"], lhsT=w["Gb"], rhs=TriN, start=True, stop=False), reads=[k_("Gb"), "TriN"], writes=[k_("pB")])
        Q.op("pe", lambda e: e.matmul(w["pB"], lhsT=identf, rhs=maskL, start=False, stop=True), reads=["identf", "maskL"], writes=[k_("pB")])
        if n >= 16:
            Q.op("act", lambda e: e.activation(out=w["ET"], in_=w["pA"], func=AF.Exp, bias=sv("ngc")), reads=[k_("pA"), "ngc"], writes=[k_("ET")])
        Q.op("act", lambda e: e.activation(out=w["E"], in_=w["pB"], func=AF.Exp, bias=sv("gc")), reads=[k_("pB"), "gc"], writes=[k_("E")])
        Q.op("pe", lambda e: e.matmul(w["pA"], lhsT=kT, rhs=kT, start=True, stop=True), reads=[kk], writes=[k_("pA")])
        Q.op("dve", lambda e: e.scalar_tensor_tensor(out=w["X"][0], in0=w["pA"], scalar=sv("nbeta"), in1=w["E"], op0=ALU.mult, op1=ALU.mult),
             reads=[k_("pA"), "nbeta", k_("E")], writes=[k_("X0")])
        if n >= 16:
            Q.op("pe", lambda e: e.matmul(w["pB"], lhsT=kT, rhs=qT, start=True, stop=True), reads=[kk, kq], writes=[k_("pB")])
            Q.op("dve", lambda e: e.tensor_tensor(out=w[f"attnT{pb}"], in0=w["pB"], in1=w["ET"], op=ALU.mult),
                 reads=[k_("pB"), k_("ET")], writes=[k_(f"attnT{pb}")])
        Q.op("pe", lambda e: e.transpose(out=w["pT"][:, 0, :], in_=vT, identity=ident), reads=[kv, "ident"], writes=[k_("pT")])
        Q.op("pe", lambda e: e.transpose(out=w["pT"][:, 1, :], in_=kT, identity=ident), reads=[kk, "ident"], writes=[k_("pT")])
        Y = w[f"Y{pb}"]
        Q.op("act", lambda e: e.activation(out=Y[:, 0:128], in_=w["pT"][:, 0, :], func=AF.Copy, scale=sv("beta")),
             reads=[k_("pT"), "beta"], writes=[k_(f"Y{pb}")])
        Q.op("act", lambda e: e.activation(out=Y[:, 128:256], in_=w["pT"][:, 1, :], func=AF.Copy, scale=sv("be1")),
             reads=[k_("pT"), "be1"], writes=[k_(f"Y{pb}")])
        Q.op("act", lambda e: e.activation(out=w[f"kout{pb}"], in_=w["pT"][:, 1, :], func=AF.Copy, scale=sv("e2")),
             reads=[k_("pT"), "e2"], writes=[k_(f"kout{pb}")])
        Q.op("act", lambda e: e.copy(out=w["Yb"], in_=Y), reads=[k_(f"Y{pb}")], writes=[k_("Yb")])
        Q.op("pe", lambda e: e.transpose(out=w["pT"][:, 0, :], in_=w["X"][0], identity=ident), reads=[k_("X0"), "ident"], writes=[k_("pT")])
        Q.op("act", lambda e: e.copy(out=w["XT"][0], in_=w["pT"][:, 0, :]), reads=[k_("pT")], writes=[k_("XT0")])
        Am, Bm, Pm = w["X"][1], w["XT"][1], w["Pm"]
        Q.op("pool", lambda e: e.tensor_tensor(out=Am, in0=w["X"][0], in1=gmk[:, 7, :], op=ALU.mult), reads=[k_("X0"), "gmk"], writes=[k_("Am")])
        Q.op("pool", lambda e: e.tensor_tensor(out=Am, in0=Am, in1=ident, op=ALU.add), reads=[k_("Am"), "ident"], writes=[k_("Am")])
        Q.op("pool", lambda e: e.tensor_tensor(out=Bm, in0=w["XT"][0], in1=gmk[:, 0, :], op=ALU.mult), reads=[k_("XT0"), "gmk"], writes=[k_("Bm")])
        Q.op("pool", lambda e: e.tensor_tensor(out=Bm, in0=Bm, in1=ident, op=ALU.add), reads=[k_("Bm"), "ident"], writes=[k_("Bm")])
        for lvl in range(1, 7):
            Q.op("pool", lambda e, lvl=lvl: e.tensor_tensor(out=w["NkTs"][lvl - 1], in0=w["XT"][0], in1=gmk[:, lvl, :], op=ALU.mult),
                 reads=[k_("XT0"), "gmk"], writes=[k_(f"NkT{lvl}")])
        for lvl in range(1, 7):
            NkT_l = w["NkTs"][lvl - 1]
            Q.op("pe", lambda e, NkT_l=NkT_l: e.matmul(w["pA"], lhsT=NkT_l, rhs=Am, start=True, stop=True), reads=[k_(f"NkT{lvl}"), k_("Am")], writes=[k_("pA")])
            if lvl % 2 == 0:
                Q.op("act", lambda e: e.copy(out=Pm, in_=w["pA"]), reads=[k_("pA")], writes=[k_("Pm")])
            else:
                Q.op("dve", lambda e: e.tensor_copy(out=Pm, in_=w["pA"]), reads=[k_("pA")], writes=[k_("Pm")])
            Q.op("pe", lambda e: e.matmul(w["pB"], lhsT=Bm, rhs=Pm, start=True, stop=False), reads=[k_("Bm"), k_("Pm")], writes=[k_("pB")])
            Q.op("pe", lambda e: e.matmul(w["pB"], lhsT=ident, rhs=Am, start=False, stop=True), reads=["ident", k_("Am")], writes=[k_("pB")])
            Q.op("pe", lambda e: e.matmul(w["pC"][:, 0:128], lhsT=Pm, rhs=Bm, start=True, stop=False), reads=[k_("Bm"), k_("Pm")], writes=[k_("pC")])
            Q.op("pe", lambda e: e.matmul(w["pC"][:, 0:128], lhsT=ident, rhs=Bm, start=False, stop=True), reads=["ident", k_("Bm")], writes=[k_("pC")])
            Q.op("act", lambda e: e.copy(out=Am, in_=w["pB"]), reads=[k_("pB")], writes=[k_("Am")])
            Q.op("dve", lambda e: e.tensor_copy(out=Bm, in_=w["pC"][:, 0:128]), reads=[k_("pC")], writes=[k_("Bm")])
        Q.op("pe", lambda e: e.matmul(w["pC"], lhsT=Bm, rhs=w["Yb"], start=True, stop=True), reads=[k_("Bm"), k_("Yb")], writes=[k_("pC")])
        Q.op("dve", lambda e: e.tensor_copy(out=Y, in_=w["pC"]), reads=[k_("pC")], writes=[k_(f"Y{pb}")])
        Q.op("act", lambda e: e.copy(out=w["Yb"], in_=Y), reads=[k_(f"Y{pb}")], writes=[k_("Yb")])
        Q.op("pe", lambda e: e.transpose(out=w["pT"][:, 1, :], in_=w["Yb"][:, 128:256], identity=ident), reads=[k_("Yb"), "ident"], writes=[k_("pT")])
        Q.op("act", lambda e: e.copy(out=w[f"wT{pb}"], in_=w["pT"][:, 1, :]), reads=[k_("pT")], writes=[k_(f"wT{pb}")])

    def seq(grp, hs, n, Q):
        h = grp * 2 + hs
        w = W[hs]
        pb = n % 2
        col = n * 16 + h
        cs = slice(n * 128, (n + 1) * 128)
        qT = qkv[hs][0][:, cs]
        kq = K(hs, "qkv0")
        sv = lambda nm: sc[nm][:, col:col + 1]
        k_ = lambda nm: K(hs, nm)
        Y = w[f"Y{pb}"]
        Q.op("pe", lambda e: e.matmul(w["pA"], lhsT=w[f"wT{pb}"], rhs=w["Sb"], start=True, stop=True),
             reads=[k_(f"wT{pb}"), k_("Sb")], writes=[k_("pA")])
        Q.op("dve", lambda e: e.tensor_tensor(out=w["vnew"], in0=Y[:, 0:128], in1=w["pA"], op=ALU.subtract),
             reads=[k_("pA"), k_(f"Y{pb}")], writes=[k_("vnew")])
        if n >= 16:
            Q.op("pe", lambda e: e.matmul(w["pB"], lhsT=qT, rhs=w["Sb"], start=True, stop=True), reads=[kq, k_("Sb")], writes=[k_("pB")])
            Q.op("act", lambda e: e.activation(out=w["oS"], in_=w["pB"], func=AF.Copy, scale=sv("e1")), reads=[k_("pB"), "e1"], writes=[k_("oS")])
            Q.op("pe", lambda e: e.matmul(w["pC"][:, 0:128], lhsT=w[f"attnT{pb}"], rhs=w["vnew"], start=True, stop=True),
                 reads=[k_(f"attnT{pb}"), k_("vnew")], writes=[k_("pC")])
            Q.op("dve", lambda e: e.tensor_tensor(out=w["o"], in0=w["pC"][:, 0:128], in1=w["oS"], op=ALU.add),
                 reads=[k_("pC"), k_("oS")], writes=[k_("o")])
        Q.op("pe", lambda e: e.matmul(w["pC"][:, 128:256], lhsT=w[f"kout{pb}"], rhs=w["vnew"], start=True, stop=True),
             reads=[k_(f"kout{pb}"), k_("vnew")], writes=[k_("pC")])
        Q.op("dve", lambda e: e.scalar_tensor_tensor(out=w["S"], in0=w["S"], scalar=sv("dch"), in1=w["pC"][:, 128:256], op0=ALU.mult, op1=ALU.add),
             reads=[k_("pC"), "dch", k_("S")], writes=[k_("S")])
        Q.op("act", lambda e: e.copy(out=w["Sb"], in_=w["S"]), reads=[k_("S")], writes=[k_("Sb")])
        if n >= 16:
            Q.op("act", lambda e: e.activation(out=w["junk"], in_=w["o"], func=AF.Square, accum_out=w["ss"]), reads=[k_("o")], writes=[k_("ss"), k_("junk")])
            rsqrt_col(Q, w["ss"], w["ss"], EPS, k_("ss"), k_("ss"), mul=1.0 / 128)
            Q.op("dve", lambda e: e.tensor_scalar(out=w["on"], in0=w["o"], scalar1=w["ss"], scalar2=None, op0=ALU.mult),
                 reads=[k_("o"), k_("ss")], writes=[k_("on")])
            Q.op("pe", lambda e: e.transpose(out=w["pT"][:, 0, :], in_=w["on"], identity=ident), reads=[k_("on"), "ident"], writes=[k_("pT")])
            oc = slice((n - 16) * 128, (n - 15) * 128)
            Q.op("act", lambda e: e.copy(out=w["junk"], in_=w["pT"][:, 0, :]), reads=[k_("pT")], writes=[k_("junk")])
            Q.op("dve", lambda e: e.scalar_tensor_tensor(out=OGb[hs][:, oc], in0=w["junk"], scalar=normw, in1=zs[hs][:, oc],
                                                         op0=ALU.mult, op1=ALU.mult),
                 reads=[k_("junk"), "normw", k_("zs")], writes=[k_("OGb")])

    class Rec:
        def __init__(self):
            self.items = []

        def op(self, *a, **kw):
            self.items.append((a, kw))

    def interleave(fn, grp, n):
        recs = []
        for hs in range(2):
            r = Rec()
            fn(grp, hs, n, r)
            recs.append(r.items)
        for k in range(max(len(r) for r in recs)):
            for r in recs:
                if k < len(r):
                    a, kw = r[k]
                    P.op(*a, **kw)

    for grp in groups:
        preprocess(grp)
        interleave(pre, grp, 0)
        for n in range(NT):
            if n + 1 < NT:
                interleave(pre, grp, n + 1)
            interleave(seq, grp, n)
        for hs in range(2):
            h = grp * 2 + hs
            P.dma(OG_d[h * 128:(h + 1) * 128, :], OGb[hs], reads=[K(hs, "OGb")])
    st.close()


def nsa_host_consts(half):
    import ml_dtypes
    bf = ml_dtypes.bfloat16
    c = {}
    n = np.arange(256)
    q = np.arange(2048)
    cm = np.where((16 * n[:, None] + 31) <= (OWN0 + q[None, :]), 0.0, NEG).astype(np.float32)
    c["cmask"] = np.ascontiguousarray(cm.reshape(2, 128, 2048).transpose(1, 0, 2)).astype(bf)
    nvalid = (n <= 254) & ((16 * n >= OWN0) if half == 0 else True)
    c["cbias"] = np.ascontiguousarray(np.where(nvalid, 0.0, NEG).astype(np.float32).reshape(2, 128).T)
    pos = np.arange(TL)
    kvalid = (pos >= OWN0) if half == 0 else np.ones(TL, bool)
    c["kbias"] = np.ascontiguousarray(np.where(kvalid, 0.0, NEG).astype(np.float32).reshape(32, 128).T)
    key = np.arange(128)
    dk = np.arange(4)
    qq = np.arange(512)
    rel = dk[None, :, None] * 128 + key[:, None, None]
    c["causm"] = np.where(rel <= qq[None, None, :], 0.0, NEG).astype(bf)
    c["winm"] = np.where(rel > qq[None, None, :], 0.0, NEG).astype(bf)
    j = np.arange(64)
    kt = np.arange(32)
    c["esel"] = (j[:, None, None] == (2 * kt[None, :, None] + key[None, None, :] // 64)).astype(bf)
    cmp_start = np.arange(255) * 16
    sel_start = np.arange(64) * 64
    ov = np.minimum(cmp_start[:, None] + 32, sel_start[None, :] + 64) - np.maximum(cmp_start[:, None], sel_start[None, :])
    sm = np.zeros((256, 65), np.float32)
    sm[:255, :64] = np.clip(ov, 0, None) / 16.0
    sm[:, 64] = 1.0
    c["selmap"] = np.ascontiguousarray(sm.reshape(2, 128, 65).transpose(1, 0, 2)).astype(bf)
    t = OWN0 + q
    cur = t // 64
    blk0 = 32 if half == 0 else 0
    forced = (j[None, :] == blk0) | (j[None, :] == cur[:, None]) | (j[None, :] == cur[:, None] - 1)
    causal = (j[None, :] * 64 <= t[:, None]) & (j[None, :] >= blk0)
    use_imp = causal & ~forced
    selm = use_imp.astype(np.float32)
    sela = np.where(forced, 1e6, np.where(causal, 0.0, -1e30)).astype(np.float32)
    c["selm"] = np.ascontiguousarray(selm.reshape(16, 128, 64).transpose(1, 0, 2))
    c["sela"] = np.ascontiguousarray(sela.reshape(16, 128, 64).transpose(1, 0, 2))
    r = np.arange(48)
    c["selrow"] = np.ascontiguousarray(np.broadcast_to((r[:, None, None] == r[None, :, None]), (48, 48, 128))).astype(bf)
    return c


NSA_CONST_SPECS = [("cmask", [128, 2, 2048], BF16), ("cbias", [128, 2], F32), ("kbias", [128, 32], F32),
                   ("causm", [128, 4, 512], BF16), ("winm", [128, 4, 512], BF16), ("esel", [64, 32, 128], BF16),
                   ("selmap", [128, 2, 65], BF16), ("selm", [128, 16, 64], F32), ("sela", [128, 16, 64], F32),
                   ("selrow", [48, 48, 128], BF16)]


def stage_nsa(nc, P, cst, PT_d, KD, w1k_d, w2k_d, pek_d, w1v_d, w2v_d, pev_d, ON_d, ngroups=4, nqc=4):
    st = Stage(nc, P)
    ident, ones = cst["ident"], cst["ones"]
    SCALE = float(128 ** -0.5)
    TINY = 1e-30
    C = {}
    for nm, shp, dt in NSA_CONST_SPECS:
        C[nm] = st.sb("c_" + nm, shp, dt)
        P.dma(C[nm], KD[nm], writes=["c_" + nm])
    gst = st.sb("gst", [48, 2048], F32)
    gsig = st.sb("gsig", [48, 2048], BF16)
    P.dma(gst, PT_d[C_NG:C_NG + 48, OWN0:TL], writes=["gst"])
    P.op("act", lambda e: e.activation(out=gsig, in_=gst, func=AF.Sigmoid), reads=["gst"], writes=["gsig"])
    pS = [st.ps(f"pS{i}", [128, 512], F32) for i in range(2)]
    pO = st.ps("pO", [128, 512], F32)
    pD = st.ps("pD", [128, 512], F32)
    pG = st.ps("pG", [128, 512], F32)
    pI = st.ps("pI", [128, 4, 128], F32)
    pTm = st.ps("pTm", [128, 4, 128], BF16)
    pO2 = st.ps("pO2", [128, 512], F32)
    pD2 = pI.rearrange("p a b -> p (a b)")
    ACC = [(pO, "pO", pD, "pD"), (pO2, "pO2", pD2, "pI")]
    cw_ = {}
    for tag, w1d, w2d, ped in (("k", w1k_d, w2k_d, pek_d), ("v", w1v_d, w2v_d, pev_d)):
        w1 = st.sb("w1" + tag, [128, 32, 128], BF16)
        w2 = st.sb("w2" + tag, [128, 128], BF16)
        peT = st.sb("peT" + tag, [128, 32], BF16)
        hb = st.sb("hb" + tag, [128, 1], F32)
        P.dma(w1, w1d.rearrange("l d e -> d l e"), writes=["w1" + tag], q="pool")
        P.dma(w2, w2d, writes=["w2" + tag], q="pool")
        P.dma(peT, ped, writes=["peT" + tag], q="pool")
        for l in range(32):
            P.op("pe", lambda e, l=l, w1=w1, peT=peT: e.matmul(pS[0][:, 0:1], lhsT=w1[:, l, :], rhs=peT[:, l:l + 1],
                                                              start=(l == 0), stop=(l == 31)),
                 reads=["w1" + tag, "peT" + tag], writes=["pS0"])
        P.op("act", lambda e, hb=hb: e.copy(out=hb, in_=pS[0][:, 0:1]), reads=["pS0"], writes=["hb" + tag])
        cw_[tag] = (w1, w2, hb)
    kcT = st.sb("kcT", [128, TL], BF16)
    vcT = st.sb("vcT", [128, TL], BF16)
    ksT = st.sb("ksT", [128, TL], BF16)
    kwT = st.sb("kwT", [128, TL], BF16)
    vtmp = st.sb("vtmp", [128, TL], BF16)
    vs_tok = st.sb("vs_tok", [128, 32, 128], BF16)
    vw_tok = st.sb("vw_tok", [128, 32, 128], BF16)
    qT = [st.sb(f"nq{i}", [128, 2048], BF16) for i in range(4)]
    hid = st.sb("hid", [128, 256], BF16)
    kcmpT = st.sb("kcmpT", [128, 256], BF16)
    vcmpT = st.sb("vcmpT", [128, 256], BF16)
    vcmp_tok = st.sb("vcmp_tok", [128, 2, 128], BF16)
    Pc = [[st.sb(f"Pc{h}_{j}", [128, 512], BF16) for j in range(2)] for h in range(4)]
    PTl = [st.sb(f"PTl{i}", [128, 512], BF16) for i in range(3)]
    accO = [st.sb(f"accO{h}", [128, 512], F32) for h in range(4)]
    accI = st.sb("accI", [128, 4, 64], F32)
    rr = st.sb("rr", [128, 512], F32)
    tmpo = st.sb("tmpo", [128, 512], F32)
    rc = st.sb("rc", [128, 1], F32)
    score = st.sb("score", [128, 64], F32)
    work = st.sb("work", [128, 64], F32)
    mx8 = st.sb("mx8", [128, 8], F32)
    madd = st.sb("madd", [128, 64], BF16)
    maddT = st.sb("maddT", [64, 512], BF16)
    osb = st.sb("osb", [128, 512], BF16)
    P.op("pool", lambda e: e.memset(hid, 0.0), writes=["hid"])

    def combine(h, first, aset=0):
        pO_, kO, pD_, kD = ACC[aset]
        P.op("dve", lambda e: e.tensor_scalar(out=rr, in0=pD_, scalar1=TINY, scalar2=None, op0=ALU.max), reads=[kD], writes=["rr"])
        P.op("dve", lambda e: e.reciprocal(out=rr, in_=rr), reads=["rr"], writes=["rr"])
        P.op("dve", lambda e: e.tensor_tensor(out=rr, in0=rr, in1=pG, op=ALU.mult), reads=["rr", "pG"], writes=["rr"])
        if first:
            P.op("dve", lambda e: e.tensor_tensor(out=accO[h], in0=pO_, in1=rr, op=ALU.mult), reads=[kO, "rr"], writes=[f"accO{h}"])
        else:
            P.op("dve", lambda e: e.tensor_tensor(out=tmpo, in0=pO_, in1=rr, op=ALU.mult), reads=[kO, "rr"], writes=["tmpo"])
            P.op("dve", lambda e: e.tensor_tensor(out=accO[h], in0=accO[h], in1=tmpo, op=ALU.add), reads=["tmpo", f"accO{h}"], writes=[f"accO{h}"])

    def compress(tag, srcT, srckey, dstT, dstkey):
        w1, w2, hb = cw_[tag]
        for l in range(32):
            v3 = srcT.rearrange("p (n s) -> p n s", s=16)
            rhs_ = v3[:, 0:255, l] if l < 16 else v3[:, 1:256, l - 16]
            P.op("pe", lambda e, l=l, rhs_=rhs_: e.matmul(pS[0][:, 0:255], lhsT=w1[:, l, :], rhs=rhs_,
                                               start=(l == 0), stop=(l == 31)),
                 reads=["w1" + tag, srckey], writes=["pS0"])
        P.op("act", lambda e: e.activation(out=hid[:, 0:255], in_=pS[0][:, 0:255], func=AF.Silu, bias=hb), reads=["pS0", "hb" + tag], writes=["hid"])
        P.op("pe", lambda e: e.matmul(pS[1][:, 0:256], lhsT=w2, rhs=hid, start=True, stop=True), reads=["w2" + tag, "hid"], writes=["pS1"])
        P.op("act", lambda e: e.copy(out=dstT, in_=pS[1][:, 0:256]), reads=["pS1"], writes=[dstkey])

    def to_tok(dst, dstkey, ntile, src, srckey):
        for g4 in range(0, ntile, 4):
            nn = min(4, ntile - g4)
            for j in range(nn):
                P.op("pe", lambda e, j=j, g4=g4: e.transpose(out=pTm[:, j, :], in_=src[:, (g4 + j) * 128:(g4 + j + 1) * 128], identity=ident),
                     reads=[srckey, "ident"], writes=["pTm"])
            P.op("act", lambda e, g4=g4, nn=nn: e.copy(out=dst[:, g4:g4 + nn, :], in_=pTm[:, 0:nn, :]), reads=["pTm"], writes=[dstkey])

    pcount = [0]
    scount = [0]

    def attend(h, qs, kT, kTkey, vtok, vkey, kts, maskfn, bias_fn, aset=0):
        n = len(kts)
        prev = None
        pO_, kO, pD_, kD = ACC[aset]

        def pv(pt, pi, kt, i):
            P.op("pe", lambda e: e.matmul(pO_, lhsT=vtok[:, kt, :], rhs=pt, start=(i == 0), stop=(i == n - 1)),
                 reads=[vkey, f"PTl{pi}"], writes=[kO])
            P.op("pe", lambda e: e.matmul(pD_, lhsT=ones, rhs=pt, start=(i == 0), stop=(i == n - 1)),
                 reads=["ones", f"PTl{pi}"], writes=[kD])

        for i, kt in enumerate(kts):
            sb_ = scount[0] % 2
            scount[0] += 1
            ps = pS[sb_]
            extra = maskfn(kt)
            P.op("pe", lambda e, qs=qs, kt=kt, ps=ps, extra=extra: e.matmul(ps, lhsT=kT[:, kt * 128:(kt + 1) * 128], rhs=qT[h][:, qs],
                                                                    start=True, stop=(len(extra) == 0)),
                 reads=[kTkey, f"nq{h}"], writes=[f"pS{sb_}"])
            for mi, (l_, r_, keys_) in enumerate(extra):
                P.op("pe", lambda e, ps=ps, l_=l_, r_=r_, last=(mi == len(extra) - 1): e.matmul(ps, lhsT=l_, rhs=r_, start=False, stop=last),
                     reads=keys_, writes=[f"pS{sb_}"])
            pi = pcount[0] % 3
            pcount[0] += 1
            pt = PTl[pi]
            P.op("act", lambda e, ps=ps, pt=pt, kt=kt: e.activation(out=pt, in_=ps, func=AF.Exp, scale=SCALE, bias=bias_fn(kt)),
                 reads=[f"pS{sb_}", "c_kbias", "c_cbias"], writes=[f"PTl{pi}"])
            if prev is not None:
                pv(*prev)
            prev = (pt, pi, kt, i)
        pv(*prev)

    for g in range(ngroups):
        rows = lambda jj: slice(C_NKV + jj * 512 + g * 128, C_NKV + jj * 512 + (g + 1) * 128)
        P.dma(kcT, PT_d[rows(0), 0:TL], writes=["kcT"], q="pool")
        P.dma(vcT, PT_d[rows(1), 0:TL], writes=["vcT"], q="pool")
        P.dma(ksT, PT_d[rows(2), 0:TL], writes=["ksT"], q="pool")
        P.dma(kwT, PT_d[rows(4), 0:TL], writes=["kwT"], q="pool")
        P.dma(vtmp, PT_d[rows(3), 0:TL], writes=["vtmp"], q="pool")
        to_tok(vs_tok, "vs_tok", 32, vtmp, "vtmp")
        P.dma(vtmp, PT_d[rows(5), 0:TL], writes=["vtmp"], q="pool")
        to_tok(vw_tok, "vw_tok", 32, vtmp, "vtmp")
        for hg in range(4):
            hq = g * 4 + hg
            P.dma(qT[hg], PT_d[C_NQ + hq * 128:C_NQ + (hq + 1) * 128, OWN0:TL], writes=[f"nq{hg}"], q="pool")
        compress("k", kcT, "kcT", kcmpT, "kcmpT")
        compress("v", vcT, "vcT", vcmpT, "vcmpT")
        to_tok(vcmp_tok, "vcmp_tok", 2, vcmpT, "vcmpT")
        for qc in range(nqc):
            qs = slice(qc * 512, (qc + 1) * 512)
            for hg in range(4):
                hq = g * 4 + hg
                for j in range(2):
                    sb_ = scount[0] % 2
                    scount[0] += 1
                    ps = pS[sb_]
                    P.op("pe", lambda e, qs=qs, j=j, ps=ps, hg=hg: e.matmul(ps, lhsT=kcmpT[:, j * 128:(j + 1) * 128], rhs=qT[hg][:, qs], start=True, stop=False),
                         reads=["kcmpT", f"nq{hg}"], writes=[f"pS{sb_}"])
                    P.op("pe", lambda e, qs=qs, j=j, ps=ps: e.matmul(ps, lhsT=ident, rhs=C["cmask"][:, j, qs], start=False, stop=True),
                         reads=["ident", "c_cmask"], writes=[f"pS{sb_}"])
                    P.op("act", lambda e, j=j, ps=ps, hg=hg: e.activation(out=Pc[hg][j], in_=ps, func=AF.Exp, scale=SCALE, bias=C["cbias"][:, j:j + 1]),
                         reads=[f"pS{sb_}", "c_cbias"], writes=[f"Pc{hg}_{j}"])
                for j in range(2):
                    P.op("pe", lambda e, j=j, hg=hg: e.matmul(pO, lhsT=vcmp_tok[:, j, :], rhs=Pc[hg][j], start=(j == 0), stop=(j == 1)),
                         reads=["vcmp_tok", f"Pc{hg}_{j}"], writes=["pO"])
                for j in range(2):
                    P.op("pe", lambda e, j=j, hg=hg: e.matmul(pD, lhsT=ones, rhs=Pc[hg][j], start=(j == 0), stop=(j == 1)),
                         reads=["ones", f"Pc{hg}_{j}"], writes=["pD"])
                P.op("pe", lambda e, qs=qs, hq=hq: e.matmul(pG, lhsT=C["selrow"][:, hq * 3 + 0, :], rhs=gsig[:, qs], start=True, stop=True),
                     reads=["c_selrow", "gsig"], writes=["pG"])
                combine(hg, True)
                for sub in range(4):
                    for j in range(2):
                        P.op("pe", lambda e, j=j, hg=hg, sub=sub: e.matmul(pI[:, sub, 0:65], lhsT=Pc[hg][j][:, sub * 128:(sub + 1) * 128],
                                                                         rhs=C["selmap"][:, j, :], start=(j == 0), stop=(j == 1)),
                             reads=["c_selmap", f"Pc{hg}_{j}"], writes=["pI"])
                    P.op("dve", lambda e, sub=sub: e.tensor_scalar(out=rc, in0=pI[:, sub, 64:65], scalar1=TINY, scalar2=None, op0=ALU.max),
                         reads=["pI"], writes=["rc"])
                    P.op("dve", lambda e: e.reciprocal(out=rc, in_=rc), reads=["rc"], writes=["rc"])
                    if hg == 0:
                        P.op("dve", lambda e, sub=sub: e.tensor_scalar(out=accI[:, sub, :], in0=pI[:, sub, 0:64], scalar1=rc, scalar2=None, op0=ALU.mult),
                             reads=["pI", "rc"], writes=["accI"])
                    else:
                        P.op("dve", lambda e, sub=sub: e.scalar_tensor_tensor(out=accI[:, sub, :], in0=pI[:, sub, 0:64], scalar=rc, in1=accI[:, sub, :],
                                                                             op0=ALU.mult, op1=ALU.add),
                             reads=["pI", "rc", "accI"], writes=["accI"])
            for sub in range(4):
                ts_ = qc * 4 + sub
                P.op("dve", lambda e, sub=sub, ts_=ts_: e.tensor_tensor(out=score, in0=accI[:, sub, :], in1=C["selm"][:, ts_, :], op=ALU.mult),
                     reads=["accI", "c_selm"], writes=["score"])
                P.op("dve", lambda e, ts_=ts_: e.tensor_tensor(out=score, in0=score, in1=C["sela"][:, ts_, :], op=ALU.add),
                     reads=["score", "c_sela"], writes=["score"])
                P.op("dve", lambda e: e.max(out=mx8, in_=score), reads=["score"], writes=["mx8"])
                P.op("dve", lambda e: e.match_replace(out=work, in_to_replace=mx8, in_values=score, imm_value=-3.0e38),
                     reads=["score", "mx8"], writes=["work"])
                P.op("dve", lambda e: e.max(out=mx8, in_=work), reads=["work"], writes=["mx8"])
                P.op("dve", lambda e: e.tensor_scalar(out=work, in0=score, scalar1=mx8[:, 7:8], scalar2=None, op0=ALU.is_ge),
                     reads=["score", "mx8"], writes=["work"])
                P.op("dve", lambda e: e.tensor_scalar(out=madd, in0=work, scalar1=1.0, scalar2=-NEG, op0=ALU.subtract, op1=ALU.mult),
                     reads=["work"], writes=["madd"])
                P.op("pe", lambda e: e.transpose(out=pTm[0:64, 0, :], in_=madd, identity=ident), reads=["madd", "ident"], writes=["pTm"])
                P.op("act", lambda e, sub=sub: e.copy(out=maddT[:, sub * 128:(sub + 1) * 128], in_=pTm[0:64, 0, :]), reads=["pTm"], writes=["maddT"])
            kt_diag0 = 16 + 4 * qc
            for hg in range(4):
                hq = g * 4 + hg

                def sel_mask(kt):
                    ex = [(C["esel"][:, kt, :], maddT, ["c_esel", "maddT"])]
                    if kt >= kt_diag0:
                        ex.append((ident, C["causm"][:, kt - kt_diag0, :], ["ident", "c_causm"]))
                    return ex

                def win_mask(kt):
                    if kt >= kt_diag0:
                        return [(ident, C["causm"][:, kt - kt_diag0, :], ["ident", "c_causm"])]
                    return [(ident, C["winm"][:, kt - (kt_diag0 - 4), :], ["ident", "c_winm"])]

                kb = lambda kt: C["kbias"][:, kt:kt + 1]
                attend(hg, qs, ksT, "ksT", vs_tok, "vs_tok", list(range(0, kt_diag0 + 4)), sel_mask, kb, aset=1)
                P.op("pe", lambda e, qs=qs, hq=hq: e.matmul(pG, lhsT=C["selrow"][:, hq * 3 + 1, :], rhs=gsig[:, qs], start=True, stop=True),
                     reads=["c_selrow", "gsig"], writes=["pG"])
                combine(hg, False, aset=1)
                attend(hg, qs, kwT, "kwT", vw_tok, "vw_tok", list(range(kt_diag0 - 4, kt_diag0 + 4)), win_mask, kb, aset=0)
                P.op("pe", lambda e, qs=qs, hq=hq: e.matmul(pG, lhsT=C["selrow"][:, hq * 3 + 2, :], rhs=gsig[:, qs], start=True, stop=True),
                     reads=["c_selrow", "gsig"], writes=["pG"])
                combine(hg, False)
                P.op("act", lambda e, hg=hg: e.copy(out=osb, in_=accO[hg]), reads=[f"accO{hg}"], writes=["osb"])
                P.dma(ON_d[hq * 128:(hq + 1) * 128, qs], osb, reads=["osb"])
    st.close()


def stage_merge(nc, P, cst, PT_d, OG_d, ON_d, wa_d, wb_d, wo_d, Hd, nblk=4):
    st = Stage(nc, P)
    ogT = st.sb("ogT", [128, 16, 512], BF16)
    onT = st.sb("onT", [128, 16, 512], BF16)
    mT = st.sb("mT", [128, 32, 512], BF16)
    wa = [st.sb(f"wa{i}", [128, 16, 256], BF16) for i in range(2)]
    wb = [st.sb(f"wb{i}", [128, 16, 256], BF16) for i in range(2)]
    ga = [st.sb(f"ga{i}", [128, 512], F32) for i in range(2)]
    gb = [st.sb(f"gb{i}", [128, 512], F32) for i in range(2)]
    m1 = st.sb("m1", [128, 512], F32)
    m2 = st.sb("m2", [128, 512], F32)
    wo = [st.sb(f"wo{i}", [128, 32, 256], BF16) for i in range(2)]
    hs = [st.sb(f"hs{i}", [128, 256], F32) for i in range(2)]
    pA = [st.ps(f"mpA{i}", [128, 512], F32) for i in range(2)]
    pB = [st.ps(f"mpB{i}", [128, 512], F32) for i in range(2)]
    pH = [st.ps(f"mpH{i}", [128, 512], F32) for i in range(2)]
    wc = 0
    gc_ = 0
    woc = 0
    hc = 0
    for tb in range(nblk):
        ts_ = slice(tb * 512, (tb + 1) * 512)
        P.dma(ogT, OG_d[:, ts_].rearrange("(c p) t -> p c t", p=128), writes=["ogT"])
        P.dma(onT, ON_d[:, ts_].rearrange("(c p) t -> p c t", p=128), writes=["onT"])
        for c2 in range(16):
            j = wc % 2
            wc += 1
            P.dma(wa[j], wa_d[:, c2 * 256:(c2 + 1) * 256].rearrange("(k p) n -> p k n", p=128), writes=[f"wa{j}"], q="pool")
            P.dma(wb[j], wb_d[:, c2 * 256:(c2 + 1) * 256].rearrange("(k p) n -> p k n", p=128), writes=[f"wb{j}"], q="pool")
            for sub in range(2):
                cc = c2 * 2 + sub
                i = gc_ % 2
                gc_ += 1
                P.dma(ga[i], PT_d[C_MG + cc * 128:C_MG + (cc + 1) * 128, OWN0 + tb * 512:OWN0 + (tb + 1) * 512], writes=[f"ga{i}"])
                P.dma(gb[i], PT_d[C_MG + 4096 + cc * 128:C_MG + 4096 + (cc + 1) * 128, OWN0 + tb * 512:OWN0 + (tb + 1) * 512], writes=[f"gb{i}"])
                P.op("act", lambda e, i=i: e.activation(out=ga[i], in_=ga[i], func=AF.Sigmoid), reads=[f"ga{i}"], writes=[f"ga{i}"])
                P.op("act", lambda e, i=i: e.activation(out=gb[i], in_=gb[i], func=AF.Sigmoid), reads=[f"gb{i}"], writes=[f"gb{i}"])
                for k in range(16):
                    P.op("pe", lambda e, k=k, i=i, j=j, sub=sub: e.matmul(pA[i], lhsT=wa[j][:, k, sub * 128:(sub + 1) * 128], rhs=ogT[:, k, :],
                                                                        start=(k == 0), stop=(k == 15)),
                         reads=[f"wa{j}", "ogT"], writes=[f"mpA{i}"])
                for k in range(16):
                    P.op("pe", lambda e, k=k, i=i, j=j, sub=sub: e.matmul(pB[i], lhsT=wb[j][:, k, sub * 128:(sub + 1) * 128], rhs=onT[:, k, :],
                                                                        start=(k == 0), stop=(k == 15)),
                         reads=[f"wb{j}", "onT"], writes=[f"mpB{i}"])
                P.op("dve", lambda e, i=i: e.tensor_tensor(out=m1, in0=pA[i], in1=ga[i], op=ALU.mult), reads=[f"mpA{i}", f"ga{i}"], writes=["m1"])
                P.op("dve", lambda e, i=i: e.tensor_tensor(out=m2, in0=pB[i], in1=gb[i], op=ALU.mult), reads=[f"mpB{i}", f"gb{i}"], writes=["m2"])
                P.op("dve", lambda e, cc=cc: e.tensor_tensor(out=mT[:, cc, :], in0=m1, in1=m2, op=ALU.add), reads=["m1", "m2"], writes=[f"mT{cc}"])
        for ct in range(16):
            j = woc % 2
            woc += 1
            P.dma(wo[j], wo_d[:, ct * 256:(ct + 1) * 256].rearrange("(k p) n -> p k n", p=128), writes=[f"wo{j}"], q="pool")
            for tt in range(4):
                i = hc % 2
                hc += 1
                for k in range(32):
                    P.op("pe", lambda e, k=k, i=i, j=j, tt=tt: e.matmul(pH[i][:, 0:256], lhsT=mT[:, k, tt * 128:(tt + 1) * 128], rhs=wo[j][:, k, :],
                                                                      start=(k == 0), stop=(k == 31)),
                         reads=[f"wo{j}", f"mT{k}"], writes=[f"mpH{i}"])
                evac(P, i, hs[i], pH[i][:, 0:256], [f"mpH{i}"], [f"hs{i}"])
                r0 = tb * 512 + tt * 128
                P.dma(Hd[r0:r0 + 128, ct * 256:(ct + 1) * 256], hs[i], reads=[f"hs{i}"])
    st.close()


CAP = 128


def stage_route(nc, P, cst, x_d, Hd, gffn_d, wr_d, br_d, eb_d, Xd, slots_i, wts):
    st = Stage(nc, P)
    identf, ones, onesf = cst["identf"], cst["ones"], cst["onesf"]
    xt = [st.sb(f"rx{i}", [128, D], F32) for i in range(2)]
    hd = [st.sb(f"rh{i}", [128, D], F32) for i in range(2)]
    hn2 = [st.sb(f"rhn{i}", [128, D], F32) for i in range(2)]
    hnb = [st.sb(f"rhnb{i}", [128, D], BF16) for i in range(2)]
    junk2 = [st.sb(f"rjunk{i}", [128, D], BF16) for i in range(2)]
    gf = st.sb("rgf", [128, D], F32)
    hnT2 = [st.sb(f"rhnT{i}", [128, 32, 128], F32) for i in range(2)]
    wr = st.sb("rwr", [128, 32, 72], F32)
    br = st.sb("rbr", [128, 72], F32)
    eb = st.sb("reb", [128, 64], F32)
    sut = st.sb("rsut", [128, 128], BF16)
    cnt = st.sb("rcnt", [128, 64], F32)
    zt = st.sb("rzt", [128, 2048], BF16)
    sm2 = [{}, {}]
    for i_ in range(2):
        for nm, w in (("ss", 1), ("lg", 72), ("mxg", 1), ("ohg", 8), ("eg", 8), ("sumg", 1), ("t3", 64), ("es", 8), ("mx8", 8),
                      ("oh1", 8), ("sel2", 8), ("oh2", 8), ("dd", 1), ("w1", 1), ("w2", 1), ("o1", 64), ("o2", 64), ("posf", 64),
                      ("tmp", 64), ("s1", 1), ("s2", 1)):
            sm2[i_][nm] = st.sb(f"r{i_}_" + nm, [128, w], F32)
    oh64_2 = [st.sb(f"r_oh64_{i_}", [128, 64], BF16) for i_ in range(2)]
    pT2 = [st.ps(f"rpT{i_}", [128, 4, 128], F32) for i_ in range(2)]
    pL2 = [st.ps(f"rpL{i_}", [128, 512], F32)[:, 0:72] for i_ in range(2)]
    pP2 = [st.ps(f"rpP{i_}", [128, 512], F32)[:, 0:64] for i_ in range(2)]
    pC2 = [st.ps(f"rpC{i_}", [128, 512], F32)[:, 0:64] for i_ in range(2)]

    P.dma(gf, gffn_d.partition_broadcast(128), writes=["rgf"])
    P.dma(wr, wr_d, writes=["rwr"])
    P.dma(br, br_d.partition_broadcast(128), writes=["rbr"])
    P.dma(eb, eb_d.partition_broadcast(128), writes=["reb"])
    P.op("pool", lambda e: e.memset(cnt, 0.0), writes=["rcnt"])
    P.op("pool", lambda e: e.affine_select(out=sut, in_=ones, pattern=[[1, 128]], compare_op=ALU.is_gt, fill=0.0,
                                           base=0, channel_multiplier=-1), reads=["ones"], writes=["rsut"])
    P.op("pool", lambda e: e.memset(zt, 0.0), writes=["rzt"])
    Xz = Xd.rearrange("(a p) (b n) -> a b p n", p=128, n=2048)
    zero_done = []
    for a in range(64):
        for b_ in range(2):
            P.dma(Xz[a, b_], zt, reads=["rzt"], writes=[f"Xz{a}_{b_}"])
            zero_done.append(f"Xz{a}_{b_}")

    def route_tile(tt, Q):
        i = tt % 2
        hn, junk, hnT, sm, oh64 = hn2[i], junk2[i], hnT2[i], sm2[i], oh64_2[i]
        pT, pL, pP, pC = pT2[i], pL2[i], pP2[i], pC2[i]

        def v(nm):
            return sm[nm]
        r0 = tt * 128
        Q.dma(xt[i], x_d[OWN0 + r0:OWN0 + r0 + 128, :], writes=[f"rx{i}"])
        Q.dma(hd[i], Hd[r0:r0 + 128, :], writes=[f"rh{i}"])
        Q.op("dve", lambda e, i=i: e.tensor_tensor(out=hd[i], in0=hd[i], in1=xt[i], op=ALU.add), reads=[f"rx{i}", f"rh{i}"], writes=[f"rh{i}"])
        Q.dma(Hd[r0:r0 + 128, :], hd[i], reads=[f"rh{i}"])
        Q.op("act", lambda e, i=i: e.activation(out=junk, in_=hd[i], func=AF.Square, accum_out=v("ss")), reads=[f"rh{i}"], writes=["rjunk", "r_ss"])
        rsqrt_col(Q, v("ss"), v("ss"), EPS, "r_ss", "r_ss", mul=1.0 / D)
        Q.op("dve", lambda e, i=i: e.scalar_tensor_tensor(out=hn, in0=hd[i], scalar=v("ss"), in1=gf, op0=ALU.mult, op1=ALU.mult),
             reads=[f"rh{i}", "r_ss", "rgf"], writes=["rhn"])
        Q.op("act", lambda e, i=i: e.copy(out=hnb[i], in_=hn), reads=["rhn"], writes=[f"rhnb{i}"])
        for g4 in range(8):
            for j in range(4):
                c = g4 * 4 + j
                Q.op("pe", lambda e, c=c, j=j: e.transpose(out=pT[:, j, :], in_=hn[:, c * 128:(c + 1) * 128], identity=identf),
                     reads=["rhn", "identf"], writes=["rpT"])
            Q.op("act", lambda e, g4=g4: e.copy(out=hnT[:, g4 * 4:(g4 + 1) * 4, :], in_=pT), reads=["rpT"], writes=[f"rhnT{g4}"])
        for c in range(32):
            Q.op("pe", lambda e, c=c: e.matmul(pL, lhsT=hnT[:, c, :], rhs=wr[:, c, :], start=(c == 0), stop=(c == 31)),
                 reads=[f"rhnT{c // 4}", "rwr"], writes=["rpL"])
        Q.op("dve", lambda e: e.tensor_tensor(out=v("lg"), in0=pL, in1=br, op=ALU.add), reads=["rpL", "rbr"], writes=["r_lg"])
        lgG = v("lg")[:, 0:8]
        le3 = v("lg")[:, 8:72].rearrange("p (g e) -> p g e", e=8)
        t3 = v("t3").rearrange("p (g e) -> p g e", e=8)
        o1 = v("o1").rearrange("p (g e) -> p g e", e=8)
        o2 = v("o2").rearrange("p (g e) -> p g e", e=8)
        D_ = "dve"
        Q.op(D_, lambda e: e.reduce_max(out=v("mxg"), in_=lgG, axis=AX.X), reads=["r_lg"], writes=["r_mxg"])
        Q.op(D_, lambda e: e.tensor_scalar(out=v("ohg"), in0=lgG, scalar1=v("mxg"), scalar2=None, op0=ALU.is_ge), reads=["r_lg", "r_mxg"], writes=["r_ohg"])
        Q.op(D_, lambda e: e.tensor_scalar(out=v("mxg"), in0=v("mxg"), scalar1=-1.0, scalar2=None, op0=ALU.mult), reads=["r_mxg", "r_ohg"], writes=["r_mxg"])
        Q.op("act", lambda e: e.activation(out=v("eg"), in_=lgG, func=AF.Exp, bias=v("mxg"), accum_out=v("sumg")), reads=["r_lg", "r_mxg"], writes=["r_eg", "r_sumg"])
        Q.op(D_, lambda e: e.reciprocal(out=v("sumg"), in_=v("sumg")), reads=["r_sumg"], writes=["r_sumg"])
        Q.op(D_, lambda e: e.tensor_tensor(out=t3, in0=le3, in1=v("ohg").unsqueeze(2).to_broadcast([128, 8, 8]), op=ALU.mult),
             reads=["r_lg", "r_ohg"], writes=["r_t3"])
        Q.op(D_, lambda e: e.tensor_reduce(out=v("es"), in_=t3.rearrange("p g e -> p e g"), axis=AX.X, op=ALU.add), reads=["r_t3"], writes=["r_es"])
        Q.op(D_, lambda e: e.max(out=v("mx8"), in_=v("es")), reads=["r_es"], writes=["r_mx8"])
        Q.op(D_, lambda e: e.tensor_scalar(out=v("oh1"), in0=v("es"), scalar1=v("mx8")[:, 0:1], scalar2=None, op0=ALU.is_ge), reads=["r_es", "r_mx8"], writes=["r_oh1"])
        Q.op(D_, lambda e: e.tensor_scalar(out=v("sel2"), in0=v("es"), scalar1=v("mx8")[:, 1:2], scalar2=None, op0=ALU.is_ge), reads=["r_es", "r_mx8"], writes=["r_sel2"])
        Q.op(D_, lambda e: e.tensor_tensor(out=v("oh2"), in0=v("sel2"), in1=v("oh1"), op=ALU.subtract), reads=["r_sel2", "r_oh1"], writes=["r_oh2"])
        Q.op(D_, lambda e: e.tensor_tensor(out=v("dd"), in0=v("mx8")[:, 1:2], in1=v("mx8")[:, 0:1], op=ALU.subtract), reads=["r_mx8"], writes=["r_dd"])
        Q.op("act", lambda e: e.activation(out=v("dd"), in_=v("dd"), func=AF.Exp), reads=["r_dd"], writes=["r_dd"])
        Q.op(D_, lambda e: e.tensor_scalar(out=v("w1"), in0=v("dd"), scalar1=1.0, scalar2=None, op0=ALU.add), reads=["r_dd"], writes=["r_w1"])
        Q.op(D_, lambda e: e.reciprocal(out=v("w1"), in_=v("w1")), reads=["r_w1"], writes=["r_w1"])
        Q.op(D_, lambda e: e.tensor_tensor(out=v("w2"), in0=v("dd"), in1=v("w1"), op=ALU.mult), reads=["r_dd", "r_w1"], writes=["r_w2"])
        Q.op(D_, lambda e, tt=tt: e.tensor_tensor(out=wts[:, tt, 0:1], in0=v("w1"), in1=v("sumg"), op=ALU.mult), reads=["r_w1", "r_sumg"], writes=[f"wts{tt}"])
        Q.op(D_, lambda e, tt=tt: e.tensor_tensor(out=wts[:, tt, 1:2], in0=v("w2"), in1=v("sumg"), op=ALU.mult), reads=["r_w2", "r_sumg"], writes=[f"wts{tt}"])
        for onm, src in (("o1", "oh1"), ("o2", "oh2")):
            o3 = o1 if onm == "o1" else o2
            Q.op(D_, lambda e, o3=o3: e.tensor_copy(out=o3, in_=v("ohg").unsqueeze(2).to_broadcast([128, 8, 8])), reads=["r_ohg"], writes=["r_" + onm])
            Q.op(D_, lambda e, o3=o3, src=src: e.tensor_tensor(out=o3, in0=o3, in1=v(src).unsqueeze(1).to_broadcast([128, 8, 8]), op=ALU.mult),
                 reads=["r_" + onm, "r_" + src], writes=["r_" + onm])
        Q.op(D_, lambda e: e.tensor_tensor(out=oh64, in0=v("o1"), in1=v("o2"), op=ALU.add), reads=["r_o1", "r_o2"], writes=["r_oh64"])
        Q.op("pe", lambda e: e.matmul(pP, lhsT=sut, rhs=oh64, start=True, stop=True), reads=["rsut", "r_oh64"], writes=["rpP"])
        Q.op("pe", lambda e: e.matmul(pC, lhsT=ones, rhs=oh64, start=True, stop=True), reads=["ones", "r_oh64"], writes=["rpC"])
        Q.op(D_, lambda e: e.tensor_tensor(out=v("posf"), in0=pP, in1=cnt, op=ALU.add), reads=["rpP", "rcnt"], writes=["r_posf"])
        Q.op(D_, lambda e: e.tensor_tensor(out=cnt, in0=cnt, in1=pC, op=ALU.add), reads=["rpC", "rcnt", "r_posf"], writes=["rcnt"])
        Q.op(D_, lambda e: e.tensor_scalar(out=v("posf"), in0=v("posf"), scalar1=float(CAP - 1), scalar2=None, op0=ALU.min), reads=["r_posf"], writes=["r_posf"])
        Q.op(D_, lambda e: e.tensor_tensor(out=v("posf"), in0=v("posf"), in1=eb, op=ALU.add), reads=["r_posf", "reb"], writes=["r_posf"])
        for k, (onm, snm) in enumerate((("o1", "s1"), ("o2", "s2"))):
            Q.op(D_, lambda e, onm=onm: e.tensor_tensor(out=v("tmp"), in0=v("posf"), in1=v(onm), op=ALU.mult), reads=["r_posf", "r_" + onm], writes=["r_tmp"])
            Q.op(D_, lambda e, snm=snm: e.reduce_sum(out=v(snm), in_=v("tmp"), axis=AX.X), reads=["r_tmp"], writes=["r_" + snm])
            Q.op(D_, lambda e, snm=snm, tt=tt, k=k: e.tensor_copy(out=slots_i[:, tt, k:k + 1], in_=v(snm)), reads=["r_" + snm], writes=[f"slot{tt}_{k}"])
            Q.op("pool", lambda e, tt=tt, k=k, i=i: e.indirect_dma_start(
                out=Xd, out_offset=bass.IndirectOffsetOnAxis(ap=slots_i[:, tt, k:k + 1], axis=0), in_=hnb[i], in_offset=None),
                reads=[f"slot{tt}_{k}", f"rhnb{i}"] + zero_done, writes=(), dma=True)

    SHARED = ("rgf", "rbr", "reb", "rsut", "rwr", "rcnt", "identf", "ones", "onesf", "wts", "slot", "rzt", "Xz")

    class LaneRec:
        def __init__(self, lane):
            self.items, self.lane = [], lane

        def _k(self, keys):
            return [k if k.startswith(SHARED) else f"{k}#{self.lane}" for k in keys]

        def op(self, eng, fn, reads=(), writes=(), dma=False):
            self.items.append(("op", (eng, fn, self._k(reads), self._k(writes)), dict(dma=dma)))

        def dma(self, out, in_, reads=(), writes=(), q="sp"):
            self.items.append(("dma", (out, in_, self._k(reads), self._k(writes)), dict(q=q)))

    streams = []
    for tt in range(16):
        r = LaneRec(tt % 2)
        route_tile(tt, r)
        streams.append(r.items)
    m = max(len(r) for r in streams)
    order = sorted((j * (m // 2) + k, j, k) for j, r in enumerate(streams) for k in range(len(r)))
    for _, j, k in order:
        kind, a, kw = streams[j][k]
        (P.op if kind == "op" else P.dma)(*a, **kw)
    st.close()


def stage_experts(nc, P, cst, Xd, wgu_d, wdn_d, Yd, nexp=64):
    st = Stage(nc, P)
    ident = cst["ident"]
    xe = [st.sb(f"xe{i}", [128, D], BF16) for i in range(2)]
    xeT = [st.sb(f"xeT{i}", [128, 32, 128], BF16) for i in range(2)]
    wg = [st.sb(f"wg{i}", [128, 8, 1536], BF16) for i in range(2)]
    wd = [st.sb(f"wd{i}", [128, 6, 2048], BF16) for i in range(2)]
    sg = st.sb("sg", [128, 768], F32)
    usb = st.sb("usb", [128, 256], F32)
    hh = st.sb("hh", [128, 768], BF16)
    hT = st.sb("hT", [128, 6, 128], BF16)
    ysb = [st.sb(f"ysb{i}", [128, D], F32) for i in range(2)]
    pXb = [st.ps(f"epX{i}", [128, 8, 128], BF16) for i in range(2)]
    pGU = [st.ps(f"epGU{i}", [128, 512], F32) for i in range(3)]
    pHT = st.ps("epHT", [128, 8, 128], BF16)
    pY = [st.ps(f"epY{i}", [128, 512], F32) for i in range(2)]
    gcnt = 0
    dcnt = 0
    for ex in range(nexp):
        i = ex % 2
        P.dma(xe[i], Xd[ex * 128:(ex + 1) * 128, :], writes=[f"xe{i}"])
        for g4 in range(8):
            hf = g4 % 2
            for j in range(4):
                c = g4 * 4 + j
                P.op("pe", lambda e, c=c, j=j, hf=hf, i=i: e.transpose(out=pXb[hf][:, j, :], in_=xe[i][:, c * 128:(c + 1) * 128], identity=ident),
                     reads=[f"xe{i}", "ident"], writes=[f"epX{hf}"])
            P.op("act", lambda e, g4=g4, hf=hf, i=i: e.copy(out=xeT[i][:, g4 * 4:(g4 + 1) * 4, :], in_=pXb[hf][:, 0:4, :]),
                 reads=[f"epX{hf}"], writes=[f"xeT{i}_{g4}", f"epX{hf}"])
        for g8 in range(4):
            j = gcnt % 2
            gcnt += 1
            P.dma(wg[j], wgu_d[ex, g8 * 1024:(g8 + 1) * 1024, :].rearrange("(c p) n -> p c n", p=128), writes=[f"wg{j}"], q="pool")
            for c8 in range(8):
                kc = g8 * 8 + c8
                for nt in range(3):
                    P.op("pe", lambda e, kc=kc, c8=c8, nt=nt, j=j, i=i: e.matmul(pGU[nt], lhsT=xeT[i][:, kc, :], rhs=wg[j][:, c8, nt * 512:(nt + 1) * 512],
                                                                              start=(kc == 0), stop=(kc == 31)),
                         reads=[f"xeT{i}_{kc // 4}", f"wg{j}"], writes=[f"epGU{nt}"])
        P.op("act", lambda e: e.activation(out=sg[:, 0:512], in_=pGU[0], func=AF.Silu), reads=["epGU0"], writes=["sg", "epGU0"])
        P.op("act", lambda e: e.activation(out=sg[:, 512:768], in_=pGU[1][:, 0:256], func=AF.Silu), reads=["epGU1"], writes=["sg", "epGU1"])
        P.op("act", lambda e: e.copy(out=usb, in_=pGU[1][:, 256:512]), reads=["epGU1"], writes=["usb", "epGU1"])
        P.op("dve", lambda e: e.tensor_tensor(out=hh[:, 256:768], in0=pGU[2], in1=sg[:, 256:768], op=ALU.mult), reads=["epGU2", "sg"], writes=["hh", "epGU2"])
        P.op("dve", lambda e: e.tensor_tensor(out=hh[:, 0:256], in0=usb, in1=sg[:, 0:256], op=ALU.mult), reads=["usb", "sg"], writes=["hh"])
        for fc in range(6):
            P.op("pe", lambda e, fc=fc: e.transpose(out=pHT[:, fc, :], in_=hh[:, fc * 128:(fc + 1) * 128], identity=ident), reads=["hh", "ident"], writes=["epHT"])
        P.op("act", lambda e: e.copy(out=hT, in_=pHT[:, 0:6, :]), reads=["epHT"], writes=["hT", "epHT"])
        yi = ex % 2
        for hf in range(2):
            j = dcnt % 2
            dcnt += 1
            P.dma(wd[j], wdn_d[ex, :, hf * 2048:(hf + 1) * 2048].rearrange("(c p) n -> p c n", p=128), writes=[f"wd{j}"], q="pool")
            for c4 in range(4):
                ct = hf * 4 + c4
                pb = ct % 2
                for fc in range(6):
                    P.op("pe", lambda e, fc=fc, c4=c4, pb=pb, j=j: e.matmul(pY[pb], lhsT=hT[:, fc, :], rhs=wd[j][:, fc, c4 * 512:(c4 + 1) * 512],
                                                                         start=(fc == 0), stop=(fc == 5)),
                         reads=["hT", f"wd{j}"], writes=[f"epY{pb}"])
                evac(P, pb, ysb[yi][:, ct * 512:(ct + 1) * 512], pY[pb], [f"epY{pb}"], [f"ysb{yi}_{ct}", f"epY{pb}"])
        P.dma(Yd[ex * 128:(ex + 1) * 128, :], ysb[yi], reads=[f"ysb{yi}_{ct}" for ct in range(8)])
    st.close()


def stage_final(nc, P, Hd, Yd, gfin_d, slots_i, wts, out_d):
    st = Stage(nc, P)
    ht = [st.sb(f"fh{i}", [128, D], F32) for i in range(2)]
    y1 = [st.sb(f"fy1{i}", [128, D], F32) for i in range(2)]
    y2 = [st.sb(f"fy2{i}", [128, D], F32) for i in range(2)]
    gf = st.sb("fgf", [128, D], F32)
    junk = st.sb("fjunk", [128, D], BF16)
    ss = st.sb("fss", [128, 2], F32)
    P.dma(gf, gfin_d.partition_broadcast(128), writes=["fgf"])
    for tt in range(16):
        i = tt % 2
        r0 = tt * 128
        P.dma(ht[i], Hd[r0:r0 + 128, :], writes=[f"fh{i}"])
        for k, yb in enumerate((y1, y2)):
            P.op("pool", lambda e, tt=tt, k=k, yb=yb, i=i: e.indirect_dma_start(
                out=yb[i], out_offset=None, in_=Yd, in_offset=bass.IndirectOffsetOnAxis(ap=slots_i[:, tt, k:k + 1], axis=0)),
                reads=(), writes=[f"fy{k}{i}"], dma=True)
        P.op("dve", lambda e, i=i, tt=tt: e.scalar_tensor_tensor(out=ht[i], in0=y1[i], scalar=wts[:, tt, 0:1], in1=ht[i], op0=ALU.mult, op1=ALU.add),
             reads=[f"fy0{i}", f"fh{i}"], writes=[f"fh{i}"])
        P.op("dve", lambda e, i=i, tt=tt: e.scalar_tensor_tensor(out=ht[i], in0=y2[i], scalar=wts[:, tt, 1:2], in1=ht[i], op0=ALU.mult, op1=ALU.add),
             reads=[f"fy1{i}", f"fh{i}"], writes=[f"fh{i}"])
        P.op("act", lambda e, i=i: e.activation(out=junk, in_=ht[i], func=AF.Square, accum_out=ss[:, i:i + 1]), reads=[f"fh{i}"], writes=["fjunk", f"fss{i}"])
        rsqrt_col(P, ss[:, i:i + 1], ss[:, i:i + 1], EPS, f"fss{i}", f"fss{i}", mul=1.0 / D)
        P.op("dve", lambda e, i=i: e.scalar_tensor_tensor(out=y1[i], in0=ht[i], scalar=ss[:, i:i + 1], in1=gf, op0=ALU.mult, op1=ALU.mult),
             reads=[f"fh{i}", f"fss{i}", "fgf"], writes=[f"fy0{i}"])
        P.dma(out_d[r0:r0 + 128, :], y1[i], reads=[f"fy0{i}"])
    st.close()


def stage_tail(nc, P, x_d, gfin_d, out_d):
    st = Stage(nc, P)
    xt = [st.sb(f"tx{i}", [128, D], F32) for i in range(2)]
    ot = [st.sb(f"to{i}", [128, D], F32) for i in range(2)]
    gf = st.sb("tgf", [128, D], F32)
    junk = st.sb("tjunk", [128, D], BF16)
    ss = st.sb("tss", [128, 2], F32)
    P.dma(gf, gfin_d.partition_broadcast(128), writes=["gf"])
    for t in range(16):
        i = t % 2
        r0 = OWN0 + t * 128
        P.dma(xt[i], x_d[r0:r0 + 128, :], writes=[f"tx{i}"])
        P.op("act", lambda e, i=i: e.activation(out=junk, in_=xt[i], func=AF.Square, accum_out=ss[:, i:i + 1]),
             reads=[f"tx{i}"], writes=["tjunk", f"tss{i}"])
        rsqrt_col(P, ss[:, i:i + 1], ss[:, i:i + 1], EPS, f"tss{i}", f"tss{i}", mul=1.0 / D)
        P.op("dve", lambda e, i=i: e.scalar_tensor_tensor(out=ot[i], in0=xt[i], scalar=ss[:, i:i + 1], in1=gf, op0=ALU.mult, op1=ALU.mult),
             reads=[f"tx{i}", f"tss{i}", "gf"], writes=[f"to{i}"])
        P.dma(out_d[t * 128:(t + 1) * 128, :], ot[i], reads=[f"to{i}"])
    st.close()


class SplitRows:
    def __init__(self, parts, split):
        self.parts, self.split = parts, split

    def __getitem__(self, key):
        rs, cs = key
        if rs.stop <= self.split:
            return self.parts[0][rs.start:rs.stop, cs]
        assert rs.start >= self.split
        return self.parts[1][rs.start - self.split:rs.stop - self.split, cs]


def build_program(debug=False):
    nc = bass.Bass("TRN2", target_bir_lowering=False)
    ext = lambda name, shape, dt=F32: nc.dram_tensor(name, shape, dt, kind="ExternalInput").ap()
    x_d = ext("xs", [TL, D])
    gmix_d = ext("g_mix", [D])
    win_d = ext("w_in", [D, NCOL])
    convw = ext("convw", [128, 48, 4])
    alog = ext("a_log", [16])
    dtbias = ext("dt_bias", [16])
    gnormw = ext("gdn_norm_w", [128])
    gmk_d = ext("gdn_lvl_masks", [128, 8, 128], BF16)
    KD = {nm: ext("k_" + nm, shp, dt) for nm, shp, dt in NSA_CONST_SPECS}
    w1k, w2k, pek = ext("cmp_w1_k", [32, 128, 128]), ext("cmp_w2_k", [128, 128]), ext("cmp_peT_k", [128, 32])
    w1v, w2v, pev = ext("cmp_w1_v", [32, 128, 128]), ext("cmp_w2_v", [128, 128]), ext("cmp_peT_v", [128, 32])
    wa_d, wb_d, wo_d = ext("w_branch_gdn", [2048, D]), ext("w_branch_nsa", [2048, D]), ext("w_out", [D, D])
    gffn_d = ext("g_ffn", [D])
    wr_d, br_d, eb_d = ext("w_router", [128, 32, 72]), ext("b_router", [72]), ext("ebase", [64])
    wgu_d, wdn_d = ext("w_gate_up", [64, D, 1536]), ext("w_down", [64, 768, D])
    gfin = ext("g_final", [D])
    out_d = nc.dram_tensor("out", [TL - OWN0, D], F32, kind="ExternalOutput").ap()
    PT_d = SplitRows([nc.dram_tensor("PT_scratch_a", [C_NKV, TL], F32).ap(),
                      nc.dram_tensor("PT_scratch_b", [NCOL - C_NKV, TL], F32).ap()], C_NKV)
    sk = "ExternalOutput" if debug else "Internal"
    OG_d = nc.dram_tensor("OG_scratch", [2048, 2048], BF16, kind=sk).ap()
    ON_d = nc.dram_tensor("ON_scratch", [2048, 2048], BF16, kind=sk).ap()
    Hd = nc.dram_tensor("H_scratch", [2048, D], F32, kind=sk).ap()
    Xd = nc.dram_tensor("X_scratch", [64 * CAP, D], BF16).ap()
    Yd = nc.dram_tensor("Y_scratch", [64 * CAP, D], F32).ap()
    P = Prog(nc)
    cst = make_consts(nc, P)
    ab_tok = nc.alloc_sbuf_tensor("ab_tok", [128, 32, 32], F32).ap()
    slots_i = nc.alloc_sbuf_tensor("slots_i", [128, 16, 2], I32).ap()
    wts = nc.alloc_sbuf_tensor("wts", [128, 16, 2], F32).ap()
    stage_proj(nc, P, cst, x_d, gmix_d, win_d, PT_d, ab_tok)
    stage_gdn(nc, P, cst, PT_d, ab_tok, convw, alog, dtbias, gnormw, gmk_d, OG_d)
    stage_nsa(nc, P, cst, PT_d, KD, w1k, w2k, pek, w1v, w2v, pev, ON_d)
    stage_merge(nc, P, cst, PT_d, OG_d, ON_d, wa_d, wb_d, wo_d, Hd)
    stage_route(nc, P, cst, x_d, Hd, gffn_d, wr_d, br_d, eb_d, Xd, slots_i, wts)
    stage_experts(nc, P, cst, Xd, wgu_d, wdn_d, Yd)
    stage_final(nc, P, Hd, Yd, gfin, slots_i, wts, out_d)
    P.emit()
    return nc


def gdn_level_masks():
    import ml_dtypes
    c = np.arange(128)[:, None]
    s_ = np.arange(128)[None, :]
    m = np.zeros((128, 8, 128), np.float32)
    for k in range(7):
        mk = (((c >> k) & 1) == 1) & (((s_ >> k) & 1) == 0) & ((c >> (k + 1)) == (s_ >> (k + 1)))
        m[:, k, :] = mk.T
        if k == 0:
            m[:, 7, :] = mk
    return m.astype(ml_dtypes.bfloat16)


def host_shared(inputs):
    f = lambda k: np.ascontiguousarray(np.asarray(inputs[k])[0])
    wr = np.concatenate([f("w_group"), f("w_expert")], axis=1)
    return {
        "g_mix": f("g_mix"), "w_in": f("w_in"),
        "convw": np.ascontiguousarray(f("gdn_conv_w").reshape(4, 48, 128).transpose(2, 1, 0)),
        "gdn_lvl_masks": gdn_level_masks(),
        "a_log": f("gdn_a_log"), "dt_bias": f("gdn_dt_bias"), "gdn_norm_w": f("gdn_norm_w"),
        "cmp_w1_k": f("cmp_w1_k"), "cmp_w2_k": f("cmp_w2_k"), "cmp_peT_k": np.ascontiguousarray(f("cmp_pe_k").T),
        "cmp_w1_v": f("cmp_w1_v"), "cmp_w2_v": f("cmp_w2_v"), "cmp_peT_v": np.ascontiguousarray(f("cmp_pe_v").T),
        "w_branch_gdn": f("w_branch_gdn"), "w_branch_nsa": f("w_branch_nsa"), "w_out": f("w_out"),
        "g_ffn": f("g_ffn"),
        "w_router": np.ascontiguousarray(wr.reshape(32, 128, 72).transpose(1, 0, 2)),
        "b_router": np.concatenate([f("b_group"), f("b_expert")]),
        "ebase": (np.arange(64) * CAP).astype(np.float32),
        "w_gate_up": f("w_gate_up"), "w_down": f("w_down"),
        "g_final": np.ascontiguousarray(np.asarray(inputs["g_final"])),
    }


def kernel(**inputs):
    x = np.asarray(inputs["x"], dtype=np.float32)
    B, T, _ = x.shape
    nc = build_program()
    shared = host_shared(inputs)
    consts = [nsa_host_consts(0), nsa_host_consts(1)]
    in_maps = []
    for c in range(8):
        b, half = c // 2, c % 2
        xs = np.zeros((TL, D), np.float32)
        if half == 0:
            xs[OWN0:] = x[b, :2048]
        else:
            xs[:] = x[b]
        m = dict(shared)
        m["xs"] = xs
        for k, v in consts[half].items():
            m["k_" + k] = v
        in_maps.append(m)
    res = run_bass_kernel_spmd(nc, in_maps, core_ids=list(range(8)))
    out = np.zeros((B, T, D), np.float32)
    for c in range(8):
        b, half = c // 2, c % 2
        out[b, half * 2048:(half + 1) * 2048] = res.results[c]["out"]
    return out
```
